# Optimizing a Trainium2 kernel written in Bass

```python
import math
import jax, jax.numpy as jnp
from jax import lax
import numpy as np

D_MODEL = 1024
BATCH = 4
SEQ = 8192
DEPTH = 2

N_META = 16
BLOCK = 128
PAD = BLOCK - N_META
ATT_HEADS = 8
ATT_HEAD_DIM = 64
ATT_WIDTH = ATT_HEADS * ATT_HEAD_DIM
SSD_HEADS = 16
SSD_HEAD_DIM = 64
SSD_INNER = SSD_HEADS * SSD_HEAD_DIM
SSD_GROUPS = 2
SSD_HPG = SSD_HEADS // SSD_GROUPS
SSD_STATE = 128
SSD_CONV = 4
SSD_CONV_DIM = SSD_INNER + 2 * SSD_GROUPS * SSD_STATE
SC_WIDTH = 512
SC_CONV = 3
N_BRANCH = 3
FF_DENSE = 2816
N_EXPERTS = 8
TOP_K = 2
FF_EXPERT = 3584
N_DENSE = (DEPTH + 1) // 2
N_MOE = DEPTH // 2
ALPHA = (2 * DEPTH) ** 0.25
BETA = (8 * DEPTH) ** -0.25
LN_EPS = 1e-5
RMS_EPS = 1e-5
NEG_INF = -1e30
IN_SIZES = (ATT_WIDTH, ATT_WIDTH, ATT_WIDTH, ATT_HEADS,
            SSD_INNER, SSD_CONV_DIM, SSD_HEADS,
            3 * SC_WIDTH, N_BRANCH * D_MODEL)
N_IN = sum(IN_SIZES)

kernel_name = "hybrid_fox_ssd_shortconv_moe_deepnorm"


def layer_norm(x, g, b):
    xf = x.astype(jnp.float32)
    mu = jnp.mean(xf, axis=-1, keepdims=True)
    var = jnp.mean(jnp.square(xf - mu), axis=-1, keepdims=True)
    return ((xf - mu) * lax.rsqrt(var + LN_EPS) * g + b).astype(x.dtype)


def front_pad(t):
    return jnp.pad(t, [(0, 0), (PAD, 0)] + [(0, 0)] * (t.ndim - 2))


def causal_depthwise_conv(u, w):
    k_w, c = w.shape
    return lax.conv_general_dilated(
        u, w[:, None, :].astype(u.dtype), window_strides=(1,),
        padding=[(k_w - 1, 0)], dimension_numbers=("NWC", "WIO", "NWC"),
        feature_group_count=c)


def forgetting_attention(q, k, v, log_f):
    b, lp, h, hd = q.shape
    n_blocks = lp // BLOCK
    c_t = jnp.moveaxis(jnp.cumsum(log_f, axis=1), -1, 1)
    kpos = jnp.arange(lp)
    key_valid = kpos >= PAD
    scale = hd ** -0.5

    def one_block(i):
        start = i * BLOCK
        qb = lax.dynamic_slice_in_dim(q, start, BLOCK, axis=1)
        cq = lax.dynamic_slice_in_dim(c_t, start, BLOCK, axis=2)
        s = jnp.einsum("bqhd,bkhd->bhqk", qb, k,
                       preferred_element_type=jnp.float32) * scale
        s = s + (cq[..., :, None] - c_t[..., None, :])
        qpos = start + jnp.arange(BLOCK)
        mask = (kpos[None, :] <= qpos[:, None]) & key_valid[None, :]
        p = jax.nn.softmax(jnp.where(mask[None, None], s, NEG_INF), axis=-1)
        return jnp.einsum("bhqk,bkhd->bqhd", p.astype(v.dtype), v)

    out = lax.map(one_block, jnp.arange(n_blocks))
    return jnp.moveaxis(out, 0, 1).reshape(b, lp, h, hd)


def ssd_chunked(x, dt, a, bm, cm):
    b, lp, g, r, p = x.shape
    nc = lp // BLOCK
    x = x.reshape(b, nc, BLOCK, g, r, p)
    dt = dt.reshape(b, nc, BLOCK, g, r)
    bm = bm.reshape(b, nc, BLOCK, g, -1)
    cm = cm.reshape(b, nc, BLOCK, g, -1)
    a_cum = jnp.cumsum(dt * a, axis=2)
    xdt = x * dt[..., None]
    seg = a_cum[:, :, :, None] - a_cum[:, :, None, :]
    causal = jnp.tril(jnp.ones((BLOCK, BLOCK), dtype=bool))[:, :, None, None]
    decay = jnp.exp(jnp.where(causal, seg, -jnp.inf))
    cb = jnp.einsum("bclgn,bcsgn->bclsg", cm, bm)
    y_diag = jnp.einsum("bclsg,bclsgr,bcsgrp->bclgrp", cb, decay, xdt)
    decay_states = jnp.exp(a_cum[:, :, -1:] - a_cum)
    states = jnp.einsum("bclgn,bclgr,bclgrp->bcgrpn", bm, decay_states, xdt)
    chunk_decay = jnp.exp(a_cum[:, :, -1])

    def step(h, inp):
        s_c, d_c = inp
        return h * d_c[..., None, None] + s_c, h

    h0 = jnp.zeros(states.shape[:1] + states.shape[2:], states.dtype)
    _, prev = lax.scan(step, h0, (jnp.moveaxis(states, 1, 0), jnp.moveaxis(chunk_decay, 1, 0)))
    prev = jnp.moveaxis(prev, 0, 1)
    y_off = jnp.einsum("bclgn,bcgrpn,bclgr->bclgrp", cm, prev, jnp.exp(a_cum))
    return (y_diag + y_off).reshape(b, lp, g, r, p)


def gated_group_rmsnorm(y, z, w):
    b, l, d = y.shape
    u = (y * jax.nn.silu(z)).astype(jnp.float32).reshape(b, l, SSD_GROUPS, d // SSD_GROUPS)
    u = u * lax.rsqrt(jnp.mean(jnp.square(u), axis=-1, keepdims=True) + RMS_EPS)
    return u.reshape(b, l, d) * w


def hybrid_mixer(x, w_in, b_forget, ssd_conv_w, ssd_conv_b, ssd_dt_bias, ssd_a_log,
                 ssd_d, ssd_norm_w, sc_conv_w, w_proj_attn, w_proj_ssd, w_proj_conv, w_out):
    b, l, _ = x.shape
    proj = x @ w_in
    split_idx = np.cumsum(IN_SIZES)[:-1].tolist()
    q, k, v, f_logit, z, xbc, dt_raw, sc_in, gate_logit = jnp.split(proj, split_idx, axis=-1)

    def heads(t):
        return front_pad(t.reshape(b, l, ATT_HEADS, ATT_HEAD_DIM))
    log_f = jax.nn.log_sigmoid((f_logit + b_forget).astype(jnp.float32))
    y_att = forgetting_attention(heads(q), heads(k), heads(v), front_pad(log_f))
    y_att = y_att[:, PAD:].reshape(b, l, ATT_WIDTH)

    xbc = jax.nn.silu(causal_depthwise_conv(xbc, ssd_conv_w) + ssd_conv_b)
    xs, bs, cs = jnp.split(xbc, [SSD_INNER, SSD_INNER + SSD_GROUPS * SSD_STATE], axis=-1)
    dt = jax.nn.softplus((dt_raw + ssd_dt_bias).astype(jnp.float32))
    a = -jnp.exp(ssd_a_log.astype(jnp.float32)).reshape(SSD_GROUPS, SSD_HPG)
    xs_h = xs.reshape(b, l, SSD_GROUPS, SSD_HPG, SSD_HEAD_DIM)
    y = ssd_chunked(front_pad(xs_h),
                    front_pad(dt.reshape(b, l, SSD_GROUPS, SSD_HPG)), a,
                    front_pad(bs.reshape(b, l, SSD_GROUPS, SSD_STATE)),
                    front_pad(cs.reshape(b, l, SSD_GROUPS, SSD_STATE)))[:, PAD:]
    y = y + ssd_d.reshape(SSD_GROUPS, SSD_HPG)[:, :, None] * xs_h
    y_ssd = gated_group_rmsnorm(y.reshape(b, l, SSD_INNER), z, ssd_norm_w)

    sc_b, sc_c, sc_h = jnp.split(sc_in, 3, axis=-1)
    y_conv = sc_b * causal_depthwise_conv(sc_c * sc_h, sc_conv_w)

    g_att, g_ssd, g_conv = jnp.split(jax.nn.sigmoid(gate_logit), N_BRANCH, axis=-1)
    merged = (g_att * (y_att @ w_proj_attn)
              + g_ssd * (y_ssd @ w_proj_ssd)
              + g_conv * (y_conv @ w_proj_conv))
    return merged @ w_out


def swiglu(t, w_gu, w_down):
    gate, up = jnp.split(t @ w_gu, 2, axis=-1)
    return (jax.nn.silu(gate) * up) @ w_down


def moe_swiglu(x, router_w, router_b, w_gu, w_down):
    b, l, d = x.shape
    t = x.reshape(b * l, d)
    logits = (t @ router_w + router_b).astype(jnp.float32)
    top_val, top_idx = lax.top_k(logits, TOP_K)
    top_w = jax.nn.softmax(top_val, axis=-1)
    combine = jnp.sum(jax.nn.one_hot(top_idx, N_EXPERTS, dtype=jnp.float32) * top_w[..., None], axis=1)
    out = jnp.zeros((b * l, d), jnp.float32)
    for e in range(N_EXPERTS):
        out = out + combine[:, e:e + 1] * swiglu(t, w_gu[e], w_down[e])
    return out.reshape(b, l, d).astype(x.dtype)


def setup_inputs(seed: int = 0) -> dict:
    key = jax.random.key(seed)
    ks = jax.random.split(key, 32)
    f32 = jnp.float32

    def nrm(k, shape, scale):
        return jax.random.normal(k, shape, f32) * scale

    dt0 = jnp.exp(jax.random.uniform(ks[8], (DEPTH, SSD_HEADS), f32,
                                     math.log(0.001), math.log(0.1)))
    return {
        "x": nrm(ks[0], (BATCH, SEQ, D_MODEL), 1.0),
        "meta_tokens": nrm(ks[1], (N_META, D_MODEL), 1.0),
        "ln_in_g": 1.0 + nrm(ks[2], (D_MODEL,), 0.02),
        "ln_in_b": nrm(ks[3], (D_MODEL,), 0.02),
        "w_in": nrm(ks[4], (DEPTH, D_MODEL, N_IN), D_MODEL ** -0.5),
        "b_forget": 2.0 + nrm(ks[5], (DEPTH, ATT_HEADS), 0.5),
        "ssd_conv_w": nrm(ks[6], (DEPTH, SSD_CONV, SSD_CONV_DIM), SSD_CONV ** -0.5),
        "ssd_conv_b": nrm(ks[7], (DEPTH, SSD_CONV_DIM), 0.02),
        "ssd_dt_bias": dt0 + jnp.log(-jnp.expm1(-dt0)),
        "ssd_a_log": jnp.log(jax.random.uniform(ks[9], (DEPTH, SSD_HEADS), f32, 1.0, 16.0)),
        "ssd_d": 1.0 + nrm(ks[10], (DEPTH, SSD_HEADS), 0.02),
        "ssd_norm_w": 1.0 + nrm(ks[11], (DEPTH, SSD_INNER), 0.02),
        "sc_conv_w": nrm(ks[12], (DEPTH, SC_CONV, SC_WIDTH), SC_CONV ** -0.5),
        "w_proj_attn": nrm(ks[13], (DEPTH, ATT_WIDTH, D_MODEL), ATT_WIDTH ** -0.5),
        "w_proj_ssd": nrm(ks[14], (DEPTH, SSD_INNER, D_MODEL), SSD_INNER ** -0.5),
        "w_proj_conv": nrm(ks[15], (DEPTH, SC_WIDTH, D_MODEL), SC_WIDTH ** -0.5),
        "w_out": nrm(ks[16], (DEPTH, D_MODEL, D_MODEL), BETA * D_MODEL ** -0.5),
        "ln_mix_g": 1.0 + nrm(ks[17], (DEPTH, D_MODEL), 0.02),
        "ln_mix_b": nrm(ks[18], (DEPTH, D_MODEL), 0.02),
        "dense_w_gu": nrm(ks[19], (N_DENSE, D_MODEL, 2 * FF_DENSE), D_MODEL ** -0.5),
        "dense_w_down": nrm(ks[20], (N_DENSE, FF_DENSE, D_MODEL), BETA * FF_DENSE ** -0.5),
        "router_w": nrm(ks[21], (N_MOE, D_MODEL, N_EXPERTS), D_MODEL ** -0.5),
        "router_b": nrm(ks[22], (N_MOE, N_EXPERTS), 0.01),
        "moe_w_gu": nrm(ks[23], (N_MOE, N_EXPERTS, D_MODEL, 2 * FF_EXPERT), D_MODEL ** -0.5),
        "moe_w_down": nrm(ks[24], (N_MOE, N_EXPERTS, FF_EXPERT, D_MODEL), BETA * FF_EXPERT ** -0.5),
        "ln_ffn_g": 1.0 + nrm(ks[25], (DEPTH, D_MODEL), 0.02),
        "ln_ffn_b": nrm(ks[26], (DEPTH, D_MODEL), 0.02),
    }


def reference(x, meta_tokens, ln_in_g, ln_in_b, w_in, b_forget, ssd_conv_w, ssd_conv_b,
              ssd_dt_bias, ssd_a_log, ssd_d, ssd_norm_w, sc_conv_w, w_proj_attn, w_proj_ssd,
              w_proj_conv, w_out, ln_mix_g, ln_mix_b, dense_w_gu, dense_w_down, router_w,
              router_b, moe_w_gu, moe_w_down, ln_ffn_g, ln_ffn_b):
    b = x.shape[0]
    meta = jnp.broadcast_to(meta_tokens.astype(x.dtype)[None], (b, N_META, D_MODEL))
    h = layer_norm(jnp.concatenate([meta, x], axis=1), ln_in_g, ln_in_b)
    for layer in range(DEPTH):
        mix = hybrid_mixer(h, w_in[layer], b_forget[layer], ssd_conv_w[layer], ssd_conv_b[layer],
                           ssd_dt_bias[layer], ssd_a_log[layer], ssd_d[layer], ssd_norm_w[layer],
                           sc_conv_w[layer], w_proj_attn[layer], w_proj_ssd[layer],
                           w_proj_conv[layer], w_out[layer])
        h = layer_norm(ALPHA * h + mix, ln_mix_g[layer], ln_mix_b[layer])
        j = layer // 2
        if layer % 2 == 0:
            ff = swiglu(h, dense_w_gu[j], dense_w_down[j])
        else:
            ff = moe_swiglu(h, router_w[j], router_b[j], moe_w_gu[j], moe_w_down[j])
        h = layer_norm(ALPHA * h + ff, ln_ffn_g[layer], ln_ffn_b[layer])
    return h[:, N_META:]
```

```python
import numpy as np
from contextlib import ExitStack
import concourse.bass as bass
import concourse.mybir as mybir
from concourse.bass_utils import run_bass_kernel_spmd

F32 = mybir.dt.float32
BF16 = mybir.dt.bfloat16
AF = mybir.ActivationFunctionType
ALU = mybir.AluOpType
AX = mybir.AxisListType

D = 1024
KC = 8
NQ = 512
FF_DENSE = 2816
FF_EXP = 3584
NEXP = 8
ALPHA = 4 ** 0.25
LN_EPS = 1e-5
RMS_EPS = 1e-5
O_Q, O_K, O_V, O_F, O_Z, O_XBC, O_DT, O_SC, O_G = 0, 512, 1024, 1536, 1544, 2568, 4104, 4120, 5656
N_IN = 8728


class Res:
    __slots__ = ("w", "r")

    def __init__(self):
        self.w = None
        self.r = []


class Eng:
    def __init__(self, name, h, sem):
        self.name, self.h, self.sem = name, h, sem
        self.count = 0
        self.known = {}


class Ctx:
    def __init__(self, nc, st, n_dma_sems=24):
        self.nc = nc
        self.res = {}
        self.eng = {}
        for name, h in (("pe", nc.tensor), ("act", nc.scalar), ("dve", nc.vector), ("pool", nc.gpsimd), ("sp", nc.sync)):
            self.eng[name] = Eng(name, h, st.enter_context(nc.semaphore("sem_" + name)))
        self.dsems = [st.enter_context(nc.semaphore("dsem%d" % i)) for i in range(2 * n_dma_sems)]
        self.dval = [0] * len(self.dsems)
        self.dq = {"sp": list(range(0, n_dma_sems)), "pool": list(range(n_dma_sems, 2 * n_dma_sems))}
        self.dqi = {"sp": 0, "pool": 0}

    def R(self, key):
        r = self.res.get(key)
        if r is None:
            r = self.res[key] = Res()
        return r

    def _wait(self, E, deps, raw_self_only):
        need = {}
        for d in deps:
            k = (d[0], d[1])
            if need.get(k, 0) < d[2]:
                need[k] = d[2]
        for (kind, key), val in need.items():
            if kind == "e" and key == E.name:
                if E.name in ("pe", "sp"):
                    continue
            if E.known.get((kind, key), 0) >= val:
                continue
            sem = self.eng[key].sem if kind == "e" else self.dsems[key]
            E.h.wait_ge(sem, val)
            E.known[(kind, key)] = val

    def _collect(self, reads, writes):
        deps, raw = [], set()
        for k in reads:
            r = self.R(k)
            if r.w is not None:
                deps.append(r.w)
                raw.add(r.w)
        for k in writes:
            r = self.R(k)
            if r.w is not None:
                deps.append(r.w)
            deps.extend(r.r)
        return deps, raw

    def _commit(self, tok, reads, writes):
        for k in reads:
            self.R(k).r.append(tok)
        for k in writes:
            r = self.R(k)
            r.w = tok
            r.r = []

    def op(self, en, fn, reads=(), writes=()):
        E = self.eng[en]
        deps, raw = self._collect(reads, writes)
        self._wait(E, deps, raw)
        inst = fn(E.h)
        E.count += 1
        inst.then_inc(E.sem, 1)
        self._commit(("e", en, E.count), reads, writes)

    def dma(self, q, out, in_, reads=(), writes=(), **kw):
        E = self.eng[q]
        lst = self.dq[q]
        si = lst[self.dqi[q] % len(lst)]
        self.dqi[q] += 1
        deps, raw = self._collect(reads, writes)
        if self.dval[si] > 0:
            deps.append(("d", si, self.dval[si]))
        self._wait(E, deps, raw)
        E.h.dma_start(out=out, in_=in_, **kw).then_inc(self.dsems[si], 16)
        self.dval[si] += 16
        self._commit(("d", si, self.dval[si]), reads, writes)

    def barrier(self):
        deps = [("e", n, e.count) for n, e in self.eng.items() if e.count > 0 and n != "sp"]
        deps += [("d", i, v) for i, v in enumerate(self.dval) if v > 0]
        for E in self.eng.values():
            self._wait(E, deps, set(deps))
        self.res = {}


def _blocks(nb, g):
    out = []
    b = 0
    while b < nb:
        n = min(g, nb - b)
        out.append((b, n))
        b += n
    return out


def build_program(NB, debug=(), STAGES=("ln_in", "win0", "cprep0", "attn0")):
    T = NB * 128
    HB = (NB - 1) // 2
    TH = HB * 128
    nc = bass.Bass("TRN2", target_bir_lowering=False)
    dbg = set(debug)

    in_names = []

    def dram_in(name, shape, dt=F32):
        if name.startswith("moe_w") and "moe" not in STAGES:
            return None
        in_names.append(name)
        return nc.dram_tensor(name, list(shape), dt, kind="ExternalInput").ap()

    def dram_scr(name, shape, dt):
        kind = "ExternalOutput" if name in dbg else "Internal"
        return nc.dram_tensor(name, list(shape), dt, kind=kind).ap()

    xin = dram_in("xin", [T, D])
    ln_in_gb = dram_in("ln_in_gb", [2, D])
    w_in = dram_in("w_in", [2, D, N_IN])
    b_forget = dram_in("b_forget", [2, 8])
    ssd_conv_w = dram_in("ssd_conv_w", [2, 4, 1536])
    ssd_conv_b = dram_in("ssd_conv_b", [2, 1536])
    ssd_vec = dram_in("ssd_vec", [2, 3, 16])
    ssd_norm_w = dram_in("ssd_norm_w", [2, 1024])
    sc_conv_w = dram_in("sc_conv_w", [2, 3, 512])
    w_proj = dram_in("w_proj", [2, 2048, D])
    w_out = dram_in("w_out", [2, D, D])
    ln_mix_gb = dram_in("ln_mix_gb", [2, 2, D])
    dense_w_gu = dram_in("dense_w_gu", [D, 2 * FF_DENSE])
    dense_w_down = dram_in("dense_w_down", [FF_DENSE, D])
    router_w = dram_in("router_w", [D, NEXP])
    router_b = dram_in("router_b", [1, NEXP])
    moe_w_gu = dram_in("moe_w_gu", [NEXP, D, 2 * FF_EXP])
    moe_w_down = dram_in("moe_w_down", [NEXP, FF_EXP, D])
    ln_ffn_gb = dram_in("ln_ffn_gb", [2, 2, D])
    halfsel = dram_in("halfsel", [128, 2])
    out = nc.dram_tensor("out", [TH, D], F32, kind="ExternalOutput").ap()

    hT = [dram_scr("hT%d" % i, [KC, 128, T], F32) for i in range(2)]
    qkT = dram_scr("qkT", [8, 128, T], BF16)
    xbcT = dram_scr("xbcT", [12, 128, T + 128], BF16)
    scT = dram_scr("scT", [12, 128, T + 128], BF16)
    gT = dram_scr("gT", [24, 128, T], BF16)
    vtm = dram_scr("vtm", [T, 512], BF16)
    ztm = dram_scr("ztm", [T, 1024], BF16)
    dtr = dram_scr("dtr", [T, 16], F32)
    fT = dram_scr("fT", [8, T], F32)
    caugQ = dram_scr("caugQ", [8, 6, T], BF16)
    caugK = dram_scr("caugK", [8, 6, T], BF16)
    yattT = dram_scr("yattT", [4, 128, T], BF16)
    yssdT = dram_scr("yssdT", [8, 128, T], BF16)

    with ExitStack() as st:
        cx = Ctx(nc, st)
        Rk = cx.R
        rec_list = [None]

        def op(en, fn, reads=(), writes=()):
            if rec_list[0] is not None:
                rec_list[0].append(("op", en, fn, tuple(reads), tuple(writes), None))
            else:
                cx.op(en, fn, reads, writes)

        def dma(q, out, in_, reads=(), writes=(), **kw):
            if rec_list[0] is not None:
                rec_list[0].append(("dma", q, (out, in_), tuple(reads), tuple(writes), kw))
            else:
                cx.dma(q, out, in_, reads, writes, **kw)

        def record(f, *a):
            rec_list[0] = []
            f(*a)
            lst = rec_list[0]
            rec_list[0] = None
            return lst

        def replay(item):
            kind, e, x, rd, wr, kw = item
            if kind == "op":
                cx.op(e, x, rd, wr)
            else:
                cx.dma(e, x[0], x[1], rd, wr, **kw)

        def replay_interleaved(l1, l2):
            i = j = 0
            n1, n2 = len(l1), len(l2)
            while i < n1 or j < n2:
                if j >= n2 or (i < n1 and i * n2 <= j * n1):
                    replay(l1[i])
                    i += 1
                else:
                    replay(l2[j])
                    j += 1

        uid = [0]

        def sb(name, shape, dt, stk):
            uid[0] += 1
            return stk.enter_context(nc.sbuf_tensor("%s_u%d" % (name, uid[0]), list(shape), dt))

        def ps(name, shape, dt, stk):
            uid[0] += 1
            return stk.enter_context(nc.psum_tensor("%s_u%d" % (name, uid[0]), list(shape), dt))

        ident = sb("ident", [128, 128], F32, st)
        identb = sb("identb", [128, 128], BF16, st)
        ones_f = sb("ones_f", [128, 128], F32, st)
        tri_le = sb("tri_le", [128, 128], F32, st)
        tri_leb = sb("tri_leb", [128, 128], BF16, st)
        smask = sb("smask", [128, 128], F32, st)
        validcol = sb("validcol", [128, 1], F32, st)
        hsel = sb("hsel", [128, 2], F32, st)

        def c_init(g):
            return g.memset(ones_f[:], 1.0)
        op("pool", c_init, writes=["ones_f"])
        op("pool", lambda g: g.memset(ident[:], 0.0), writes=["ident"])
        op("pool", lambda g: g.affine_select(out=ident[:], in_=ones_f[:], pattern=[[-1, 128]], base=0,
                                              channel_multiplier=1, compare_op=ALU.is_equal, fill=0.0),
           reads=["ones_f"], writes=["ident"])
        op("pool", lambda g: g.affine_select(out=tri_le[:], in_=ones_f[:], pattern=[[1, 128]], base=0,
                                              channel_multiplier=-1, compare_op=ALU.is_ge, fill=0.0),
           reads=["ones_f"], writes=["tri_le"])
        op("pool", lambda g: g.affine_select(out=smask[:], in_=ones_f[:], pattern=[[-1, 128]], base=0,
                                              channel_multiplier=1, compare_op=ALU.is_gt, fill=0.0),
           reads=["ones_f"], writes=["smask"])
        op("pool", lambda g: g.affine_select(out=validcol[:], in_=ones_f[:, 0:1], pattern=[[0, 1]], base=-112,
                                              channel_multiplier=1, compare_op=ALU.is_ge, fill=0.0),
           reads=["ones_f"], writes=["validcol"])
        op("dve", lambda v: v.tensor_copy(out=identb[:], in_=ident[:]), reads=["ident"], writes=["identb"])
        op("dve", lambda v: v.tensor_copy(out=tri_leb[:], in_=tri_le[:]), reads=["tri_le"], writes=["tri_leb"])
        dma("sp", hsel[:], halfsel[:, :], writes=["hsel"])

        def phase_ln_in(hT_out):
            with ExitStack() as s:
                gbc = sb("p0_gb", [128, 2, KC], F32, s)
                xt = [sb("p0_x%d" % i, [128, D], F32, s) for i in range(2)]
                xn = [sb("p0_xn%d" % i, [128, D], F32, s) for i in range(2)]
                stt = [sb("p0_st%d" % i, [128, 2, 6], F32, s) for i in range(2)]
                mv = [sb("p0_mv%d" % i, [128, 4], F32, s) for i in range(2)]
                ho = [sb("p0_ho%d" % i, [128, KC, 128], F32, s) for i in range(2)]
                pt = [ps("p0_pt%d" % i, [128, D], F32, s) for i in range(2)]
                with nc.allow_non_contiguous_dma(reason="tiny param"):
                    dma("sp", gbc[:], ln_in_gb.rearrange("t (c p) -> p t c", p=128), writes=["p0_gb"])
                for b in range(NB):
                    i = b % 2
                    kx, kn, ks, km, kh, kp = ("p0_x%d" % i, "p0_xn%d" % i, "p0_st%d" % i, "p0_mv%d" % i, "p0_ho%d" % i, "p0_pt%d" % i)
                    dma("sp", xt[i][:], xin[b * 128:(b + 1) * 128, :], writes=[kx])
                    for hh in range(2):
                        op("dve", lambda v, hh=hh: v.bn_stats(out=stt[i][:, hh, :], in_=xt[i][:, hh * 512:(hh + 1) * 512]),
                           reads=[kx], writes=[ks])
                    op("dve", lambda v: v.bn_aggr(out=mv[i][:, 0:2], in_=stt[i][:].rearrange("p a b -> p (a b)")),
                       reads=[ks], writes=[km])
                    op("act", lambda a: a.activation(out=mv[i][:, 2:3], in_=mv[i][:, 1:2], func=AF.Sqrt, bias=LN_EPS_T[:], scale=1.0),
                       reads=[km, "eps"], writes=[km])
                    op("dve", lambda v: v.reciprocal(out=mv[i][:, 3:4], in_=mv[i][:, 2:3]), reads=[km], writes=[km])
                    op("dve", lambda v: v.tensor_scalar(out=xn[i][:], in0=xt[i][:], scalar1=mv[i][:, 0:1], scalar2=mv[i][:, 3:4],
                                                        op0=ALU.subtract, op1=ALU.mult), reads=[kx, km], writes=[kn])
                    for c in range(KC):
                        op("pe", lambda p, c=c: p.transpose(out=pt[i][:, c * 128:(c + 1) * 128], in_=xn[i][:, c * 128:(c + 1) * 128], identity=ident[:]),
                           reads=[kn, "ident"], writes=[kp])
                    for c in range(KC):
                        op("act", lambda a, c=c: a.activation(out=ho[i][:, c, :], in_=pt[i][:, c * 128:(c + 1) * 128], func=AF.Identity,
                                                               scale=gbc[:, 0, c:c + 1], bias=gbc[:, 1, c:c + 1]),
                           reads=[kp, "p0_gb"], writes=[kh])
                    if b == 0:
                        op("dve", lambda v: v.memset(ho[i][:, :, 0:112], 0.0), reads=[], writes=[kh])
                    dma("pool", hT_out[:, :, b * 128:(b + 1) * 128].rearrange("c p t -> p c t"), ho[i][:], reads=[kh], writes=[("hT", b)])
                cx.barrier()

        LN_EPS_T = sb("eps_t", [128, 1], F32, st)
        op("pool", lambda g: g.memset(LN_EPS_T[:], LN_EPS), writes=["eps"])
        RMS_EPS_T = LN_EPS_T

        def evac(i, out_ap, in_ap, reads, writes):
            if i % 2 == 0:
                op("act", lambda a: a.activation(out=out_ap, in_=in_ap, func=AF.Copy), reads=reads, writes=writes)
            else:
                op("dve", lambda v: v.tensor_copy(out=out_ap, in_=in_ap), reads=reads, writes=writes)

        wst_i = [0]

        def mk_wstg(s):
            return [sb("wstg%d" % i, [128, 2048], F32, s) for i in range(3)]

        def load_w(dst, src2d, ncols, key, rows_c, wstg):
            for c in range(rows_c):
                n0 = 0
                while n0 < ncols:
                    n1 = min(ncols, n0 + 2048)
                    si = wst_i[0] % 3
                    wst_i[0] += 1
                    dma("sp", wstg[si][:, :n1 - n0], src2d[c * 128:(c + 1) * 128, n0:n1], writes=["wstg%d" % si])
                    if si == 0:
                        op("act", lambda a: a.activation(out=dst[:, c, n0:n1], in_=wstg[si][:, :n1 - n0], func=AF.Copy), reads=["wstg%d" % si], writes=[key])
                    elif si == 1:
                        op("dve", lambda v: v.tensor_copy(out=dst[:, c, n0:n1], in_=wstg[si][:, :n1 - n0]), reads=["wstg%d" % si], writes=[key])
                    else:
                        op("pool", lambda v: v.tensor_copy(out=dst[:, c, n0:n1], in_=wstg[si][:, :n1 - n0]), reads=["wstg%d" % si], writes=[key])
                    n0 = n1

        def phase_win(l, h_in):
            for pas in (0, 1):
                with ExitStack() as s:
                    if pas == 0:
                        segs = [(O_Q, 1024), (O_XBC, 1536), (O_V, 512), (O_Z, 1024), (O_F, 8), (O_DT, 16)]
                    else:
                        segs = [(O_SC, 1536), (O_G, 3072)]
                    ncols = sum(n for _, n in segs)
                    W = sb("p1_W", [128, KC, ncols], BF16, s)
                    wstg = mk_wstg(s)
                    off = {}
                    o = 0
                    for (c0, n) in segs:
                        load_w(W[:, :, o:o + n], w_in[l][:, c0:c0 + n], n, "p1_W", KC, wstg)
                        off[c0] = o
                        o += n
                    hf = [sb("p1_hf%d" % i, [128, KC, 512], F32, s) for i in range(2)]
                    hb = [sb("p1_hb%d" % i, [128, KC, 512], BF16, s) for i in range(2)]
                    ob = [sb("p1_ob%d" % i, [128, 4, 512], BF16, s) for i in range(3)]
                    pf = [ps("p1_pf%d" % i, [128, 512], F32, s) for i in range(4)]
                    if pas == 0:
                        zt = sb("p1_z", [128, 128], BF16, s)
                        op("dve", lambda v: v.memset(zt[:], 0.0), writes=["p1_z"])
                        for cc in range(12):
                            dma("pool", xbcT[cc, :, 0:128], zt[:], reads=["p1_z"], writes=[("xbcT", -1)])
                            dma("pool", scT[cc, :, 0:128], zt[:], reads=["p1_z"], writes=[("scT", -1)])
                        tv = sb("p1_tv", [128, 4, 512], BF16, s)
                        tz = sb("p1_tz", [128, 4, 1024], BF16, s)
                        tdt = sb("p1_tdt", [128, 4, 16], F32, s)
                        fst = sb("p1_fst", [8, 512], F32, s)
                        pt = [ps("p1_pt%d" % i, [128, 512], F32, s) for i in range(3)]
                    ei = 0
                    pi = 0
                    obi = 0
                    for gi, (b0, nb) in enumerate(_blocks(NB, 4)):
                        i = gi % 2
                        t0, nt = b0 * 128, nb * 128
                        dma("sp", hf[i][:, :, :nt], h_in[:, :, t0:t0 + nt].rearrange("c p t -> p c t"), writes=["p1_hf%d" % i])
                        for c in range(KC):
                            evac(c, hb[i][:, c, :nt], hf[i][:, c, :nt], ["p1_hf%d" % i], ["p1_hb%d" % i])
                        if pas == 0:
                            fm = [(O_Q, 8, qkT, 0, 0), (O_XBC, 12, xbcT, 0, 128)]
                        else:
                            fm = [(O_SC, 12, scT, 0, 128), (O_G, 24, gT, 0, 0)]
                        for (c0, nch, dst, dch, dof) in fm:
                            for n4 in range(0, nch, 4):
                                oi = obi % 3
                                obi += 1
                                for n in range(n4, n4 + 4):
                                    pp = pi % 4
                                    pi += 1
                                    col = off[c0] + n * 128
                                    for c in range(KC):
                                        op("pe", lambda p, c=c, col=col, pp=pp: p.matmul(pf[pp][:, :nt], lhsT=W[:, c, col:col + 128], rhs=hb[i][:, c, :nt],
                                                                                          start=(c == 0), stop=(c == KC - 1)),
                                           reads=["p1_W", "p1_hb%d" % i], writes=["p1_pf%d" % pp])
                                    evac(ei, ob[oi][:, n - n4, :nt], pf[pp][:, :nt], ["p1_pf%d" % pp], ["p1_ob%d" % oi])
                                    ei += 1
                                dma("pool", dst[dch + n4:dch + n4 + 4, :, dof + t0:dof + t0 + nt].rearrange("c p t -> p c t"), ob[oi][:, :, :nt],
                                    reads=["p1_ob%d" % oi], writes=[(id(dst), gi, n4)])
                        if pas == 0:
                            pp = pi % 4
                            pi += 1
                            col = off[O_F]
                            for c in range(KC):
                                op("pe", lambda p, c=c, col=col, pp=pp: p.matmul(pf[pp][0:8, :nt], lhsT=W[:, c, col:col + 8], rhs=hb[i][:, c, :nt],
                                                                                  start=(c == 0), stop=(c == KC - 1)),
                                   reads=["p1_W", "p1_hb%d" % i], writes=["p1_pf%d" % pp])
                            op("dve", lambda v, pp=pp: v.tensor_copy(out=fst[:, :nt], in_=pf[pp][0:8, :nt]), reads=["p1_pf%d" % pp], writes=["p1_fst"])
                            dma("pool", fT[:, t0:t0 + nt], fst[:, :nt], reads=["p1_fst"], writes=[("fT", gi)])
                            pti = 0
                            for bi in range(nb):
                                for (c0, n, dstt, do) in ((O_V, 512, tv, 0), (O_Z, 512, tz, 0), (O_Z + 512, 512, tz, 512), (O_DT, 16, tdt, 0)):
                                    pq = pti % 3
                                    pti += 1
                                    col = off[O_Z] + (c0 - O_Z) if c0 >= O_Z and c0 < O_XBC else off[c0]
                                    for c in range(KC):
                                        op("pe", lambda p, c=c, col=col, pq=pq, n=n: p.matmul(pt[pq][:, :n], lhsT=hb[i][:, c, bi * 128:(bi + 1) * 128], rhs=W[:, c, col:col + n],
                                                                                                start=(c == 0), stop=(c == KC - 1)),
                                           reads=["p1_W", "p1_hb%d" % i], writes=["p1_pt%d" % pq])
                                    evac(ei, dstt[:, bi, do:do + n], pt[pq][:, :n], ["p1_pt%d" % pq], [id(dstt)])
                                    ei += 1
                            dma("pool", vtm[t0:t0 + nt, :].rearrange("(b p) n -> p b n", p=128), tv[:, :nb, :], reads=[id(tv)], writes=[("vtm", gi)])
                            dma("pool", ztm[t0:t0 + nt, :].rearrange("(b p) n -> p b n", p=128), tz[:, :nb, :], reads=[id(tz)], writes=[("ztm", gi)])
                            with nc.allow_non_contiguous_dma(reason="small dt rows"):
                                dma("pool", dtr[t0:t0 + nt, :].rearrange("(b p) n -> p b n", p=128), tdt[:, :nb, :], reads=[id(tdt)], writes=[("dtr", gi)])
                    cx.barrier()

        def phase_cprep(l):
            with ExitStack() as s:
                t1 = sb("c_t1", [8, T], F32, s)
                t2 = sb("c_t2", [8, T], F32, s)
                t3 = sb("c_t3", [8, T], F32, s)
                b1 = sb("c_b1", [8, T], BF16, s)
                b2 = sb("c_b2", [8, T], BF16, s)
                b3 = sb("c_b3", [8, T], BF16, s)
                nb_ = sb("c_nb", [8, 1], F32, s)
                with nc.allow_non_contiguous_dma(reason="tiny param"):
                    dma("sp", nb_[:], b_forget[l].rearrange("(h o) -> h o", o=1), writes=["c_nb"])
                dma("sp", t1[:], fT[:, :], writes=["c_t1"])
                op("dve", lambda v: v.tensor_scalar(out=nb_[:], in0=nb_[:], scalar1=-1.0, scalar2=None, op0=ALU.mult), reads=["c_nb"], writes=["c_nb"])
                op("act", lambda a: a.activation(out=t1[:], in_=t1[:], func=AF.Exp, scale=-1.0, bias=nb_[:]), reads=["c_t1", "c_nb"], writes=["c_t1"])
                op("act", lambda a: a.activation(out=t1[:], in_=t1[:], func=AF.Ln, scale=1.0, bias=ones_f[0:8, 0:1]), reads=["c_t1", "ones_f"], writes=["c_t1"])
                op("dve", lambda v: v.memset(t1[:, 0:112], 0.0), writes=["c_t1"])
                op("dve", lambda v: v.memset(t2[:], 1.0), writes=["c_t2"])
                op("dve", lambda v: v.tensor_tensor_scan(out=t3[:], data0=t2[:], data1=t1[:], initial=0.0, op0=ALU.mult, op1=ALU.add),
                   reads=["c_t1", "c_t2"], writes=["c_t3"])

                def split3(dst, rows):
                    bb = [b1, b2, b1]
                    kk = ["c_b1", "c_b2", "c_b1"]
                    for j in range(3):
                        op("dve", lambda v, j=j: v.tensor_copy(out=bb[j][:], in_=t1[:]), reads=["c_t1"], writes=[kk[j]])
                        dma("sp", dst[:, rows[j], :], bb[j][:], reads=[kk[j]], writes=[(id(dst), rows[j])])
                        if j < 2:
                            op("dve", lambda v, j=j: v.tensor_copy(out=t2[:], in_=bb[j][:]), reads=[kk[j]], writes=["c_t2"])
                            op("dve", lambda v: v.tensor_tensor(out=t1[:], in0=t1[:], in1=t2[:], op=ALU.subtract), reads=["c_t1", "c_t2"], writes=["c_t1"])

                op("dve", lambda v: v.tensor_scalar(out=t1[:], in0=t3[:], scalar1=8.0, scalar2=None, op0=ALU.mult), reads=["c_t3"], writes=["c_t1"])
                op("dve", lambda v: v.memset(t1[:, 0:112], -240000.0), writes=["c_t1"])
                split3(caugK, [3, 4, 5])
                t3v = t3[:, :].rearrange("h (b t) -> h b t", t=128)
                op("dve", lambda v: v.tensor_scalar(out=t1[:, :].rearrange("h (b t) -> h b t", t=128), in0=t3v[:, :, 127:128].broadcast_to([8, NB, 128]),
                                                    scalar1=-4.0, scalar2=None, op0=ALU.mult), reads=["c_t3"], writes=["c_t1"])
                if NB > 1:
                    src_ = t3[:, 0:T - 128].rearrange("h (b t) -> h b t", t=128)[:, :, 127:128].broadcast_to([8, NB - 1, 128])
                    op("dve", lambda v: v.scalar_tensor_tensor(out=t1[:, 128:T].rearrange("h (b t) -> h b t", t=128), in0=src_, scalar=-4.0,
                                                               in1=t1[:, 128:T].rearrange("h (b t) -> h b t", t=128), op0=ALU.mult, op1=ALU.add),
                       reads=["c_t3", "c_t1"], writes=["c_t1"])
                split3(caugQ, [0, 1, 2])
                op("dve", lambda v: v.memset(b3[:], 1.0), writes=["c_b3"])
                for r in (3, 4, 5):
                    dma("sp", caugQ[:, r, :], b3[:], reads=["c_b3"], writes=[(id(caugQ), r)])
                for r in (0, 1, 2):
                    dma("sp", caugK[:, r, :], b3[:], reads=["c_b3"], writes=[(id(caugK), r)])
                cx.barrier()

        def phase_attn(l):
            with ExitStack() as s:
                Qp = [sb("a_Q%d" % i, [128, T], BF16, s) for i in range(2)]
                Kp = [sb("a_K%d" % i, [128, T], BF16, s) for i in range(2)]
                Vp = [sb("a_V%d" % i, [128, NB, 128], BF16, s) for i in range(2)]
                NSB = 3
                sbase = [0]
                obase = [0]
                Pt = [sb("a_P%d" % i, [128, 1024], BF16, s) for i in range(NSB)]
                rec = sb("a_rec", [128, 512], F32, s)
                yo = [sb("a_yo%d" % i, [64, 512], BF16, s) for i in range(2)]
                Sp = [ps("a_S%d" % i, [128, 1024], F32, s) for i in range(NSB)]
                Op = [ps("a_O%d" % i, [128, 512], F32, s) for i in range(2)]
                for i in range(2):
                    op("dve", lambda v, i=i: v.memset(Vp[i][:, :, 64:128], 1.0), writes=["a_V%d" % i])
                for h in range(8):
                    i = h % 2
                    r0 = (h % 2) * 64
                    dma("sp", Qp[i][0:64, :], qkT[h // 2, r0:r0 + 64, :], writes=["a_Q%d" % i])
                    dma("sp", Qp[i][64:70, :], caugQ[h], writes=["a_Q%d" % i])
                    dma("sp", Kp[i][0:64, :], qkT[4 + h // 2, r0:r0 + 64, :], writes=["a_K%d" % i])
                    dma("sp", Kp[i][64:70, :], caugK[h], writes=["a_K%d" % i])
                    with nc.allow_non_contiguous_dma(reason="128B v rows"):
                        for (vb0, vnb) in _blocks(NB, 8):
                            dma("sp", Vp[i][:, vb0:vb0 + vnb, 0:64],
                                vtm[vb0 * 128:(vb0 + vnb) * 128, h * 64:(h + 1) * 64].rearrange("(b p) d -> p b d", p=128), writes=["a_V%d" % i])
                    steps = []
                    for gi, (q0b, nqb) in enumerate(_blocks(NB, 4)):
                        js = list(range(q0b + nqb))
                        for a_ in range(0, len(js), 2):
                            parts = []
                            off = 0
                            for j in js[a_:a_ + 2]:
                                qs = max(q0b, j)
                                ncol = (q0b + nqb - qs) * 128
                                if off + ncol > 512 and off > 0:
                                    off = 512
                                parts.append((j, qs, ncol, off))
                                off += ncol
                            steps.append((gi, q0b, nqb, parts))
                    LOOK = NSB - 1

                    def emit_qk(k):
                        gi, q0b, nqb, parts = steps[k]
                        sp_ = (sbase[0] + k) % NSB
                        for (j, qs, ncol, off) in parts:
                            op("pe", lambda p: p.matmul(Sp[sp_][:, off:off + ncol], lhsT=Kp[i][0:70, j * 128:(j + 1) * 128],
                                                        rhs=Qp[i][0:70, qs * 128:qs * 128 + ncol], start=True, stop=True),
                               reads=["a_Q%d" % i, "a_K%d" % i], writes=["a_S%d" % sp_])

                    def emit_rest(k):
                        gi, q0b, nqb, parts = steps[k]
                        q1b = q0b + nqb
                        sp_ = (sbase[0] + k) % NSB
                        oo = (obase[0] + gi) % 2
                        tot = parts[-1][3] + parts[-1][2]
                        op("act", lambda a: a.activation(out=Pt[sp_][:, :tot], in_=Sp[sp_][:, :tot], func=AF.Exp, scale=0.125),
                           reads=["a_S%d" % sp_], writes=["a_P%d" % sp_])
                        for (j, qs, ncol, off) in parts:
                            if j >= q0b:
                                op("pool", lambda v: v.tensor_tensor(out=Pt[sp_][:, off:off + 128], in0=Pt[sp_][:, off:off + 128], in1=tri_leb[:], op=ALU.mult),
                                   reads=["a_P%d" % sp_, "tri_leb"], writes=["a_P%d" % sp_])
                        for (j, qs, ncol, off) in parts:
                            c0 = (qs - q0b) * 128
                            op("pe", lambda p: p.matmul(Op[oo][:, c0:c0 + ncol], lhsT=Vp[i][:, j, :], rhs=Pt[sp_][:, off:off + ncol],
                                                        start=(j == 0), stop=(j == q1b - 1)),
                               reads=["a_V%d" % i, "a_P%d" % sp_], writes=["a_O%d" % oo])
                        if parts[-1][0] == q1b - 1:
                            nt = nqb * 128
                            op("dve", lambda v: v.tensor_scalar(out=rec[64:128, :nt], in0=Op[oo][64:128, :nt], scalar1=1e-36, scalar2=None, op0=ALU.add),
                               reads=["a_O%d" % oo], writes=["a_rec"])
                            op("dve", lambda v: v.reciprocal(out=rec[64:128, :nt], in_=rec[64:128, :nt]), reads=["a_rec"], writes=["a_rec"])
                            yi = gi % 2
                            op("dve", lambda v: v.tensor_tensor(out=yo[yi][0:64, :nt], in0=Op[oo][0:64, :nt], in1=rec[64:128, :nt], op=ALU.mult),
                               reads=["a_O%d" % oo, "a_rec"], writes=["a_yo%d" % yi])
                            dma("pool", yattT[h // 2, r0:r0 + 64, q0b * 128:q0b * 128 + nt], yo[yi][0:64, :nt], reads=["a_yo%d" % yi], writes=[("yattT", h, gi)])

                    for k in range(min(LOOK, len(steps))):
                        emit_qk(k)
                    for k in range(len(steps)):
                        if k + LOOK < len(steps):
                            emit_qk(k + LOOK)
                        emit_rest(k)
                    sbase[0] += len(steps)
                    obase[0] += len(_blocks(NB, 4))
                cx.barrier()


        def bc(ap, dim, shape):
            return ap.unsqueeze(dim).broadcast_to(list(shape))

        def phase_ssd(l):
            with ExitStack() as s:
                cw = sb("s_cw", [128, 4, 12], F32, s)
                cbias = sb("s_cb", [128, 12], F32, s)
                vec = sb("s_vec", [128, 3, 16], F32, s)
                nw = sb("s_nw", [128, 1024], F32, s)
                dt_all = sb("s_dt", [128, NB, 16], F32, s)
                dtA = sb("s_dtA", [128, NB, 16], F32, s)
                xr = sb("s_xr", [128, 12, 3 + 512], BF16, s)
                acc = sb("s_acc", [128, 512], F32, s)
                xc = sb("s_xc", [128, 12, 512], BF16, s)
                xs_tm_2 = [sb("s_xs_%d" % i_, [128, 1024], BF16, s) for i_ in range(2)]
                Btm_2 = [sb("s_B_%d" % i_, [128, 256], BF16, s) for i_ in range(2)]
                Rt_2 = [sb("s_R_%d" % i_, [128, 16, 128], F32, s) for i_ in range(2)]
                acs_2 = [sb("s_acs_%d" % i_, [128, 32], F32, s) for i_ in range(2)]
                sm_2 = [sb("s_sm_%d" % i_, [128, 4, 16], F32, s) for i_ in range(2)]
                cbm_2 = [sb("s_cbm_%d" % i_, [128, 2, 128], BF16, s) for i_ in range(2)]
                dec = sb("s_dec", [128, 8, 128], BF16, s)
                M_2 = [sb("s_M_%d" % i_, [128, 16, 128], BF16, s) for i_ in range(2)]
                xdt_2 = [sb("s_xdt_%d" % i_, [128, 1024], BF16, s) for i_ in range(2)]
                xdtS_2 = [sb("s_xdtS_%d" % i_, [128, 1024], BF16, s) for i_ in range(2)]
                state = sb("s_state", [128, 1024], F32, s)
                prevb = sb("s_prevb", [128, 1024], BF16, s)
                yoff_2 = [sb("s_yoff_%d" % i_, [128, 1024], F32, s) for i_ in range(2)]
                ysb_2 = [sb("s_y_%d" % i_, [128, 1024], F32, s) for i_ in range(2)]
                tmp_2 = [sb("s_tmp_%d" % i_, [128, 1024], F32, s) for i_ in range(2)]
                ztg = sb("s_ztg", [128, 4, 1024], BF16, s)
                szg_2 = [sb("s_szg_%d" % i_, [128, 4, 1024], F32, s) for i_ in range(2)]
                ss = sb("s_ss", [128, 4], F32, s)
                yn_2 = [sb("s_yn_%d" % i_, [128, 1024], BF16, s) for i_ in range(2)]
                ysT_2 = [sb("s_ysT_%d" % i_, [128, 8, 512], BF16, s) for i_ in range(2)]
                ptr = ps("s_ptr", [128, 1024], BF16, s)
                ptb = ps("s_ptb", [128, 256], BF16, s)
                segp = ps("s_seg", [128, 1024], F32, s)
                Yp = ps("s_Y", [128, 512], F32, s)
                ptry = ps("s_ptry", [128, 1024], BF16, s)
                mA = ps("s_mA", [128, 512], F32, s)
                mB = ps("s_mB", [128, 512], F32, s)
                with nc.allow_non_contiguous_dma(reason="tiny params"):
                    for k in range(4):
                        dma("sp", cw[:, k, :], ssd_conv_w[l, k].rearrange("(c p) -> p c", p=128), writes=["s_cw"])
                    dma("sp", cbias[:], ssd_conv_b[l].rearrange("(c p) -> p c", p=128), writes=["s_cb"])
                    dma("sp", vec[:].rearrange("p a b -> p (a b)"), ssd_vec[l].rearrange("a b -> (a b)").partition_broadcast(128), writes=["s_vec"])
                    dma("sp", nw[:], ssd_norm_w[l].partition_broadcast(128), writes=["s_nw"])
                    dma("sp", dt_all[:], dtr.rearrange("(b p) n -> p b n", p=128), writes=["s_dt"])
                op("act", lambda a: a.activation(out=vec[:, 1, :], in_=vec[:, 1, :], func=AF.Exp), reads=["s_vec"], writes=["s_vec"])
                op("dve", lambda v: v.tensor_scalar(out=vec[:, 1, :], in0=vec[:, 1, :], scalar1=-1.0, scalar2=None, op0=ALU.mult), reads=["s_vec"], writes=["s_vec"])
                op("dve", lambda v: v.tensor_tensor(out=dt_all[:], in0=dt_all[:], in1=bc(vec[:, 0, :], 1, [128, NB, 16]), op=ALU.add), reads=["s_dt", "s_vec"], writes=["s_dt"])
                op("act", lambda a: a.activation(out=dt_all[:], in_=dt_all[:], func=AF.Exp), reads=["s_dt"], writes=["s_dt"])
                op("act", lambda a: a.activation(out=dt_all[:], in_=dt_all[:], func=AF.Ln, bias=ones_f[:, 0:1], scale=1.0), reads=["s_dt", "ones_f"], writes=["s_dt"])
                op("dve", lambda v: v.tensor_scalar(out=dt_all[:, 0, :], in0=dt_all[:, 0, :], scalar1=validcol[:, 0:1], scalar2=None, op0=ALU.mult),
                   reads=["s_dt", "validcol"], writes=["s_dt"])
                op("dve", lambda v: v.tensor_tensor(out=dtA[:], in0=dt_all[:], in1=bc(vec[:, 1, :], 1, [128, NB, 16]), op=ALU.mult), reads=["s_dt", "s_vec"], writes=["s_dtA"])
                op("dve", lambda v: v.memset(state[:], 0.0), writes=["s_state"])
                op("dve", lambda v: v.memset(prevb[:], 0.0), writes=["s_prevb"])

                def ssd_front(b0, bi, gi):
                    b = b0 + bi
                    c0 = bi * 128
                    pb = b % 2
                    xs_tm = xs_tm_2[pb]
                    Btm = Btm_2[pb]
                    Rt = Rt_2[pb]
                    acs = acs_2[pb]
                    cbm = cbm_2[pb]
                    M = M_2[pb]
                    xdt = xdt_2[pb]
                    xdtS = xdtS_2[pb]
                    yoff = yoff_2[pb]
                    ysb = ysb_2[pb]
                    tmp = tmp_2[pb]
                    yn = yn_2[pb]
                    sm = sm_2[pb]
                    for cc in range(8):
                        op("pe", lambda p, cc=cc: p.transpose(out=ptr[:, cc * 128:(cc + 1) * 128], in_=xc[:, cc, c0:c0 + 128], identity=identb[:]),
                           reads=["s_xc", "identb"], writes=["s_ptr"])
                    for g in range(2):
                        op("pe", lambda p, g=g: p.transpose(out=ptb[:, g * 128:(g + 1) * 128], in_=xc[:, 8 + g, c0:c0 + 128], identity=identb[:]),
                           reads=["s_xc", "identb"], writes=["s_ptb"])
                    op("act", lambda a: a.activation(out=xs_tm[:], in_=ptr[:], func=AF.Copy), reads=["s_ptr"], writes=["s_xs_%d" % pb])
                    op("dve", lambda v: v.tensor_copy(out=Btm[:], in_=ptb[:]), reads=["s_ptb"], writes=["s_B_%d" % pb])
                    op("pool", lambda v: v.tensor_tensor(out=Rt[:], in0=bc(tri_le[:], 1, [128, 16, 128]), in1=bc(dtA[:, b, :], 2, [128, 16, 128]), op=ALU.mult),
                       reads=["tri_le", "s_dtA"], writes=["s_R_%d" % pb])
                    op("pe", lambda p: p.matmul(mB[:, 256:272], lhsT=tri_le[:], rhs=dtA[:, b, :], start=True, stop=True), reads=["tri_le", "s_dtA"], writes=["s_mB"])
                    op("pe", lambda p: p.matmul(mB[:, 272:288], lhsT=ones_f[:], rhs=dtA[:, b, :], start=True, stop=True), reads=["ones_f", "s_dtA"], writes=["s_mB"])
                    for g in range(2):
                        op("pe", lambda p, g=g: p.matmul(mB[:, g * 128:(g + 1) * 128], lhsT=xc[:, 8 + g, c0:c0 + 128], rhs=xc[:, 10 + g, c0:c0 + 128], start=True, stop=True),
                           reads=["s_xc"], writes=["s_mB"])
                    op("dve", lambda v: v.tensor_copy(out=acs[:], in_=mB[:, 256:288]), reads=["s_mB"], writes=["s_acs_%d" % pb])
                    op("dve", lambda v: v.tensor_tensor(out=cbm[:], in0=mB[:, 0:256].rearrange("p (g l) -> p g l", g=2), in1=bc(tri_le[:], 1, [128, 2, 128]), op=ALU.mult),
                       reads=["s_mB", "tri_le"], writes=["s_cbm_%d" % pb])
                    op("act", lambda a: a.activation(out=sm[:, 0, :], in_=acs[:, 0:16], func=AF.Exp), reads=["s_acs_%d" % pb], writes=["s_sm0_%d" % pb])
                    op("dve", lambda v: v.tensor_tensor(out=sm[:, 3, :], in0=acs[:, 16:32], in1=acs[:, 0:16], op=ALU.subtract), reads=["s_acs_%d" % pb], writes=["s_sm3_%d" % pb])
                    op("act", lambda a: a.activation(out=sm[:, 1, :], in_=sm[:, 3, :], func=AF.Exp), reads=["s_sm3_%d" % pb], writes=["s_sm1_%d" % pb])
                    op("act", lambda a: a.activation(out=sm[:, 2, :], in_=acs[:, 16:32], func=AF.Exp), reads=["s_acs_%d" % pb], writes=["s_sm2_%d" % pb])
                    for hf in range(2):
                        for q in range(2):
                            h0_ = hf * 8 + q * 4
                            op("pe", lambda p, q=q, h0_=h0_: p.matmul(segp[:, q * 512:(q + 1) * 512], lhsT=smask[:], rhs=Rt[:, h0_:h0_ + 4, :].rearrange("p h l -> p (h l)"),
                                                                       start=True, stop=True), reads=["smask", "s_R_%d" % pb], writes=["s_seg"])
                        op("act", lambda a: a.activation(out=dec[:].rearrange("p h l -> p (h l)"), in_=segp[:], func=AF.Exp), reads=["s_seg"], writes=["s_dec"])
                        op("dve", lambda v, hf=hf: v.tensor_tensor(out=M[:, hf * 8:(hf + 1) * 8, :], in0=dec[:], in1=bc(cbm[:, hf, :], 1, [128, 8, 128]), op=ALU.mult),
                           reads=["s_dec", "s_cbm_%d" % pb], writes=["s_M_%d" % pb])
                    op("dve", lambda v: v.tensor_tensor(out=xdt[:].rearrange("p (h d) -> p h d", d=64), in0=xs_tm[:].rearrange("p (h d) -> p h d", d=64),
                                                        in1=bc(dt_all[:, b, :], 2, [128, 16, 64]), op=ALU.mult), reads=["s_xs_%d" % pb, "s_dt"], writes=["s_xdt_%d" % pb])
                    op("dve", lambda v: v.tensor_tensor(out=xdtS[:].rearrange("p (h d) -> p h d", d=64), in0=xdt[:].rearrange("p (h d) -> p h d", d=64),
                                                        in1=bc(sm[:, 1, :], 2, [128, 16, 64]), op=ALU.mult), reads=["s_xdt_%d" % pb, "s_sm1_%d" % pb], writes=["s_xdtS_%d" % pb])
                    for g in range(2):
                        op("pe", lambda p, g=g: p.matmul(mA[:, :], lhsT=xc[:, 10 + g, c0:c0 + 128], rhs=prevb[:, g * 512:(g + 1) * 512], start=True, stop=True),
                           reads=["s_xc", "s_prevb"], writes=["s_mA"])
                        op("dve", lambda v, g=g: v.tensor_tensor(out=yoff[:, g * 512:(g + 1) * 512].rearrange("p (h d) -> p h d", d=64), in0=mA[:, :].rearrange("p (h d) -> p h d", d=64),
                                                                 in1=bc(sm[:, 0, g * 8:(g + 1) * 8], 2, [128, 8, 64]), op=ALU.mult), reads=["s_mA", "s_sm0_%d" % pb], writes=["s_yoff_%d" % pb])
                    for g in range(2):
                        for hh in range(8 * g, 8 * g + 8):
                            op("pe", lambda p, hh=hh, g=g: p.matmul(Yp[:, (hh - 8 * g) * 64:(hh - 8 * g + 1) * 64], lhsT=M[:, hh, :], rhs=xdt[:, hh * 64:(hh + 1) * 64], start=True, stop=True),
                               reads=["s_M_%d" % pb, "s_xdt_%d" % pb], writes=["s_Y"])
                        op("dve", lambda v, g=g: v.tensor_tensor(out=ysb[:, g * 512:(g + 1) * 512], in0=Yp[:, :], in1=yoff[:, g * 512:(g + 1) * 512], op=ALU.add),
                           reads=["s_Y", "s_yoff_%d" % pb], writes=["s_y_%d" % pb])
                    for g in range(2):
                        op("pe", lambda p, g=g: p.matmul(mA[:, :], lhsT=Btm[:, g * 128:(g + 1) * 128], rhs=xdtS[:, g * 512:(g + 1) * 512], start=True, stop=True),
                           reads=["s_B_%d" % pb, "s_xdtS_%d" % pb], writes=["s_mA"])
                        op("pool", lambda v, g=g: v.tensor_tensor(out=state[:, g * 512:(g + 1) * 512].rearrange("p (h d) -> p h d", d=64),
                                                                 in0=state[:, g * 512:(g + 1) * 512].rearrange("p (h d) -> p h d", d=64),
                                                                 in1=bc(sm[:, 2, g * 8:(g + 1) * 8], 2, [128, 8, 64]), op=ALU.mult), reads=["s_state", "s_sm2_%d" % pb], writes=["s_state"])
                        op("dve", lambda v, g=g: v.tensor_tensor(out=state[:, g * 512:(g + 1) * 512], in0=state[:, g * 512:(g + 1) * 512], in1=mA[:, :], op=ALU.add),
                           reads=["s_state", "s_mA"], writes=["s_state"])
                    op("act", lambda a: a.activation(out=prevb[:], in_=state[:], func=AF.Copy), reads=["s_state"], writes=["s_prevb"])

                def ssd_tail(b0, bi, gi):
                    b = b0 + bi
                    c0 = bi * 128
                    pb = b % 2
                    xs_tm = xs_tm_2[pb]
                    Btm = Btm_2[pb]
                    Rt = Rt_2[pb]
                    acs = acs_2[pb]
                    cbm = cbm_2[pb]
                    M = M_2[pb]
                    xdt = xdt_2[pb]
                    xdtS = xdtS_2[pb]
                    yoff = yoff_2[pb]
                    ysb = ysb_2[pb]
                    tmp = tmp_2[pb]
                    yn = yn_2[pb]
                    sm = sm_2[pb]
                    op("pool", lambda v: v.tensor_tensor(out=tmp[:].rearrange("p (h d) -> p h d", d=64), in0=xs_tm[:].rearrange("p (h d) -> p h d", d=64),
                                                        in1=bc(vec[:, 2, :], 2, [128, 16, 64]), op=ALU.mult), reads=["s_xs_%d" % pb, "s_vec"], writes=["s_tmp_%d" % pb])
                    op("dve", lambda v: v.tensor_tensor(out=ysb[:], in0=ysb[:], in1=tmp[:], op=ALU.add), reads=["s_y_%d" % pb, "s_tmp_%d" % pb], writes=["s_y_%d" % pb])
                    op("dve", lambda v: v.tensor_tensor(out=ysb[:], in0=ysb[:], in1=szg_2[gi % 2][:, bi, :], op=ALU.mult), reads=["s_y_%d" % pb, "s_szg_%d" % (gi % 2)], writes=["s_y_%d" % pb])
                    op("act", lambda a: a.activation(out=tmp[:], in_=ysb[:], func=AF.Square), reads=["s_y_%d" % pb], writes=["s_tmp_%d" % pb])
                    op("dve", lambda v: v.tensor_reduce(out=ss[:, 0:2], in_=tmp[:].rearrange("p (g d) -> p g d", g=2), axis=AX.X, op=ALU.add), reads=["s_tmp_%d" % pb], writes=["s_ss"])
                    op("act", lambda a: a.activation(out=ss[:, 2:4], in_=ss[:, 0:2], func=AF.Ln, scale=1.0 / 512, bias=RMS_EPS_T[:]), reads=["s_ss", "eps"], writes=["s_ss"])
                    op("act", lambda a: a.activation(out=ss[:, 2:4], in_=ss[:, 2:4], func=AF.Exp, scale=-0.5), reads=["s_ss"], writes=["s_ss"])
                    for g in range(2):
                        op("dve", lambda v, g=g: v.scalar_tensor_tensor(out=yn[:, g * 512:(g + 1) * 512], in0=ysb[:, g * 512:(g + 1) * 512], scalar=ss[:, 2 + g:3 + g],
                                                                        in1=nw[:, g * 512:(g + 1) * 512], op0=ALU.mult, op1=ALU.mult),
                           reads=["s_y_%d" % pb, "s_ss", "s_nw"], writes=["s_yn_%d" % pb])
                    for cc in range(8):
                        op("pe", lambda p, cc=cc: p.transpose(out=ptry[:, cc * 128:(cc + 1) * 128], in_=yn[:, cc * 128:(cc + 1) * 128], identity=identb[:]),
                           reads=["s_yn_%d" % pb, "identb"], writes=["s_ptry"])
                    op("act", lambda a: a.activation(out=ysT_2[gi % 2][:, :, c0:c0 + 128], in_=ptry[:].rearrange("p (c t) -> p c t", c=8), func=AF.Copy), reads=["s_ptry"], writes=["s_ysT_%d" % (gi % 2)])


                pending = []

                def flush_tail():
                    if not pending:
                        return
                    pb0, pbi, pgi, pnb, pt0, pnt = pending.pop()
                    ssd_tail(pb0, pbi, pgi)
                    if pbi == pnb - 1:
                        dma("pool", yssdT[:, :, pt0:pt0 + pnt].rearrange("c p t -> p c t"), ysT_2[pgi % 2][:, :, :pnt], reads=["s_ysT_%d" % (pgi % 2)], writes=[("yssdT", pgi)])

                for gi, (b0, nb) in enumerate(_blocks(NB, 4)):
                    t0, nt = b0 * 128, nb * 128
                    dma("sp", xr[:, :, 0:3 + nt], xbcT[:, :, 128 + t0 - 3:128 + t0 + nt].rearrange("c p t -> p c t"), writes=["s_xr"])
                    for cc in range(12):
                        op("dve", lambda v, cc=cc: v.tensor_scalar(out=acc[:, :nt], in0=xr[:, cc, 3:3 + nt], scalar1=cw[:, 3, cc:cc + 1], scalar2=None, op0=ALU.mult),
                           reads=["s_xr", "s_cw"], writes=["s_acc"])
                        for k in range(3):
                            op("dve", lambda v, cc=cc, k=k: v.scalar_tensor_tensor(out=acc[:, :nt], in0=xr[:, cc, k:k + nt], scalar=cw[:, k, cc:cc + 1], in1=acc[:, :nt],
                                                                                   op0=ALU.mult, op1=ALU.add), reads=["s_xr", "s_cw", "s_acc"], writes=["s_acc"])
                        op("act", lambda a, cc=cc: a.activation(out=xc[:, cc, :nt], in_=acc[:, :nt], func=AF.Silu, bias=cbias[:, cc:cc + 1], scale=1.0),
                           reads=["s_acc", "s_cb"], writes=["s_xc"])
                    dma("sp", ztg[:, :nb, :], ztm[t0:t0 + nt, :].rearrange("(b p) n -> p b n", p=128), writes=["s_ztg"])
                    op("act", lambda a: a.activation(out=szg_2[gi % 2][:, :nb, :], in_=ztg[:, :nb, :], func=AF.Silu), reads=["s_ztg"], writes=["s_szg_%d" % (gi % 2)])
                    for bi in range(nb):
                        lf = record(ssd_front, b0, bi, gi)
                        lt_ = record(flush_tail)
                        replay_interleaved(lf, lt_)
                        pending.append((b0, bi, gi, nb, t0, nt))
                flush_tail()
                cx.barrier()


        def ln_fm(s_tiles, r, rkey, dst, dkey, gb, gbkey, nt, zero_pads):
            sq, st1, st2, mean, rstd, lps, lpk = s_tiles
            for c in range(KC):
                op("act", lambda a, c=c: a.activation(out=sq[:, :nt], in_=r[:, c, :nt], func=AF.Square), reads=[rkey], writes=["ln_sq"])
                op("pe", lambda p, c=c: p.matmul(lps[0][:, :nt], lhsT=ones_f[:], rhs=r[:, c, :nt], start=(c == 0), stop=(c == KC - 1)), reads=["ones_f", rkey], writes=[lpk[0]])
                op("pe", lambda p, c=c: p.matmul(lps[1][:, :nt], lhsT=ones_f[:], rhs=sq[:, :nt], start=(c == 0), stop=(c == KC - 1)), reads=["ones_f", "ln_sq"], writes=[lpk[1]])
            op("act", lambda a: a.activation(out=mean[:, :nt], in_=lps[0][:, :nt], func=AF.Copy, scale=1.0 / D), reads=[lpk[0]], writes=["ln_mean"])
            op("dve", lambda v: v.tensor_tensor(out=st1[:, :nt], in0=mean[:, :nt], in1=mean[:, :nt], op=ALU.mult), reads=["ln_mean"], writes=["ln_st1"])
            op("dve", lambda v: v.scalar_tensor_tensor(out=st2[:, :nt], in0=lps[1][:, :nt], scalar=1.0 / D, in1=st1[:, :nt], op0=ALU.mult, op1=ALU.subtract),
               reads=[lpk[1], "ln_st1"], writes=["ln_st2"])
            op("act", lambda a: a.activation(out=st2[:, :nt], in_=st2[:, :nt], func=AF.Sqrt, bias=LN_EPS_T[:], scale=1.0), reads=["ln_st2", "eps"], writes=["ln_st2"])
            op("dve", lambda v: v.reciprocal(out=rstd[:, :nt], in_=st2[:, :nt]), reads=["ln_st2"], writes=["ln_rstd"])
            for c in range(KC):
                op("dve", lambda v, c=c: v.tensor_tensor(out=sq[:, :nt], in0=r[:, c, :nt], in1=mean[:, :nt], op=ALU.subtract), reads=[rkey, "ln_mean"], writes=["ln_sq"])
                op("dve", lambda v, c=c: v.tensor_tensor(out=sq[:, :nt], in0=sq[:, :nt], in1=rstd[:, :nt], op=ALU.mult), reads=["ln_sq", "ln_rstd"], writes=["ln_sq"])
                op("act", lambda a, c=c: a.activation(out=dst[:, c, :nt], in_=sq[:, :nt], func=AF.Identity, scale=gb[:, 0, c:c + 1], bias=gb[:, 1, c:c + 1]),
                   reads=["ln_sq", gbkey], writes=[dkey])
            if zero_pads:
                op("dve", lambda v: v.memset(dst[:, :, 0:112], 0.0), writes=[dkey])

        def ln_tiles(s, n, lps=None, lpk=None):
            if lps is None:
                lps = [ps("ln_ps%d" % i, [128, n], F32, s) for i in range(2)]
                lpk = ["ln_ps0", "ln_ps1"]
            return (sb("ln_sq", [128, n], F32, s), sb("ln_st1", [128, n], F32, s), sb("ln_st2", [128, n], F32, s), sb("ln_mean", [128, n], F32, s),
                    sb("ln_rstd", [128, n], F32, s), lps, lpk)

        def load_gb(s, src, name):
            t = sb(name, [128, 2, KC], F32, s)
            with nc.allow_non_contiguous_dma(reason="tiny param"):
                dma("sp", t[:], src.rearrange("t (c p) -> p t c", p=128), writes=[name])
            return t

        def phase_merge(l, h_in, h_out):
            with ExitStack() as s:
                Wp = sb("m_Wp", [128, 16, D], BF16, s)
                Wo = sb("m_Wo", [128, 8, D], BF16, s)
                with ExitStack() as s2:
                    wstg = mk_wstg(s2)
                    load_w(Wp, w_proj[l], D, "m_Wp", 16, wstg)
                    load_w(Wo, w_out[l], D, "m_Wo", 8, wstg)
                    cx.barrier()
                gb = load_gb(s, ln_mix_gb[l], "m_gb")
                scw = sb("m_scw", [128, 3, 4], F32, s)
                with nc.allow_non_contiguous_dma(reason="tiny param"):
                    for k in range(3):
                        dma("sp", scw[:, k, :], sc_conv_w[l, k].rearrange("(c p) -> p c", p=128), writes=["m_scw"])
                NT = 512
                sct = sb("m_sct", [128, 12, 2 + NT], BF16, s)
                gt = sb("m_gt", [128, 24, NT], BF16, s)
                ya = sb("m_ya", [128, 4, NT], BF16, s)
                ys = sb("m_ys", [128, 8, NT], BF16, s)
                hf2 = [sb("m_h%d" % i, [128, KC, NT], F32, s) for i in range(2)]
                u = sb("m_u", [128, 2 + NT], F32, s)
                acc = sb("m_acc", [128, NT], F32, s)
                yc = sb("m_yc", [128, 4, NT], BF16, s)
                sg2 = [sb("m_sg%d" % i, [128, 3, NT], F32, s) for i in range(2)]
                mt2 = [sb("m_mt%d" % i, [128, 3, NT], F32, s) for i in range(2)]
                mg2 = [sb("m_mg%d" % i, [128, 8, NT], BF16, s) for i in range(2)]
                r = sb("m_r", [128, KC, NT], F32, s)
                pp2 = [[ps("m_p%d_%d" % (i, j), [128, NT], F32, s) for i in range(3)] for j in range(2)]
                po2 = [ps("m_po%d" % i, [128, NT], F32, s) for i in range(2)]
                lt = ln_tiles(s, NT, lps=[po2[0], po2[1]], lpk=["m_po0", "m_po1"])
                groups = _blocks(NB, 4)

                def stage_a(gi):
                    b0, nb = groups[gi]
                    t0, nt = b0 * 128, nb * 128
                    q = gi % 2
                    hf, kh = hf2[q], "m_h%d" % q
                    mg, kmg = mg2[q], "m_mg%d" % q
                    dma("sp", sct[:, :, 0:2 + nt], scT[:, :, 128 + t0 - 2:128 + t0 + nt].rearrange("c p t -> p c t"), writes=["m_sct"])
                    dma("sp", gt[:, :, :nt], gT[:, :, t0:t0 + nt].rearrange("c p t -> p c t"), writes=["m_gt"])
                    dma("sp", ya[:, :, :nt], yattT[:, :, t0:t0 + nt].rearrange("c p t -> p c t"), writes=["m_ya"])
                    dma("sp", ys[:, :, :nt], yssdT[:, :, t0:t0 + nt].rearrange("c p t -> p c t"), writes=["m_ys"])
                    dma("sp", hf[:, :, :nt], h_in[:, :, t0:t0 + nt].rearrange("c p t -> p c t"), writes=[kh])

                    def conv_cc(cc):
                        op("dve", lambda v: v.tensor_tensor(out=u[:, :2 + nt], in0=sct[:, 4 + cc, :2 + nt], in1=sct[:, 8 + cc, :2 + nt], op=ALU.mult), reads=["m_sct"], writes=["m_u"])
                        op("dve", lambda v: v.tensor_scalar(out=acc[:, :nt], in0=u[:, 2:2 + nt], scalar1=scw[:, 2, cc:cc + 1], scalar2=None, op0=ALU.mult),
                           reads=["m_u", "m_scw"], writes=["m_acc"])
                        for k in range(2):
                            op("dve", lambda v, k=k: v.scalar_tensor_tensor(out=acc[:, :nt], in0=u[:, k:k + nt], scalar=scw[:, k, cc:cc + 1], in1=acc[:, :nt], op0=ALU.mult, op1=ALU.add),
                               reads=["m_u", "m_scw", "m_acc"], writes=["m_acc"])
                        op("dve", lambda v: v.tensor_tensor(out=yc[:, cc, :nt], in0=acc[:, :nt], in1=sct[:, cc, 2:2 + nt], op=ALU.mult), reads=["m_acc", "m_sct"], writes=["m_yc"])

                    for cc in range(4):
                        conv_cc(cc)

                    def merge_n(n):
                        pn = n % 2
                        pp, sg, mt = pp2[pn], sg2[pn], mt2[pn]
                        branches = [(ya, "m_ya", 0, 4), (ys, "m_ys", 4, 8), (yc, "m_yc", 12, 4)]
                        for bi_, (src_t, skey, w0, nk) in enumerate(branches):
                            for kc in range(nk):
                                op("pe", lambda p, bi_=bi_, src_t=src_t, w0=w0, kc=kc, nk=nk: p.matmul(pp[bi_][:, :nt], lhsT=Wp[:, w0 + kc, n * 128:(n + 1) * 128], rhs=src_t[:, kc, :nt],
                                                                                                     start=(kc == 0), stop=(kc == nk - 1)),
                                   reads=["m_Wp", skey], writes=["m_p%d_%d" % (bi_, pn)])
                        for j in range(3):
                            op("act", lambda a, j=j: a.activation(out=sg[:, j, :nt], in_=gt[:, j * 8 + n, :nt], func=AF.Sigmoid), reads=["m_gt"], writes=[("m_sg", pn, j)])
                        for j in range(3):
                            op("dve", lambda v, j=j: v.tensor_tensor(out=mt[:, j, :nt], in0=pp[j][:, :nt], in1=sg[:, j, :nt], op=ALU.mult),
                               reads=["m_p%d_%d" % (j, pn), ("m_sg", pn, j)], writes=[("m_mt", pn, j)])
                        op("pool", lambda v: v.tensor_tensor(out=mt[:, 0, :nt], in0=mt[:, 0, :nt], in1=mt[:, 1, :nt], op=ALU.add),
                           reads=[("m_mt", pn, 0), ("m_mt", pn, 1)], writes=[("m_mt", pn, 0)])
                        op("pool", lambda v: v.tensor_tensor(out=mg[:, n, :nt], in0=mt[:, 0, :nt], in1=mt[:, 2, :nt], op=ALU.add),
                           reads=[("m_mt", pn, 0), ("m_mt", pn, 2)], writes=[(kmg, n)])

                    for n in range(8):
                        merge_n(n)

                def stage_b(gi):
                    b0, nb = groups[gi]
                    t0, nt = b0 * 128, nb * 128
                    q = gi % 2
                    hf, kh = hf2[q], "m_h%d" % q
                    mg, kmg = mg2[q], "m_mg%d" % q

                    def out_n(n):
                        po, pok_ = po2[n % 2], "m_po%d" % (n % 2)
                        for kc in range(8):
                            op("pe", lambda p, kc=kc: p.matmul(po[:, :nt], lhsT=Wo[:, kc, n * 128:(n + 1) * 128], rhs=mg[:, kc, :nt], start=(kc == 0), stop=(kc == 7)),
                               reads=["m_Wo", (kmg, kc)], writes=[pok_])
                        op("dve", lambda v: v.scalar_tensor_tensor(out=r[:, n, :nt], in0=hf[:, n, :nt], scalar=ALPHA, in1=po[:, :nt], op0=ALU.mult, op1=ALU.add),
                           reads=[kh, pok_], writes=["m_r"])

                    for n in range(8):
                        out_n(n)
                    ln_fm(lt, r, "m_r", hf, kh, gb, "m_gb", nt, zero_pads=(b0 == 0))
                    dma("pool", h_out[:, :, t0:t0 + nt].rearrange("c p t -> p c t"), hf[:, :, :nt], reads=[kh], writes=[("hT", gi)])

                for it in record(stage_a, 0):
                    replay(it)
                for gi in range(1, len(groups)):
                    la = record(stage_a, gi)
                    lb = record(stage_b, gi - 1)
                    replay_interleaved(la, lb)
                for it in record(stage_b, len(groups) - 1):
                    replay(it)
                cx.barrier()

        def phase_ffn_dense(l, h_in, h_out):
            with ExitStack() as s:
                NFF = FF_DENSE // 128
                Wg = sb("f_Wg", [128, KC, 2 * FF_DENSE], BF16, s)
                Wd = sb("f_Wd", [128, NFF, D], BF16, s)
                with ExitStack() as s2:
                    wstg = mk_wstg(s2)
                    load_w(Wg, dense_w_gu, 2 * FF_DENSE, "f_Wg", KC, wstg)
                    load_w(Wd, dense_w_down, D, "f_Wd", NFF, wstg)
                    cx.barrier()
                gb = load_gb(s, ln_ffn_gb[l], "f_gb")
                NT = 256
                hf2 = [sb("f_h%d" % i, [128, KC, NT], F32, s) for i in range(2)]
                hb2 = [sb("f_hb%d" % i, [128, KC, NT], BF16, s) for i in range(2)]
                act2 = [sb("f_act%d" % i, [128, NFF, NT], BF16, s) for i in range(2)]
                sgl2 = [sb("f_sgl%d" % i, [128, NT], F32, s) for i in range(2)]
                r = sb("f_r", [128, KC, NT], F32, s)
                lt = ln_tiles(s, NT)
                pg = [ps("f_pg%d" % i, [128, NT], F32, s) for i in range(2)]
                pu = [ps("f_pu%d" % i, [128, NT], F32, s) for i in range(2)]
                po2 = [ps("f_po%d" % i, [128, NT], F32, s) for i in range(2)]
                groups = _blocks(NB, 2)

                def load_cast(gi):
                    b0, nb = groups[gi]
                    t0, nt = b0 * 128, nb * 128
                    q = gi % 2
                    dma("sp", hf2[q][:, :, :nt], h_in[:, :, t0:t0 + nt].rearrange("c p t -> p c t"), writes=["f_h%d" % q])
                    for c in range(KC):
                        evac(c, hb2[q][:, c, :nt], hf2[q][:, c, :nt], ["f_h%d" % q], ["f_hb%d" % q])

                load_cast(0)
                for gi, (b0, nb) in enumerate(groups):
                    t0, nt = b0 * 128, nb * 128
                    q = gi % 2
                    hf, hb, act = hf2[q], hb2[q], act2[q]
                    kh, khb, kact = "f_h%d" % q, "f_hb%d" % q, "f_act%d" % q
                    for f in range(NFF):
                        i = f % 2
                        sgl = sgl2[i]
                        for c in range(KC):
                            op("pe", lambda p, c=c: p.matmul(pg[i][:, :nt], lhsT=Wg[:, c, f * 128:(f + 1) * 128], rhs=hb[:, c, :nt], start=(c == 0), stop=(c == KC - 1)),
                               reads=["f_Wg", khb], writes=["f_pg%d" % i])
                        for c in range(KC):
                            op("pe", lambda p, c=c: p.matmul(pu[i][:, :nt], lhsT=Wg[:, c, FF_DENSE + f * 128:FF_DENSE + (f + 1) * 128], rhs=hb[:, c, :nt], start=(c == 0), stop=(c == KC - 1)),
                               reads=["f_Wg", khb], writes=["f_pu%d" % i])
                        op("act", lambda a: a.activation(out=sgl[:, :nt], in_=pg[i][:, :nt], func=AF.Silu), reads=["f_pg%d" % i], writes=["f_sgl%d" % i])
                        op("dve", lambda v: v.tensor_tensor(out=act[:, f, :nt], in0=pu[i][:, :nt], in1=sgl[:, :nt], op=ALU.mult), reads=["f_pu%d" % i, "f_sgl%d" % i], writes=[(kact, f)])
                    if gi + 1 < len(groups):
                        load_cast(gi + 1)
                    for n in range(8):
                        po, kpo = po2[n % 2], "f_po%d" % (n % 2)
                        for f in range(NFF):
                            op("pe", lambda p, f=f: p.matmul(po[:, :nt], lhsT=Wd[:, f, n * 128:(n + 1) * 128], rhs=act[:, f, :nt], start=(f == 0), stop=(f == NFF - 1)),
                               reads=["f_Wd", (kact, f)], writes=[kpo])
                        op("dve", lambda v: v.scalar_tensor_tensor(out=r[:, n, :nt], in0=hf[:, n, :nt], scalar=ALPHA, in1=po[:, :nt], op0=ALU.mult, op1=ALU.add),
                           reads=[kh, kpo], writes=["f_r"])
                    ln_fm(lt, r, "f_r", hf, kh, gb, "f_gb", nt, zero_pads=(b0 == 0))
                    dma("pool", h_out[:, :, t0:t0 + nt].rearrange("c p t -> p c t"), hf[:, :, :nt], reads=[kh], writes=[("hT", gi)])
                cx.barrier()

        def phase_moe(l, h_in):
            with ExitStack() as s:
                NQF = 4
                SG = min(1024, TH)
                Wg = [sb("e_Wg%d" % i, [128, KC, 2, NQF * 128], BF16, s) for i in range(2)]
                Wd = [sb("e_Wd%d" % i, [128, NQF, D], BF16, s) for i in range(2)]
                gb = load_gb(s, ln_ffn_gb[l], "e_gb")
                wr = sb("e_wr", [128, KC, NEXP], F32, s)
                rb = sb("e_rb", [128, NEXP], F32, s)
                sel = sb("e_sel", [8, NEXP, 128], F32, s)
                with nc.allow_non_contiguous_dma(reason="tiny param"):
                    dma("sp", wr[:], router_w.rearrange("(c p) e -> p c e", p=128), writes=["e_wr"])
                    dma("sp", rb[:], router_b[0].partition_broadcast(128), writes=["e_rb"])
                op("pool", lambda g: g.memset(sel[:], 1.0), writes=["e_sel"])
                op("pool", lambda g: g.affine_select(out=sel[:], in_=sel[:], pattern=[[-1, NEXP], [0, 128]], base=0, channel_multiplier=1,
                                                      compare_op=ALU.is_equal, fill=0.0), reads=["e_sel"], writes=["e_sel"])
                hs = sb("e_hs", [128, KC, SG], F32, s)
                hb = sb("e_hb", [128, KC, SG], BF16, s)
                h2 = sb("e_h2", [128, KC, 256], F32, s)
                accm = sb("e_acc", [128, KC, SG], F32, s)
                cbc = sb("e_cbc", [128, NEXP, SG], BF16, s)
                lg = sb("e_lg", [128, 4, NEXP], F32, s)
                combT = sb("e_combT", [8, SG], F32, s)
                actb = [sb("e_act%d" % i, [128, NQF, 512], BF16, s) for i in range(2)]
                abi = [0]
                sgl2 = [sb("e_sgl%d" % i, [128, 512], F32, s) for i in range(2)]
                a12 = [sb("e_a1%d" % i, [128, 512], F32, s) for i in range(2)]
                osb = sb("e_osb", [128, D], F32, s)
                lt = ln_tiles(s, 256)
                pg = [ps("e_pg%d" % i, [128, 512], F32, s) for i in range(2)]
                pu = [ps("e_pu%d" % i, [128, 512], F32, s) for i in range(2)]
                po = ps("e_po", [128, 512], F32, s)
                po2 = [po, ps("e_po2", [128, 512], F32, s)]
                pok = ["e_po", "e_po2"]
                wi = 0
                stg = [sb("e_stg%d" % i, [128, 4, NQF * 128], F32, s) for i in range(2)]
                stgi = [0]
                for sg0 in range(0, TH, SG):
                    for t1 in range(0, SG, 256):
                        for half in range(2):
                            ta = 128 + half * TH + sg0 + t1
                            dma("sp", h2[:], h_in[:, :, ta:ta + 256].rearrange("c p t -> p c t"), writes=["e_h2"])
                            if half == 0:
                                op("dve", lambda v, t1=t1: v.tensor_scalar(out=hs[:, :, t1:t1 + 256], in0=h2[:], scalar1=hsel[:, 0:1], scalar2=None, op0=ALU.mult),
                                   reads=["e_h2", "hsel"], writes=["e_hs"])
                            else:
                                op("dve", lambda v, t1=t1: v.scalar_tensor_tensor(out=hs[:, :, t1:t1 + 256], in0=h2[:], scalar=hsel[:, 1:2], in1=hs[:, :, t1:t1 + 256], op0=ALU.mult, op1=ALU.add),
                                   reads=["e_h2", "hsel", "e_hs"], writes=["e_hs"])
                    for c in range(KC):
                        evac(c, hb[:, c, :], hs[:, c, :], ["e_hs"], ["e_hb"])
                    for tb in range(SG // 128):
                        for c in range(KC):
                            op("pe", lambda p, c=c, tb=tb: p.matmul(po[:, 0:NEXP], lhsT=hs[:, c, tb * 128:(tb + 1) * 128], rhs=wr[:, c, :], start=(c == 0), stop=(c == KC - 1)),
                               reads=["e_hs", "e_wr"], writes=["e_po"])
                        op("dve", lambda v: v.tensor_tensor(out=lg[:, 0, :], in0=po[:, 0:NEXP], in1=rb[:], op=ALU.add), reads=["e_po", "e_rb"], writes=["e_lg"])
                        op("dve", lambda v: v.max(out=lg[:, 1, :], in_=lg[:, 0, :]), reads=["e_lg"], writes=["e_lg"])
                        op("dve", lambda v: v.tensor_scalar(out=lg[:, 2, :], in0=lg[:, 0, :], scalar1=lg[:, 1, 1:2], scalar2=None, op0=ALU.is_ge), reads=["e_lg"], writes=["e_lg"])
                        op("dve", lambda v: v.tensor_scalar(out=lg[:, 3, 0:1], in0=lg[:, 1, 0:1], scalar1=-1.0, scalar2=None, op0=ALU.mult), reads=["e_lg"], writes=["e_lg"])
                        op("act", lambda a: a.activation(out=lg[:, 0, :], in_=lg[:, 0, :], func=AF.Exp, bias=lg[:, 3, 0:1], scale=1.0), reads=["e_lg"], writes=["e_lg"])
                        op("dve", lambda v: v.tensor_tensor(out=lg[:, 0, :], in0=lg[:, 0, :], in1=lg[:, 2, :], op=ALU.mult), reads=["e_lg"], writes=["e_lg"])
                        op("dve", lambda v: v.tensor_reduce(out=lg[:, 3, 1:2], in_=lg[:, 0, :], axis=AX.X, op=ALU.add), reads=["e_lg"], writes=["e_lg"])
                        op("dve", lambda v: v.reciprocal(out=lg[:, 3, 1:2], in_=lg[:, 3, 1:2]), reads=["e_lg"], writes=["e_lg"])
                        op("dve", lambda v: v.tensor_scalar(out=lg[:, 0, :], in0=lg[:, 0, :], scalar1=lg[:, 3, 1:2], scalar2=None, op0=ALU.mult), reads=["e_lg"], writes=["e_lg"])
                        op("pe", lambda p: p.transpose(out=po[0:8, 128:256], in_=lg[:, 0, :], identity=ident[:]), reads=["e_lg", "ident"], writes=["e_po"])
                        op("dve", lambda v, tb=tb: v.tensor_copy(out=combT[:, tb * 128:(tb + 1) * 128], in_=po[0:8, 128:256]), reads=["e_po"], writes=["e_combT"])
                    for e in range(NEXP):
                        for t1 in range(0, SG, 512):
                            n1 = min(512, SG - t1)
                            op("pe", lambda p, e=e, t1=t1, n1=n1: p.matmul(po[:, :n1], lhsT=sel[:, e, :], rhs=combT[:, t1:t1 + n1], start=True, stop=True),
                               reads=["e_sel", "e_combT"], writes=["e_po"])
                            op("act", lambda a, e=e, t1=t1, n1=n1: a.activation(out=cbc[:, e, t1:t1 + n1], in_=po[:, :n1], func=AF.Copy), reads=["e_po"], writes=["e_cbc"])
                    for e in range(NEXP):
                        for qq in range(FF_EXP // 128 // NQF):
                            w = wi % 2
                            wi += 1
                            f0 = qq * NQF * 128
                            for gu in range(2):
                                srcw = moe_w_gu[e][:, gu * FF_EXP + f0:gu * FF_EXP + f0 + NQF * 128].rearrange("(c p) n -> p c n", p=128)
                                for hc in range(2):
                                    sgi = stgi[0] % 2
                                    stgi[0] += 1
                                    dma("sp", stg[sgi][:], srcw[:, hc * 4:(hc + 1) * 4, :], writes=["e_stg%d" % sgi])
                                    op("act", lambda a, gu=gu, sgi=sgi, hc=hc: a.activation(out=Wg[w][:, hc * 4:(hc + 1) * 4, gu, :], in_=stg[sgi][:], func=AF.Copy),
                                       reads=["e_stg%d" % sgi], writes=[("e_Wg%d" % w, gu, hc)])
                            srcw = moe_w_down[e][f0:f0 + NQF * 128, :].rearrange("(c p) n -> p c n", p=128)
                            for hc in range(2):
                                sgi = stgi[0] % 2
                                stgi[0] += 1
                                stv = stg[sgi][:].rearrange("p c n -> p (c n)").rearrange("p (c n) -> p c n", c=2)
                                dma("sp", stv, srcw[:, hc * 2:(hc + 1) * 2, :], writes=["e_stg%d" % sgi])
                                op("act", lambda a, sgi=sgi, hc=hc, stv=stv: a.activation(out=Wd[w][:, hc * 2:(hc + 1) * 2, :], in_=stv, func=AF.Copy),
                                   reads=["e_stg%d" % sgi], writes=[("e_Wd%d" % w, hc)])
                            for t1 in range(0, SG, 512):
                                n1 = min(512, SG - t1)
                                ab = abi[0] % 2
                                for f in range(NQF):
                                    i = f % 2
                                    for c in range(KC):
                                        op("pe", lambda p, c=c, f=f: p.matmul(pg[i][:, :n1], lhsT=Wg[w][:, c, 0, f * 128:(f + 1) * 128], rhs=hb[:, c, t1:t1 + n1], start=(c == 0), stop=(c == KC - 1)),
                                           reads=[("e_Wg%d" % w, 0, 0), ("e_Wg%d" % w, 0, 1), "e_hb"], writes=["e_pg%d" % i])
                                    for c in range(KC):
                                        op("pe", lambda p, c=c, f=f: p.matmul(pu[i][:, :n1], lhsT=Wg[w][:, c, 1, f * 128:(f + 1) * 128], rhs=hb[:, c, t1:t1 + n1], start=(c == 0), stop=(c == KC - 1)),
                                           reads=[("e_Wg%d" % w, 1, 0), ("e_Wg%d" % w, 1, 1), "e_hb"], writes=["e_pu%d" % i])
                                    sgl, a1 = sgl2[i], a12[i]
                                    op("act", lambda a: a.activation(out=sgl[:, :n1], in_=pg[i][:, :n1], func=AF.Silu), reads=["e_pg%d" % i], writes=["e_sgl%d" % i])
                                    op("dve", lambda v: v.tensor_tensor(out=a1[:, :n1], in0=pu[i][:, :n1], in1=sgl[:, :n1], op=ALU.mult), reads=["e_pu%d" % i, "e_sgl%d" % i], writes=["e_a1%d" % i])
                                    op("dve", lambda v, f=f: v.tensor_tensor(out=actb[ab][:, f, :n1], in0=a1[:, :n1], in1=cbc[:, e, t1:t1 + n1], op=ALU.mult), reads=["e_a1%d" % i, "e_cbc"], writes=["e_act%d" % ab])
                                for n in range(8):
                                    pq = po2[n % 2]
                                    for f in range(NQF):
                                        op("pe", lambda p, f=f: p.matmul(pq[:, :n1], lhsT=Wd[w][:, f, n * 128:(n + 1) * 128], rhs=actb[ab][:, f, :n1], start=(f == 0), stop=(f == NQF - 1)),
                                           reads=[("e_Wd%d" % w, 0), ("e_Wd%d" % w, 1), "e_act%d" % ab], writes=[pok[n % 2]])
                                    if e == 0 and qq == 0:
                                        op("dve", lambda v, n=n: v.tensor_copy(out=accm[:, n, t1:t1 + n1], in_=pq[:, :n1]), reads=[pok[n % 2]], writes=[("e_acc", n, t1)])
                                    else:
                                        op("dve", lambda v, n=n: v.tensor_tensor(out=accm[:, n, t1:t1 + n1], in0=accm[:, n, t1:t1 + n1], in1=pq[:, :n1], op=ALU.add), reads=[pok[n % 2], ("e_acc", n, t1)], writes=[("e_acc", n, t1)])
                                abi[0] += 1
                    for t1 in range(0, SG, 256):
                        for n in range(8):
                            op("dve", lambda v, n=n: v.scalar_tensor_tensor(out=h2[:, n, :], in0=hs[:, n, t1:t1 + 256], scalar=ALPHA, in1=accm[:, n, t1:t1 + 256], op0=ALU.mult, op1=ALU.add),
                               reads=["e_hs", ("e_acc", n, (t1 // 512) * 512)], writes=["e_h2"])
                        ln_fm(lt, h2, "e_h2", h2, "e_h2o", gb, "e_gb", 256, zero_pads=False)
                        for tb in range(2):
                            for c in range(KC):
                                op("pe", lambda p, c=c, tb=tb: p.transpose(out=pg[c // 4][:, (c % 4) * 128:(c % 4 + 1) * 128], in_=h2[:, c, tb * 128:(tb + 1) * 128], identity=ident[:]),
                                   reads=["e_h2o", "e_h2", "ident"], writes=["e_pg%d" % (c // 4)])
                            for hh in range(2):
                                evac(hh, osb[:, hh * 512:(hh + 1) * 512], pg[hh][:, :], ["e_pg%d" % hh], ["e_osb"])
                            r0 = sg0 + t1 + tb * 128
                            dma("pool", out[r0:r0 + 128, :], osb[:], reads=["e_osb"], writes=[("out", r0)])
                cx.barrier()

        stages = set(STAGES)
        if "ln_in" in stages:
            phase_ln_in(hT[0])
        if "win0" in stages:
            phase_win(0, hT[0])
        if "cprep0" in stages:
            phase_cprep(0)
        if "attn0" in stages:
            phase_attn(0)
        if "ssd0" in stages:
            phase_ssd(0)
        if "merge0" in stages:
            phase_merge(0, hT[0], hT[1])
        if "ffn0" in stages:
            phase_ffn_dense(0, hT[1], hT[0])
        if "layer1" in stages:
            phase_win(1, hT[0])
            phase_cprep(1)
            phase_attn(1)
            phase_ssd(1)
            phase_merge(1, hT[0], hT[1])
        if "moe" in stages:
            phase_moe(1, hT[1])

        cx.barrier()
    nc._in_names = in_names
    return nc


ALL_STAGES = ("ln_in", "win0", "cprep0", "attn0", "ssd0", "merge0", "ffn0", "layer1", "moe")


def kernel(**inputs):
    x = np.asarray(inputs["x"])
    B, S, _ = x.shape
    NB = S // 128 + 1
    HB = (NB - 1) // 2
    nc = build_program(NB, STAGES=ALL_STAGES)
    maps = prep_inputs(inputs, NB)
    maps = [{k: m[k] for k in nc._in_names} for m in maps]
    res = run_bass_kernel_spmd(nc, maps, core_ids=list(range(8)))
    out = np.zeros((B, S, D), np.float32)
    for c in range(8):
        b, half = c // 2, c % 2
        out[b, half * HB * 128:(half + 1) * HB * 128] = np.asarray(res.results[c]["out"])
    return out


def prep_inputs(inp, NB):
    S = (NB - 1) * 128
    f = lambda a: np.ascontiguousarray(np.asarray(a, dtype=np.float32))
    x = f(inp["x"])
    B = x.shape[0]
    shared = {
        "ln_in_gb": f(np.stack([inp["ln_in_g"], inp["ln_in_b"]], 0)),
        "w_in": f(inp["w_in"]),
        "b_forget": f(inp["b_forget"]),
        "ssd_conv_w": f(inp["ssd_conv_w"]),
        "ssd_conv_b": f(inp["ssd_conv_b"]),
        "ssd_vec": f(np.stack([inp["ssd_dt_bias"], inp["ssd_a_log"], inp["ssd_d"]], 1)),
        "ssd_norm_w": f(inp["ssd_norm_w"]),
        "sc_conv_w": f(inp["sc_conv_w"]),
        "w_proj": f(np.concatenate([inp["w_proj_attn"], inp["w_proj_ssd"], inp["w_proj_conv"]], 1)),
        "w_out": f(inp["w_out"]),
        "ln_mix_gb": f(np.stack([inp["ln_mix_g"], inp["ln_mix_b"]], 1)),
        "dense_w_gu": f(inp["dense_w_gu"][0]),
        "dense_w_down": f(inp["dense_w_down"][0]),
        "router_w": f(inp["router_w"][0]),
        "router_b": f(np.asarray(inp["router_b"]).reshape(1, NEXP)),
        "moe_w_gu": f(inp["moe_w_gu"][0]),
        "moe_w_down": f(inp["moe_w_down"][0]),
        "ln_ffn_gb": f(np.stack([inp["ln_ffn_g"], inp["ln_ffn_b"]], 1)),
    }
    maps = []
    for c in range(8):
        b, half = (c // 2) % B, c % 2
        xin = np.zeros((NB * 128, D), np.float32)
        xin[112:128] = f(inp["meta_tokens"])
        xin[128:] = x[b, :S]
        hs = np.zeros((128, 2), np.float32)
        hs[:, half] = 1.0
        m = dict(shared)
        m["xin"] = xin
        m["halfsel"] = hs
        maps.append(m)
    return maps
```

```python
import numpy as np
from contextlib import ExitStack
import concourse.bass as bass
import concourse.mybir as mybir
from concourse.bass_utils import run_bass_kernel_spmd

F32 = mybir.dt.float32
BF16 = mybir.dt.bfloat16
AF = mybir.ActivationFunctionType
ALU = mybir.AluOpType
AX = mybir.AxisListType

D = 1024
KC = 8
NQ = 512
FF_DENSE = 2816
FF_EXP = 3584
NEXP = 8
ALPHA = 4 ** 0.25
LN_EPS = 1e-5
RMS_EPS = 1e-5
O_Q, O_K, O_V, O_F, O_Z, O_XBC, O_DT, O_SC, O_G = 0, 512, 1024, 1536, 1544, 2568, 4104, 4120, 5656
N_IN = 8728


class Res:
    __slots__ = ("w", "r")

    def __init__(self):
        self.w = None
        self.r = []


class Eng:
    def __init__(self, name, h, sem):
        self.name, self.h, self.sem = name, h, sem
        self.count = 0
        self.known = {}


class Ctx:
    def __init__(self, nc, st, n_dma_sems=24):
        self.nc = nc
        self.res = {}
        self.eng = {}
        for name, h in (("pe", nc.tensor), ("act", nc.scalar), ("dve", nc.vector), ("pool", nc.gpsimd), ("sp", nc.sync)):
            self.eng[name] = Eng(name, h, st.enter_context(nc.semaphore("sem_" + name)))
        self.dsems = [st.enter_context(nc.semaphore("dsem%d" % i)) for i in range(2 * n_dma_sems)]
        self.dval = [0] * len(self.dsems)
        self.dq = {"sp": list(range(0, n_dma_sems)), "pool": list(range(n_dma_sems, 2 * n_dma_sems))}
        self.dqi = {"sp": 0, "pool": 0}

    def R(self, key):
        r = self.res.get(key)
        if r is None:
            r = self.res[key] = Res()
        return r

    def _wait(self, E, deps, raw_self_only):
        need = {}
        for d in deps:
            k = (d[0], d[1])
            if need.get(k, 0) < d[2]:
                need[k] = d[2]
        for (kind, key), val in need.items():
            if kind == "e" and key == E.name:
                if E.name in ("pe", "sp"):
                    continue
            if E.known.get((kind, key), 0) >= val:
                continue
            sem = self.eng[key].sem if kind == "e" else self.dsems[key]
            E.h.wait_ge(sem, val)
            E.known[(kind, key)] = val

    def _collect(self, reads, writes):
        deps, raw = [], set()
        for k in reads:
            r = self.R(k)
            if r.w is not None:
                deps.append(r.w)
                raw.add(r.w)
        for k in writes:
            r = self.R(k)
            if r.w is not None:
                deps.append(r.w)
            deps.extend(r.r)
        return deps, raw

    def _commit(self, tok, reads, writes):
        for k in reads:
            self.R(k).r.append(tok)
        for k in writes:
            r = self.R(k)
            r.w = tok
            r.r = []

    def op(self, en, fn, reads=(), writes=()):
        E = self.eng[en]
        deps, raw = self._collect(reads, writes)
        self._wait(E, deps, raw)
        inst = fn(E.h)
        E.count += 1
        inst.then_inc(E.sem, 1)
        self._commit(("e", en, E.count), reads, writes)

    def dma(self, q, out, in_, reads=(), writes=(), **kw):
        E = self.eng[q]
        lst = self.dq[q]
        si = lst[self.dqi[q] % len(lst)]
        self.dqi[q] += 1
        deps, raw = self._collect(reads, writes)
        if self.dval[si] > 0:
            deps.append(("d", si, self.dval[si]))
        self._wait(E, deps, raw)
        E.h.dma_start(out=out, in_=in_, **kw).then_inc(self.dsems[si], 16)
        self.dval[si] += 16
        self._commit(("d", si, self.dval[si]), reads, writes)

    def barrier(self):
        deps = [("e", n, e.count) for n, e in self.eng.items() if e.count > 0 and n != "sp"]
        deps += [("d", i, v) for i, v in enumerate(self.dval) if v > 0]
        for E in self.eng.values():
            self._wait(E, deps, set(deps))
        self.res = {}


def _blocks(nb, g):
    out = []
    b = 0
    while b < nb:
        n = min(g, nb - b)
        out.append((b, n))
        b += n
    return out


def build_program(NB, debug=(), STAGES=("ln_in", "win0", "cprep0", "attn0")):
    T = NB * 128
    HB = (NB - 1) // 2
    TH = HB * 128
    nc = bass.Bass("TRN2", target_bir_lowering=False)
    dbg = set(debug)

    in_names = []

    def dram_in(name, shape, dt=F32):
        if name.startswith("moe_w") and "moe" not in STAGES:
            return None
        in_names.append(name)
        return nc.dram_tensor(name, list(shape), dt, kind="ExternalInput").ap()

    def dram_scr(name, shape, dt):
        kind = "ExternalOutput" if name in dbg else "Internal"
        return nc.dram_tensor(name, list(shape), dt, kind=kind).ap()

    xin = dram_in("xin", [T, D])
    ln_in_gb = dram_in("ln_in_gb", [2, D])
    w_in = dram_in("w_in", [2, D, N_IN])
    b_forget = dram_in("b_forget", [2, 8])
    ssd_conv_w = dram_in("ssd_conv_w", [2, 4, 1536])
    ssd_conv_b = dram_in("ssd_conv_b", [2, 1536])
    ssd_vec = dram_in("ssd_vec", [2, 3, 16])
    ssd_norm_w = dram_in("ssd_norm_w", [2, 1024])
    sc_conv_w = dram_in("sc_conv_w", [2, 3, 512])
    w_proj = dram_in("w_proj", [2, 2048, D])
    w_out = dram_in("w_out", [2, D, D])
    ln_mix_gb = dram_in("ln_mix_gb", [2, 2, D])
    dense_w_gu = dram_in("dense_w_gu", [D, 2 * FF_DENSE])
    dense_w_down = dram_in("dense_w_down", [FF_DENSE, D])
    router_w = dram_in("router_w", [D, NEXP])
    router_b = dram_in("router_b", [1, NEXP])
    moe_w_gu = dram_in("moe_w_gu", [NEXP, D, 2 * FF_EXP])
    moe_w_down = dram_in("moe_w_down", [NEXP, FF_EXP, D])
    ln_ffn_gb = dram_in("ln_ffn_gb", [2, 2, D])
    halfsel = dram_in("halfsel", [128, 2])
    out = nc.dram_tensor("out", [TH, D], F32, kind="ExternalOutput").ap()

    hT = [dram_scr("hT%d" % i, [KC, 128, T], F32) for i in range(2)]
    qkT = dram_scr("qkT", [8, 128, T], BF16)
    xbcT = dram_scr("xbcT", [12, 128, T + 128], BF16)
    scT = dram_scr("scT", [12, 128, T + 128], BF16)
    gT = dram_scr("gT", [24, 128, T], BF16)
    vtm = dram_scr("vtm", [T, 512], BF16)
    ztm = dram_scr("ztm", [T, 1024], BF16)
    dtr = dram_scr("dtr", [T, 16], F32)
    fT = dram_scr("fT", [8, T], F32)
    caugQ = dram_scr("caugQ", [8, 6, T], BF16)
    caugK = dram_scr("caugK", [8, 6, T], BF16)
    yattT = dram_scr("yattT", [4, 128, T], BF16)
    yssdT = dram_scr("yssdT", [8, 128, T], BF16)

    with ExitStack() as st:
        cx = Ctx(nc, st)
        Rk = cx.R
        rec_list = [None]

        def op(en, fn, reads=(), writes=()):
            if rec_list[0] is not None:
                rec_list[0].append(("op", en, fn, tuple(reads), tuple(writes), None))
            else:
                cx.op(en, fn, reads, writes)

        def dma(q, out, in_, reads=(), writes=(), **kw):
            if rec_list[0] is not None:
                rec_list[0].append(("dma", q, (out, in_), tuple(reads), tuple(writes), kw))
            else:
                cx.dma(q, out, in_, reads, writes, **kw)

        def record(f, *a):
            rec_list[0] = []
            f(*a)
            lst = rec_list[0]
            rec_list[0] = None
            return lst

        def replay(item):
            kind, e, x, rd, wr, kw = item
            if kind == "op":
                cx.op(e, x, rd, wr)
            else:
                cx.dma(e, x[0], x[1], rd, wr, **kw)

        def replay_interleaved(l1, l2):
            i = j = 0
            n1, n2 = len(l1), len(l2)
            while i < n1 or j < n2:
                if j >= n2 or (i < n1 and i * n2 <= j * n1):
                    replay(l1[i])
                    i += 1
                else:
                    replay(l2[j])
                    j += 1

        uid = [0]

        def sb(name, shape, dt, stk):
            uid[0] += 1
            return stk.enter_context(nc.sbuf_tensor("%s_u%d" % (name, uid[0]), list(shape), dt))

        def ps(name, shape, dt, stk):
            uid[0] += 1
            return stk.enter_context(nc.psum_tensor("%s_u%d" % (name, uid[0]), list(shape), dt))

        ident = sb("ident", [128, 128], F32, st)
        identb = sb("identb", [128, 128], BF16, st)
        ones_f = sb("ones_f", [128, 128], F32, st)
        tri_le = sb("tri_le", [128, 128], F32, st)
        tri_leb = sb("tri_leb", [128, 128], BF16, st)
        smask = sb("smask", [128, 128], F32, st)
        validcol = sb("validcol", [128, 1], F32, st)
        hsel = sb("hsel", [128, 2], F32, st)

        def c_init(g):
            return g.memset(ones_f[:], 1.0)
        op("pool", c_init, writes=["ones_f"])
        op("pool", lambda g: g.memset(ident[:], 0.0), writes=["ident"])
        op("pool", lambda g: g.affine_select(out=ident[:], in_=ones_f[:], pattern=[[-1, 128]], base=0,
                                              channel_multiplier=1, compare_op=ALU.is_equal, fill=0.0),
           reads=["ones_f"], writes=["ident"])
        op("pool", lambda g: g.affine_select(out=tri_le[:], in_=ones_f[:], pattern=[[1, 128]], base=0,
                                              channel_multiplier=-1, compare_op=ALU.is_ge, fill=0.0),
           reads=["ones_f"], writes=["tri_le"])
        op("pool", lambda g: g.affine_select(out=smask[:], in_=ones_f[:], pattern=[[-1, 128]], base=0,
                                              channel_multiplier=1, compare_op=ALU.is_gt, fill=0.0),
           reads=["ones_f"], writes=["smask"])
        op("pool", lambda g: g.affine_select(out=validcol[:], in_=ones_f[:, 0:1], pattern=[[0, 1]], base=-112,
                                              channel_multiplier=1, compare_op=ALU.is_ge, fill=0.0),
           reads=["ones_f"], writes=["validcol"])
        op("dve", lambda v: v.tensor_copy(out=identb[:], in_=ident[:]), reads=["ident"], writes=["identb"])
        op("dve", lambda v: v.tensor_copy(out=tri_leb[:], in_=tri_le[:]), reads=["tri_le"], writes=["tri_leb"])
        dma("sp", hsel[:], halfsel[:, :], writes=["hsel"])

        def phase_ln_in(hT_out):
            with ExitStack() as s:
                gbc = sb("p0_gb", [128, 2, KC], F32, s)
                xt = [sb("p0_x%d" % i, [128, D], F32, s) for i in range(2)]
                xn = [sb("p0_xn%d" % i, [128, D], F32, s) for i in range(2)]
                stt = [sb("p0_st%d" % i, [128, 2, 6], F32, s) for i in range(2)]
                mv = [sb("p0_mv%d" % i, [128, 4], F32, s) for i in range(2)]
                ho = [sb("p0_ho%d" % i, [128, KC, 128], F32, s) for i in range(2)]
                pt = [ps("p0_pt%d" % i, [128, D], F32, s) for i in range(2)]
                with nc.allow_non_contiguous_dma(reason="tiny param"):
                    dma("sp", gbc[:], ln_in_gb.rearrange("t (c p) -> p t c", p=128), writes=["p0_gb"])
                for b in range(NB):
                    i = b % 2
                    kx, kn, ks, km, kh, kp = ("p0_x%d" % i, "p0_xn%d" % i, "p0_st%d" % i, "p0_mv%d" % i, "p0_ho%d" % i, "p0_pt%d" % i)
                    dma("sp", xt[i][:], xin[b * 128:(b + 1) * 128, :], writes=[kx])
                    for hh in range(2):
                        op("dve", lambda v, hh=hh: v.bn_stats(out=stt[i][:, hh, :], in_=xt[i][:, hh * 512:(hh + 1) * 512]),
                           reads=[kx], writes=[ks])
                    op("dve", lambda v: v.bn_aggr(out=mv[i][:, 0:2], in_=stt[i][:].rearrange("p a b -> p (a b)")),
                       reads=[ks], writes=[km])
                    op("act", lambda a: a.activation(out=mv[i][:, 2:3], in_=mv[i][:, 1:2], func=AF.Sqrt, bias=LN_EPS_T[:], scale=1.0),
                       reads=[km, "eps"], writes=[km])
                    op("dve", lambda v: v.reciprocal(out=mv[i][:, 3:4], in_=mv[i][:, 2:3]), reads=[km], writes=[km])
                    op("dve", lambda v: v.tensor_scalar(out=xn[i][:], in0=xt[i][:], scalar1=mv[i][:, 0:1], scalar2=mv[i][:, 3:4],
                                                        op0=ALU.subtract, op1=ALU.mult), reads=[kx, km], writes=[kn])
                    for c in range(KC):
                        op("pe", lambda p, c=c: p.transpose(out=pt[i][:, c * 128:(c + 1) * 128], in_=xn[i][:, c * 128:(c + 1) * 128], identity=ident[:]),
                           reads=[kn, "ident"], writes=[kp])
                    for c in range(KC):
                        op("act", lambda a, c=c: a.activation(out=ho[i][:, c, :], in_=pt[i][:, c * 128:(c + 1) * 128], func=AF.Identity,
                                                               scale=gbc[:, 0, c:c + 1], bias=gbc[:, 1, c:c + 1]),
                           reads=[kp, "p0_gb"], writes=[kh])
                    if b == 0:
                        op("dve", lambda v: v.memset(ho[i][:, :, 0:112], 0.0), reads=[], writes=[kh])
                    dma("pool", hT_out[:, :, b * 128:(b + 1) * 128].rearrange("c p t -> p c t"), ho[i][:], reads=[kh], writes=[("hT", b)])
                cx.barrier()

        LN_EPS_T = sb("eps_t", [128, 1], F32, st)
        op("pool", lambda g: g.memset(LN_EPS_T[:], LN_EPS), writes=["eps"])
        RMS_EPS_T = LN_EPS_T

        def evac(i, out_ap, in_ap, reads, writes):
            if i % 2 == 0:
                op("act", lambda a: a.activation(out=out_ap, in_=in_ap, func=AF.Copy), reads=reads, writes=writes)
            else:
                op("dve", lambda v: v.tensor_copy(out=out_ap, in_=in_ap), reads=reads, writes=writes)

        wst_i = [0]

        def mk_wstg(s):
            return [sb("wstg%d" % i, [128, 2048], F32, s) for i in range(3)]

        def load_w(dst, src2d, ncols, key, rows_c, wstg):
            for c in range(rows_c):
                n0 = 0
                while n0 < ncols:
                    n1 = min(ncols, n0 + 2048)
                    si = wst_i[0] % 3
                    wst_i[0] += 1
                    dma("sp", wstg[si][:, :n1 - n0], src2d[c * 128:(c + 1) * 128, n0:n1], writes=["wstg%d" % si])
                    if si == 0:
                        op("act", lambda a: a.activation(out=dst[:, c, n0:n1], in_=wstg[si][:, :n1 - n0], func=AF.Copy), reads=["wstg%d" % si], writes=[key])
                    elif si == 1:
                        op("dve", lambda v: v.tensor_copy(out=dst[:, c, n0:n1], in_=wstg[si][:, :n1 - n0]), reads=["wstg%d" % si], writes=[key])
                    else:
                        op("pool", lambda v: v.tensor_copy(out=dst[:, c, n0:n1], in_=wstg[si][:, :n1 - n0]), reads=["wstg%d" % si], writes=[key])
                    n0 = n1

        def phase_win(l, h_in):
            for pas in (0, 1):
                with ExitStack() as s:
                    if pas == 0:
                        segs = [(O_Q, 1024), (O_XBC, 1536), (O_V, 512), (O_Z, 1024), (O_F, 8), (O_DT, 16)]
                    else:
                        segs = [(O_SC, 1536), (O_G, 3072)]
                    ncols = sum(n for _, n in segs)
                    W = sb("p1_W", [128, KC, ncols], BF16, s)
                    wstg = mk_wstg(s)
                    off = {}
                    o = 0
                    for (c0, n) in segs:
                        load_w(W[:, :, o:o + n], w_in[l][:, c0:c0 + n], n, "p1_W", KC, wstg)
                        off[c0] = o
                        o += n
                    hf = [sb("p1_hf%d" % i, [128, KC, 512], F32, s) for i in range(2)]
                    hb = [sb("p1_hb%d" % i, [128, KC, 512], BF16, s) for i in range(2)]
                    ob = [sb("p1_ob%d" % i, [128, 4, 512], BF16, s) for i in range(3)]
                    pf = [ps("p1_pf%d" % i, [128, 512], F32, s) for i in range(4)]
                    if pas == 0:
                        zt = sb("p1_z", [128, 128], BF16, s)
                        op("dve", lambda v: v.memset(zt[:], 0.0), writes=["p1_z"])
                        for cc in range(12):
                            dma("pool", xbcT[cc, :, 0:128], zt[:], reads=["p1_z"], writes=[("xbcT", -1)])
                            dma("pool", scT[cc, :, 0:128], zt[:], reads=["p1_z"], writes=[("scT", -1)])
                        tv = sb("p1_tv", [128, 4, 512], BF16, s)
                        tz = sb("p1_tz", [128, 4, 1024], BF16, s)
                        tdt = sb("p1_tdt", [128, 4, 16], F32, s)
                        fst = sb("p1_fst", [8, 512], F32, s)
                        pt = [ps("p1_pt%d" % i, [128, 512], F32, s) for i in range(3)]
                    ei = 0
                    pi = 0
                    obi = 0
                    for gi, (b0, nb) in enumerate(_blocks(NB, 4)):
                        i = gi % 2
                        t0, nt = b0 * 128, nb * 128
                        dma("sp", hf[i][:, :, :nt], h_in[:, :, t0:t0 + nt].rearrange("c p t -> p c t"), writes=["p1_hf%d" % i])
                        for c in range(KC):
                            evac(c, hb[i][:, c, :nt], hf[i][:, c, :nt], ["p1_hf%d" % i], ["p1_hb%d" % i])
                        if pas == 0:
                            fm = [(O_Q, 8, qkT, 0, 0), (O_XBC, 12, xbcT, 0, 128)]
                        else:
                            fm = [(O_SC, 12, scT, 0, 128), (O_G, 24, gT, 0, 0)]
                        for (c0, nch, dst, dch, dof) in fm:
                            for n4 in range(0, nch, 4):
                                oi = obi % 3
                                obi += 1
                                for n in range(n4, n4 + 4):
                                    pp = pi % 4
                                    pi += 1
                                    col = off[c0] + n * 128
                                    for c in range(KC):
                                        op("pe", lambda p, c=c, col=col, pp=pp: p.matmul(pf[pp][:, :nt], lhsT=W[:, c, col:col + 128], rhs=hb[i][:, c, :nt],
                                                                                          start=(c == 0), stop=(c == KC - 1)),
                                           reads=["p1_W", "p1_hb%d" % i], writes=["p1_pf%d" % pp])
                                    evac(ei, ob[oi][:, n - n4, :nt], pf[pp][:, :nt], ["p1_pf%d" % pp], ["p1_ob%d" % oi])
                                    ei += 1
                                dma("pool", dst[dch + n4:dch + n4 + 4, :, dof + t0:dof + t0 + nt].rearrange("c p t -> p c t"), ob[oi][:, :, :nt],
                                    reads=["p1_ob%d" % oi], writes=[(id(dst), gi, n4)])
                        if pas == 0:
                            pp = pi % 4
                            pi += 1
                            col = off[O_F]
                            for c in range(KC):
                                op("pe", lambda p, c=c, col=col, pp=pp: p.matmul(pf[pp][0:8, :nt], lhsT=W[:, c, col:col + 8], rhs=hb[i][:, c, :nt],
                                                                                  start=(c == 0), stop=(c == KC - 1)),
                                   reads=["p1_W", "p1_hb%d" % i], writes=["p1_pf%d" % pp])
                            op("dve", lambda v, pp=pp: v.tensor_copy(out=fst[:, :nt], in_=pf[pp][0:8, :nt]), reads=["p1_pf%d" % pp], writes=["p1_fst"])
                            dma("pool", fT[:, t0:t0 + nt], fst[:, :nt], reads=["p1_fst"], writes=[("fT", gi)])
                            pti = 0
                            for bi in range(nb):
                                for (c0, n, dstt, do) in ((O_V, 512, tv, 0), (O_Z, 512, tz, 0), (O_Z + 512, 512, tz, 512), (O_DT, 16, tdt, 0)):
                                    pq = pti % 3
                                    pti += 1
                                    col = off[O_Z] + (c0 - O_Z) if c0 >= O_Z and c0 < O_XBC else off[c0]
                                    for c in range(KC):
                                        op("pe", lambda p, c=c, col=col, pq=pq, n=n: p.matmul(pt[pq][:, :n], lhsT=hb[i][:, c, bi * 128:(bi + 1) * 128], rhs=W[:, c, col:col + n],
                                                                                                start=(c == 0), stop=(c == KC - 1)),
                                           reads=["p1_W", "p1_hb%d" % i], writes=["p1_pt%d" % pq])
                                    evac(ei, dstt[:, bi, do:do + n], pt[pq][:, :n], ["p1_pt%d" % pq], [id(dstt)])
                                    ei += 1
                            dma("pool", vtm[t0:t0 + nt, :].rearrange("(b p) n -> p b n", p=128), tv[:, :nb, :], reads=[id(tv)], writes=[("vtm", gi)])
                            dma("pool", ztm[t0:t0 + nt, :].rearrange("(b p) n -> p b n", p=128), tz[:, :nb, :], reads=[id(tz)], writes=[("ztm", gi)])
                            with nc.allow_non_contiguous_dma(reason="small dt rows"):
                                dma("pool", dtr[t0:t0 + nt, :].rearrange("(b p) n -> p b n", p=128), tdt[:, :nb, :], reads=[id(tdt)], writes=[("dtr", gi)])
                    cx.barrier()

        def phase_cprep(l):
            with ExitStack() as s:
                t1 = sb("c_t1", [8, T], F32, s)
                t2 = sb("c_t2", [8, T], F32, s)
                t3 = sb("c_t3", [8, T], F32, s)
                b1 = sb("c_b1", [8, T], BF16, s)
                b2 = sb("c_b2", [8, T], BF16, s)
                b3 = sb("c_b3", [8, T], BF16, s)
                nb_ = sb("c_nb", [8, 1], F32, s)
                with nc.allow_non_contiguous_dma(reason="tiny param"):
                    dma("sp", nb_[:], b_forget[l].rearrange("(h o) -> h o", o=1), writes=["c_nb"])
                dma("sp", t1[:], fT[:, :], writes=["c_t1"])
                op("dve", lambda v: v.tensor_scalar(out=nb_[:], in0=nb_[:], scalar1=-1.0, scalar2=None, op0=ALU.mult), reads=["c_nb"], writes=["c_nb"])
                op("act", lambda a: a.activation(out=t1[:], in_=t1[:], func=AF.Exp, scale=-1.0, bias=nb_[:]), reads=["c_t1", "c_nb"], writes=["c_t1"])
                op("act", lambda a: a.activation(out=t1[:], in_=t1[:], func=AF.Ln, scale=1.0, bias=ones_f[0:8, 0:1]), reads=["c_t1", "ones_f"], writes=["c_t1"])
                op("dve", lambda v: v.memset(t1[:, 0:112], 0.0), writes=["c_t1"])
                op("dve", lambda v: v.memset(t2[:], 1.0), writes=["c_t2"])
                op("dve", lambda v: v.tensor_tensor_scan(out=t3[:], data0=t2[:], data1=t1[:], initial=0.0, op0=ALU.mult, op1=ALU.add),
                   reads=["c_t1", "c_t2"], writes=["c_t3"])

                def split3(dst, rows):
                    bb = [b1, b2, b1]
                    kk = ["c_b1", "c_b2", "c_b1"]
                    for j in range(3):
                        op("dve", lambda v, j=j: v.tensor_copy(out=bb[j][:], in_=t1[:]), reads=["c_t1"], writes=[kk[j]])
                        dma("sp", dst[:, rows[j], :], bb[j][:], reads=[kk[j]], writes=[(id(dst), rows[j])])
                        if j < 2:
                            op("dve", lambda v, j=j: v.tensor_copy(out=t2[:], in_=bb[j][:]), reads=[kk[j]], writes=["c_t2"])
                            op("dve", lambda v: v.tensor_tensor(out=t1[:], in0=t1[:], in1=t2[:], op=ALU.subtract), reads=["c_t1", "c_t2"], writes=["c_t1"])

                op("dve", lambda v: v.tensor_scalar(out=t1[:], in0=t3[:], scalar1=8.0, scalar2=None, op0=ALU.mult), reads=["c_t3"], writes=["c_t1"])
                op("dve", lambda v: v.memset(t1[:, 0:112], -240000.0), writes=["c_t1"])
                split3(caugK, [3, 4, 5])
                t3v = t3[:, :].rearrange("h (b t) -> h b t", t=128)
                op("dve", lambda v: v.tensor_scalar(out=t1[:, :].rearrange("h (b t) -> h b t", t=128), in0=t3v[:, :, 127:128].broadcast_to([8, NB, 128]),
                                                    scalar1=-4.0, scalar2=None, op0=ALU.mult), reads=["c_t3"], writes=["c_t1"])
                if NB > 1:
                    src_ = t3[:, 0:T - 128].rearrange("h (b t) -> h b t", t=128)[:, :, 127:128].broadcast_to([8, NB - 1, 128])
                    op("dve", lambda v: v.scalar_tensor_tensor(out=t1[:, 128:T].rearrange("h (b t) -> h b t", t=128), in0=src_, scalar=-4.0,
                                                               in1=t1[:, 128:T].rearrange("h (b t) -> h b t", t=128), op0=ALU.mult, op1=ALU.add),
                       reads=["c_t3", "c_t1"], writes=["c_t1"])
                split3(caugQ, [0, 1, 2])
                op("dve", lambda v: v.memset(b3[:], 1.0), writes=["c_b3"])
                for r in (3, 4, 5):
                    dma("sp", caugQ[:, r, :], b3[:], reads=["c_b3"], writes=[(id(caugQ), r)])
                for r in (0, 1, 2):
                    dma("sp", caugK[:, r, :], b3[:], reads=["c_b3"], writes=[(id(caugK), r)])
                cx.barrier()

        def phase_attn(l):
            with ExitStack() as s:
                Qp = [sb("a_Q%d" % i, [128, T], BF16, s) for i in range(2)]
                Kp = [sb("a_K%d" % i, [128, T], BF16, s) for i in range(2)]
                Vp = [sb("a_V%d" % i, [128, NB, 128], BF16, s) for i in range(2)]
                NSB = 3
                sbase = [0]
                obase = [0]
                Pt = [sb("a_P%d" % i, [128, 1024], BF16, s) for i in range(NSB)]
                rec = sb("a_rec", [128, 512], F32, s)
                yo = [sb("a_yo%d" % i, [64, 512], BF16, s) for i in range(2)]
                Sp = [ps("a_S%d" % i, [128, 1024], F32, s) for i in range(NSB)]
                Op = [ps("a_O%d" % i, [128, 512], F32, s) for i in range(2)]
                for i in range(2):
                    op("dve", lambda v, i=i: v.memset(Vp[i][:, :, 64:128], 1.0), writes=["a_V%d" % i])
                for h in range(8):
                    i = h % 2
                    r0 = (h % 2) * 64
                    dma("sp", Qp[i][0:64, :], qkT[h // 2, r0:r0 + 64, :], writes=["a_Q%d" % i])
                    dma("sp", Qp[i][64:70, :], caugQ[h], writes=["a_Q%d" % i])
                    dma("sp", Kp[i][0:64, :], qkT[4 + h // 2, r0:r0 + 64, :], writes=["a_K%d" % i])
                    dma("sp", Kp[i][64:70, :], caugK[h], writes=["a_K%d" % i])
                    with nc.allow_non_contiguous_dma(reason="128B v rows"):
                        for (vb0, vnb) in _blocks(NB, 8):
                            dma("sp", Vp[i][:, vb0:vb0 + vnb, 0:64],
                                vtm[vb0 * 128:(vb0 + vnb) * 128, h * 64:(h + 1) * 64].rearrange("(b p) d -> p b d", p=128), writes=["a_V%d" % i])
                    steps = []
                    for gi, (q0b, nqb) in enumerate(_blocks(NB, 4)):
                        js = list(range(q0b + nqb))
                        for a_ in range(0, len(js), 2):
                            parts = []
                            off = 0
                            for j in js[a_:a_ + 2]:
                                qs = max(q0b, j)
                                ncol = (q0b + nqb - qs) * 128
                                if off + ncol > 512 and off > 0:
                                    off = 512
                                parts.append((j, qs, ncol, off))
                                off += ncol
                            steps.append((gi, q0b, nqb, parts))
                    LOOK = NSB - 1

                    def emit_qk(k):
                        gi, q0b, nqb, parts = steps[k]
                        sp_ = (sbase[0] + k) % NSB
                        for (j, qs, ncol, off) in parts:
                            op("pe", lambda p: p.matmul(Sp[sp_][:, off:off + ncol], lhsT=Kp[i][0:70, j * 128:(j + 1) * 128],
                                                        rhs=Qp[i][0:70, qs * 128:qs * 128 + ncol], start=True, stop=True),
                               reads=["a_Q%d" % i, "a_K%d" % i], writes=["a_S%d" % sp_])

                    def emit_rest(k):
                        gi, q0b, nqb, parts = steps[k]
                        q1b = q0b + nqb
                        sp_ = (sbase[0] + k) % NSB
                        oo = (obase[0] + gi) % 2
                        tot = parts[-1][3] + parts[-1][2]
                        op("act", lambda a: a.activation(out=Pt[sp_][:, :tot], in_=Sp[sp_][:, :tot], func=AF.Exp, scale=0.125),
                           reads=["a_S%d" % sp_], writes=["a_P%d" % sp_])
                        for (j, qs, ncol, off) in parts:
                            if j >= q0b:
                                op("pool", lambda v: v.tensor_tensor(out=Pt[sp_][:, off:off + 128], in0=Pt[sp_][:, off:off + 128], in1=tri_leb[:], op=ALU.mult),
                                   reads=["a_P%d" % sp_, "tri_leb"], writes=["a_P%d" % sp_])
                        for (j, qs, ncol, off) in parts:
                            c0 = (qs - q0b) * 128
                            op("pe", lambda p: p.matmul(Op[oo][:, c0:c0 + ncol], lhsT=Vp[i][:, j, :], rhs=Pt[sp_][:, off:off + ncol],
                                                        start=(j == 0), stop=(j == q1b - 1)),
                               reads=["a_V%d" % i, "a_P%d" % sp_], writes=["a_O%d" % oo])
                        if parts[-1][0] == q1b - 1:
                            nt = nqb * 128
                            op("dve", lambda v: v.tensor_scalar(out=rec[64:128, :nt], in0=Op[oo][64:128, :nt], scalar1=1e-36, scalar2=None, op0=ALU.add),
                               reads=["a_O%d" % oo], writes=["a_rec"])
                            op("dve", lambda v: v.reciprocal(out=rec[64:128, :nt], in_=rec[64:128, :nt]), reads=["a_rec"], writes=["a_rec"])
                            yi = gi % 2
                            op("dve", lambda v: v.tensor_tensor(out=yo[yi][0:64, :nt], in0=Op[oo][0:64, :nt], in1=rec[64:128, :nt], op=ALU.mult),
                               reads=["a_O%d" % oo, "a_rec"], writes=["a_yo%d" % yi])
                            dma("pool", yattT[h // 2, r0:r0 + 64, q0b * 128:q0b * 128 + nt], yo[yi][0:64, :nt], reads=["a_yo%d" % yi], writes=[("yattT", h, gi)])

                    for k in range(min(LOOK, len(steps))):
                        emit_qk(k)
                    for k in range(len(steps)):
                        if k + LOOK < len(steps):
                            emit_qk(k + LOOK)
                        emit_rest(k)
                    sbase[0] += len(steps)
                    obase[0] += len(_blocks(NB, 4))
                cx.barrier()


        def bc(ap, dim, shape):
            return ap.unsqueeze(dim).broadcast_to(list(shape))

        def phase_ssd(l):
            with ExitStack() as s:
                cw = sb("s_cw", [128, 4, 12], F32, s)
                cbias = sb("s_cb", [128, 12], F32, s)
                vec = sb("s_vec", [128, 3, 16], F32, s)
                nw = sb("s_nw", [128, 1024], F32, s)
                dt_all = sb("s_dt", [128, NB, 16], F32, s)
                dtA = sb("s_dtA", [128, NB, 16], F32, s)
                xr = sb("s_xr", [128, 12, 3 + 512], BF16, s)
                acc = sb("s_acc", [128, 512], F32, s)
                xc = sb("s_xc", [128, 12, 512], BF16, s)
                xs_tm_2 = [sb("s_xs_%d" % i_, [128, 1024], BF16, s) for i_ in range(2)]
                Btm_2 = [sb("s_B_%d" % i_, [128, 256], BF16, s) for i_ in range(2)]
                Rt_2 = [sb("s_R_%d" % i_, [128, 16, 128], F32, s) for i_ in range(2)]
                acs_2 = [sb("s_acs_%d" % i_, [128, 32], F32, s) for i_ in range(2)]
                sm_2 = [sb("s_sm_%d" % i_, [128, 4, 16], F32, s) for i_ in range(2)]
                cbm_2 = [sb("s_cbm_%d" % i_, [128, 2, 128], BF16, s) for i_ in range(2)]
                dec = sb("s_dec", [128, 8, 128], BF16, s)
                M_2 = [sb("s_M_%d" % i_, [128, 16, 128], BF16, s) for i_ in range(2)]
                xdt_2 = [sb("s_xdt_%d" % i_, [128, 1024], BF16, s) for i_ in range(2)]
                xdtS_2 = [sb("s_xdtS_%d" % i_, [128, 1024], BF16, s) for i_ in range(2)]
                state = sb("s_state", [128, 1024], F32, s)
                prevb = sb("s_prevb", [128, 1024], BF16, s)
                yoff_2 = [sb("s_yoff_%d" % i_, [128, 1024], F32, s) for i_ in range(2)]
                ysb_2 = [sb("s_y_%d" % i_, [128, 1024], F32, s) for i_ in range(2)]
                tmp_2 = [sb("s_tmp_%d" % i_, [128, 1024], F32, s) for i_ in range(2)]
                ztg = sb("s_ztg", [128, 4, 1024], BF16, s)
                szg_2 = [sb("s_szg_%d" % i_, [128, 4, 1024], F32, s) for i_ in range(2)]
                ss = sb("s_ss", [128, 4], F32, s)
                yn_2 = [sb("s_yn_%d" % i_, [128, 1024], BF16, s) for i_ in range(2)]
                ysT_2 = [sb("s_ysT_%d" % i_, [128, 8, 512], BF16, s) for i_ in range(2)]
                ptr = ps("s_ptr", [128, 1024], BF16, s)
                ptb = ps("s_ptb", [128, 256], BF16, s)
                segp = ps("s_seg", [128, 1024], F32, s)
                Yp = ps("s_Y", [128, 512], F32, s)
                ptry = ps("s_ptry", [128, 1024], BF16, s)
                mA = ps("s_mA", [128, 512], F32, s)
                mB = ps("s_mB", [128, 512], F32, s)
                with nc.allow_non_contiguous_dma(reason="tiny params"):
                    for k in range(4):
                        dma("sp", cw[:, k, :], ssd_conv_w[l, k].rearrange("(c p) -> p c", p=128), writes=["s_cw"])
                    dma("sp", cbias[:], ssd_conv_b[l].rearrange("(c p) -> p c", p=128), writes=["s_cb"])
                    dma("sp", vec[:].rearrange("p a b -> p (a b)"), ssd_vec[l].rearrange("a b -> (a b)").partition_broadcast(128), writes=["s_vec"])
                    dma("sp", nw[:], ssd_norm_w[l].partition_broadcast(128), writes=["s_nw"])
                    dma("sp", dt_all[:], dtr.rearrange("(b p) n -> p b n", p=128), writes=["s_dt"])
                op("act", lambda a: a.activation(out=vec[:, 1, :], in_=vec[:, 1, :], func=AF.Exp), reads=["s_vec"], writes=["s_vec"])
                op("dve", lambda v: v.tensor_scalar(out=vec[:, 1, :], in0=vec[:, 1, :], scalar1=-1.0, scalar2=None, op0=ALU.mult), reads=["s_vec"], writes=["s_vec"])
                op("dve", lambda v: v.tensor_tensor(out=dt_all[:], in0=dt_all[:], in1=bc(vec[:, 0, :], 1, [128, NB, 16]), op=ALU.add), reads=["s_dt", "s_vec"], writes=["s_dt"])
                op("act", lambda a: a.activation(out=dt_all[:], in_=dt_all[:], func=AF.Exp), reads=["s_dt"], writes=["s_dt"])
                op("act", lambda a: a.activation(out=dt_all[:], in_=dt_all[:], func=AF.Ln, bias=ones_f[:, 0:1], scale=1.0), reads=["s_dt", "ones_f"], writes=["s_dt"])
                op("dve", lambda v: v.tensor_scalar(out=dt_all[:, 0, :], in0=dt_all[:, 0, :], scalar1=validcol[:, 0:1], scalar2=None, op0=ALU.mult),
                   reads=["s_dt", "validcol"], writes=["s_dt"])
                op("dve", lambda v: v.tensor_tensor(out=dtA[:], in0=dt_all[:], in1=bc(vec[:, 1, :], 1, [128, NB, 16]), op=ALU.mult), reads=["s_dt", "s_vec"], writes=["s_dtA"])
                op("dve", lambda v: v.memset(state[:], 0.0), writes=["s_state"])
                op("dve", lambda v: v.memset(prevb[:], 0.0), writes=["s_prevb"])

                def ssd_front(b0, bi, gi):
                    b = b0 + bi
                    c0 = bi * 128
                    pb = b % 2
                    xs_tm = xs_tm_2[pb]
                    Btm = Btm_2[pb]
                    Rt = Rt_2[pb]
                    acs = acs_2[pb]
                    cbm = cbm_2[pb]
                    M = M_2[pb]
                    xdt = xdt_2[pb]
                    xdtS = xdtS_2[pb]
                    yoff = yoff_2[pb]
                    ysb = ysb_2[pb]
                    tmp = tmp_2[pb]
                    yn = yn_2[pb]
                    sm = sm_2[pb]
                    for cc in range(8):
                        op("pe", lambda p, cc=cc: p.transpose(out=ptr[:, cc * 128:(cc + 1) * 128], in_=xc[:, cc, c0:c0 + 128], identity=identb[:]),
                           reads=["s_xc", "identb"], writes=["s_ptr"])
                    for g in range(2):
                        op("pe", lambda p, g=g: p.transpose(out=ptb[:, g * 128:(g + 1) * 128], in_=xc[:, 8 + g, c0:c0 + 128], identity=identb[:]),
                           reads=["s_xc", "identb"], writes=["s_ptb"])
                    op("act", lambda a: a.activation(out=xs_tm[:], in_=ptr[:], func=AF.Copy), reads=["s_ptr"], writes=["s_xs_%d" % pb])
                    op("dve", lambda v: v.tensor_copy(out=Btm[:], in_=ptb[:]), reads=["s_ptb"], writes=["s_B_%d" % pb])
                    op("pool", lambda v: v.tensor_tensor(out=Rt[:], in0=bc(tri_le[:], 1, [128, 16, 128]), in1=bc(dtA[:, b, :], 2, [128, 16, 128]), op=ALU.mult),
                       reads=["tri_le", "s_dtA"], writes=["s_R_%d" % pb])
                    op("pe", lambda p: p.matmul(mB[:, 256:272], lhsT=tri_le[:], rhs=dtA[:, b, :], start=True, stop=True), reads=["tri_le", "s_dtA"], writes=["s_mB"])
                    op("pe", lambda p: p.matmul(mB[:, 272:288], lhsT=ones_f[:], rhs=dtA[:, b, :], start=True, stop=True), reads=["ones_f", "s_dtA"], writes=["s_mB"])
                    for g in range(2):
                        op("pe", lambda p, g=g: p.matmul(mB[:, g * 128:(g + 1) * 128], lhsT=xc[:, 8 + g, c0:c0 + 128], rhs=xc[:, 10 + g, c0:c0 + 128], start=True, stop=True),
                           reads=["s_xc"], writes=["s_mB"])
                    op("dve", lambda v: v.tensor_copy(out=acs[:], in_=mB[:, 256:288]), reads=["s_mB"], writes=["s_acs_%d" % pb])
                    op("dve", lambda v: v.tensor_tensor(out=cbm[:], in0=mB[:, 0:256].rearrange("p (g l) -> p g l", g=2), in1=bc(tri_le[:], 1, [128, 2, 128]), op=ALU.mult),
                       reads=["s_mB", "tri_le"], writes=["s_cbm_%d" % pb])
                    op("act", lambda a: a.activation(out=sm[:, 0, :], in_=acs[:, 0:16], func=AF.Exp), reads=["s_acs_%d" % pb], writes=["s_sm0_%d" % pb])
                    op("dve", lambda v: v.tensor_tensor(out=sm[:, 3, :], in0=acs[:, 16:32], in1=acs[:, 0:16], op=ALU.subtract), reads=["s_acs_%d" % pb], writes=["s_sm3_%d" % pb])
                    op("act", lambda a: a.activation(out=sm[:, 1, :], in_=sm[:, 3, :], func=AF.Exp), reads=["s_sm3_%d" % pb], writes=["s_sm1_%d" % pb])
                    op("act", lambda a: a.activation(out=sm[:, 2, :], in_=acs[:, 16:32], func=AF.Exp), reads=["s_acs_%d" % pb], writes=["s_sm2_%d" % pb])
                    for hf in range(2):
                        for q in range(2):
                            h0_ = hf * 8 + q * 4
                            op("pe", lambda p, q=q, h0_=h0_: p.matmul(segp[:, q * 512:(q + 1) * 512], lhsT=smask[:], rhs=Rt[:, h0_:h0_ + 4, :].rearrange("p h l -> p (h l)"),
                                                                       start=True, stop=True), reads=["smask", "s_R_%d" % pb], writes=["s_seg"])
                        op("act", lambda a: a.activation(out=dec[:].rearrange("p h l -> p (h l)"), in_=segp[:], func=AF.Exp), reads=["s_seg"], writes=["s_dec"])
                        op("dve", lambda v, hf=hf: v.tensor_tensor(out=M[:, hf * 8:(hf + 1) * 8, :], in0=dec[:], in1=bc(cbm[:, hf, :], 1, [128, 8, 128]), op=ALU.mult),
                           reads=["s_dec", "s_cbm_%d" % pb], writes=["s_M_%d" % pb])
                    op("dve", lambda v: v.tensor_tensor(out=xdt[:].rearrange("p (h d) -> p h d", d=64), in0=xs_tm[:].rearrange("p (h d) -> p h d", d=64),
                                                        in1=bc(dt_all[:, b, :], 2, [128, 16, 64]), op=ALU.mult), reads=["s_xs_%d" % pb, "s_dt"], writes=["s_xdt_%d" % pb])
                    op("dve", lambda v: v.tensor_tensor(out=xdtS[:].rearrange("p (h d) -> p h d", d=64), in0=xdt[:].rearrange("p (h d) -> p h d", d=64),
                                                        in1=bc(sm[:, 1, :], 2, [128, 16, 64]), op=ALU.mult), reads=["s_xdt_%d" % pb, "s_sm1_%d" % pb], writes=["s_xdtS_%d" % pb])
                    for g in range(2):
                        op("pe", lambda p, g=g: p.matmul(mA[:, :], lhsT=xc[:, 10 + g, c0:c0 + 128], rhs=prevb[:, g * 512:(g + 1) * 512], start=True, stop=True),
                           reads=["s_xc", "s_prevb"], writes=["s_mA"])
                        op("dve", lambda v, g=g: v.tensor_tensor(out=yoff[:, g * 512:(g + 1) * 512].rearrange("p (h d) -> p h d", d=64), in0=mA[:, :].rearrange("p (h d) -> p h d", d=64),
                                                                 in1=bc(sm[:, 0, g * 8:(g + 1) * 8], 2, [128, 8, 64]), op=ALU.mult), reads=["s_mA", "s_sm0_%d" % pb], writes=["s_yoff_%d" % pb])
                    for g in range(2):
                        for hh in range(8 * g, 8 * g + 8):
                            op("pe", lambda p, hh=hh, g=g: p.matmul(Yp[:, (hh - 8 * g) * 64:(hh - 8 * g + 1) * 64], lhsT=M[:, hh, :], rhs=xdt[:, hh * 64:(hh + 1) * 64], start=True, stop=True),
                               reads=["s_M_%d" % pb, "s_xdt_%d" % pb], writes=["s_Y"])
                        op("dve", lambda v, g=g: v.tensor_tensor(out=ysb[:, g * 512:(g + 1) * 512], in0=Yp[:, :], in1=yoff[:, g * 512:(g + 1) * 512], op=ALU.add),
                           reads=["s_Y", "s_yoff_%d" % pb], writes=["s_y_%d" % pb])
                    for g in range(2):
                        op("pe", lambda p, g=g: p.matmul(mA[:, :], lhsT=Btm[:, g * 128:(g + 1) * 128], rhs=xdtS[:, g * 512:(g + 1) * 512], start=True, stop=True),
                           reads=["s_B_%d" % pb, "s_xdtS_%d" % pb], writes=["s_mA"])
                        op("pool", lambda v, g=g: v.tensor_tensor(out=state[:, g * 512:(g + 1) * 512].rearrange("p (h d) -> p h d", d=64),
                                                                 in0=state[:, g * 512:(g + 1) * 512].rearrange("p (h d) -> p h d", d=64),
                                                                 in1=bc(sm[:, 2, g * 8:(g + 1) * 8], 2, [128, 8, 64]), op=ALU.mult), reads=["s_state", "s_sm2_%d" % pb], writes=["s_state"])
                        op("dve", lambda v, g=g: v.tensor_tensor(out=state[:, g * 512:(g + 1) * 512], in0=state[:, g * 512:(g + 1) * 512], in1=mA[:, :], op=ALU.add),
                           reads=["s_state", "s_mA"], writes=["s_state"])
                    op("act", lambda a: a.activation(out=prevb[:], in_=state[:], func=AF.Copy), reads=["s_state"], writes=["s_prevb"])

                def ssd_tail(b0, bi, gi):
                    b = b0 + bi
                    c0 = bi * 128
                    pb = b % 2
                    xs_tm = xs_tm_2[pb]
                    Btm = Btm_2[pb]
                    Rt = Rt_2[pb]
                    acs = acs_2[pb]
                    cbm = cbm_2[pb]
                    M = M_2[pb]
                    xdt = xdt_2[pb]
                    xdtS = xdtS_2[pb]
                    yoff = yoff_2[pb]
                    ysb = ysb_2[pb]
                    tmp = tmp_2[pb]
                    yn = yn_2[pb]
                    sm = sm_2[pb]
                    op("pool", lambda v: v.tensor_tensor(out=tmp[:].rearrange("p (h d) -> p h d", d=64), in0=xs_tm[:].rearrange("p (h d) -> p h d", d=64),
                                                        in1=bc(vec[:, 2, :], 2, [128, 16, 64]), op=ALU.mult), reads=["s_xs_%d" % pb, "s_vec"], writes=["s_tmp_%d" % pb])
                    op("dve", lambda v: v.tensor_tensor(out=ysb[:], in0=ysb[:], in1=tmp[:], op=ALU.add), reads=["s_y_%d" % pb, "s_tmp_%d" % pb], writes=["s_y_%d" % pb])
                    op("dve", lambda v: v.tensor_tensor(out=ysb[:], in0=ysb[:], in1=szg_2[gi % 2][:, bi, :], op=ALU.mult), reads=["s_y_%d" % pb, "s_szg_%d" % (gi % 2)], writes=["s_y_%d" % pb])
                    op("act", lambda a: a.activation(out=tmp[:], in_=ysb[:], func=AF.Square), reads=["s_y_%d" % pb], writes=["s_tmp_%d" % pb])
                    op("dve", lambda v: v.tensor_reduce(out=ss[:, 0:2], in_=tmp[:].rearrange("p (g d) -> p g d", g=2), axis=AX.X, op=ALU.add), reads=["s_tmp_%d" % pb], writes=["s_ss"])
                    op("act", lambda a: a.activation(out=ss[:, 2:4], in_=ss[:, 0:2], func=AF.Ln, scale=1.0 / 512, bias=RMS_EPS_T[:]), reads=["s_ss", "eps"], writes=["s_ss"])
                    op("act", lambda a: a.activation(out=ss[:, 2:4], in_=ss[:, 2:4], func=AF.Exp, scale=-0.5), reads=["s_ss"], writes=["s_ss"])
                    for g in range(2):
                        op("dve", lambda v, g=g: v.scalar_tensor_tensor(out=yn[:, g * 512:(g + 1) * 512], in0=ysb[:, g * 512:(g + 1) * 512], scalar=ss[:, 2 + g:3 + g],
                                                                        in1=nw[:, g * 512:(g + 1) * 512], op0=ALU.mult, op1=ALU.mult),
                           reads=["s_y_%d" % pb, "s_ss", "s_nw"], writes=["s_yn_%d" % pb])
                    for cc in range(8):
                        op("pe", lambda p, cc=cc: p.transpose(out=ptry[:, cc * 128:(cc + 1) * 128], in_=yn[:, cc * 128:(cc + 1) * 128], identity=identb[:]),
                           reads=["s_yn_%d" % pb, "identb"], writes=["s_ptry"])
                    op("act", lambda a: a.activation(out=ysT_2[gi % 2][:, :, c0:c0 + 128], in_=ptry[:].rearrange("p (c t) -> p c t", c=8), func=AF.Copy), reads=["s_ptry"], writes=["s_ysT_%d" % (gi % 2)])


                pending = []

                def flush_tail():
                    if not pending:
                        return
                    pb0, pbi, pgi, pnb, pt0, pnt = pending.pop()
                    ssd_tail(pb0, pbi, pgi)
                    if pbi == pnb - 1:
                        dma("pool", yssdT[:, :, pt0:pt0 + pnt].rearrange("c p t -> p c t"), ysT_2[pgi % 2][:, :, :pnt], reads=["s_ysT_%d" % (pgi % 2)], writes=[("yssdT", pgi)])

                for gi, (b0, nb) in enumerate(_blocks(NB, 4)):
                    t0, nt = b0 * 128, nb * 128
                    dma("sp", xr[:, :, 0:3 + nt], xbcT[:, :, 128 + t0 - 3:128 + t0 + nt].rearrange("c p t -> p c t"), writes=["s_xr"])
                    for cc in range(12):
                        op("dve", lambda v, cc=cc: v.tensor_scalar(out=acc[:, :nt], in0=xr[:, cc, 3:3 + nt], scalar1=cw[:, 3, cc:cc + 1], scalar2=None, op0=ALU.mult),
                           reads=["s_xr", "s_cw"], writes=["s_acc"])
                        for k in range(3):
                            op("dve", lambda v, cc=cc, k=k: v.scalar_tensor_tensor(out=acc[:, :nt], in0=xr[:, cc, k:k + nt], scalar=cw[:, k, cc:cc + 1], in1=acc[:, :nt],
                                                                                   op0=ALU.mult, op1=ALU.add), reads=["s_xr", "s_cw", "s_acc"], writes=["s_acc"])
                        op("act", lambda a, cc=cc: a.activation(out=xc[:, cc, :nt], in_=acc[:, :nt], func=AF.Silu, bias=cbias[:, cc:cc + 1], scale=1.0),
                           reads=["s_acc", "s_cb"], writes=["s_xc"])
                    dma("sp", ztg[:, :nb, :], ztm[t0:t0 + nt, :].rearrange("(b p) n -> p b n", p=128), writes=["s_ztg"])
                    op("act", lambda a: a.activation(out=szg_2[gi % 2][:, :nb, :], in_=ztg[:, :nb, :], func=AF.Silu), reads=["s_ztg"], writes=["s_szg_%d" % (gi % 2)])
                    for bi in range(nb):
                        lf = record(ssd_front, b0, bi, gi)
                        lt_ = record(flush_tail)
                        replay_interleaved(lf, lt_)
                        pending.append((b0, bi, gi, nb, t0, nt))
                flush_tail()
                cx.barrier()


        def ln_fm(s_tiles, r, rkey, dst, dkey, gb, gbkey, nt, zero_pads, extra_reads=()):
            xr_ = list(extra_reads)
            sq, st1, st2, mean, rstd, lps, lpk = s_tiles
            for c in range(KC):
                op("act", lambda a, c=c: a.activation(out=sq[:, :nt], in_=r[:, c, :nt], func=AF.Square), reads=[rkey] + xr_, writes=["ln_sq"])
                op("pe", lambda p, c=c: p.matmul(lps[0][:, :nt], lhsT=ones_f[:], rhs=r[:, c, :nt], start=(c == 0), stop=(c == KC - 1)), reads=["ones_f", rkey] + xr_, writes=[lpk[0]])
                op("pe", lambda p, c=c: p.matmul(lps[1][:, :nt], lhsT=ones_f[:], rhs=sq[:, :nt], start=(c == 0), stop=(c == KC - 1)), reads=["ones_f", "ln_sq"], writes=[lpk[1]])
            op("act", lambda a: a.activation(out=mean[:, :nt], in_=lps[0][:, :nt], func=AF.Copy, scale=1.0 / D), reads=[lpk[0]], writes=["ln_mean"])
            op("dve", lambda v: v.tensor_tensor(out=st1[:, :nt], in0=mean[:, :nt], in1=mean[:, :nt], op=ALU.mult), reads=["ln_mean"], writes=["ln_st1"])
            op("dve", lambda v: v.scalar_tensor_tensor(out=st2[:, :nt], in0=lps[1][:, :nt], scalar=1.0 / D, in1=st1[:, :nt], op0=ALU.mult, op1=ALU.subtract),
               reads=[lpk[1], "ln_st1"], writes=["ln_st2"])
            op("act", lambda a: a.activation(out=st2[:, :nt], in_=st2[:, :nt], func=AF.Sqrt, bias=LN_EPS_T[:], scale=1.0), reads=["ln_st2", "eps"], writes=["ln_st2"])
            op("dve", lambda v: v.reciprocal(out=rstd[:, :nt], in_=st2[:, :nt]), reads=["ln_st2"], writes=["ln_rstd"])
            for c in range(KC):
                op("dve", lambda v, c=c: v.tensor_tensor(out=sq[:, :nt], in0=r[:, c, :nt], in1=mean[:, :nt], op=ALU.subtract), reads=[rkey, "ln_mean"] + xr_, writes=["ln_sq"])
                op("dve", lambda v, c=c: v.tensor_tensor(out=sq[:, :nt], in0=sq[:, :nt], in1=rstd[:, :nt], op=ALU.mult), reads=["ln_sq", "ln_rstd"], writes=["ln_sq"])
                op("act", lambda a, c=c: a.activation(out=dst[:, c, :nt], in_=sq[:, :nt], func=AF.Identity, scale=gb[:, 0, c:c + 1], bias=gb[:, 1, c:c + 1]),
                   reads=["ln_sq", gbkey], writes=[dkey])
            if zero_pads:
                op("dve", lambda v: v.memset(dst[:, :, 0:112], 0.0), writes=[dkey])

        def ln_tiles(s, n, lps=None, lpk=None):
            if lps is None:
                lps = [ps("ln_ps%d" % i, [128, n], F32, s) for i in range(2)]
                lpk = ["ln_ps0", "ln_ps1"]
            return (sb("ln_sq", [128, n], F32, s), sb("ln_st1", [128, n], F32, s), sb("ln_st2", [128, n], F32, s), sb("ln_mean", [128, n], F32, s),
                    sb("ln_rstd", [128, n], F32, s), lps, lpk)

        def load_gb(s, src, name):
            t = sb(name, [128, 2, KC], F32, s)
            with nc.allow_non_contiguous_dma(reason="tiny param"):
                dma("sp", t[:], src.rearrange("t (c p) -> p t c", p=128), writes=[name])
            return t

        def phase_merge(l, h_in, h_out):
            with ExitStack() as s:
                Wp = sb("m_Wp", [128, 16, D], BF16, s)
                Wo = sb("m_Wo", [128, 8, D], BF16, s)
                with ExitStack() as s2:
                    wstg = mk_wstg(s2)
                    load_w(Wp, w_proj[l], D, "m_Wp", 16, wstg)
                    load_w(Wo, w_out[l], D, "m_Wo", 8, wstg)
                    cx.barrier()
                gb = load_gb(s, ln_mix_gb[l], "m_gb")
                scw = sb("m_scw", [128, 3, 4], F32, s)
                with nc.allow_non_contiguous_dma(reason="tiny param"):
                    for k in range(3):
                        dma("sp", scw[:, k, :], sc_conv_w[l, k].rearrange("(c p) -> p c", p=128), writes=["m_scw"])
                NT = 512
                sct = sb("m_sct", [128, 12, 2 + NT], BF16, s)
                gt = sb("m_gt", [128, 24, NT], BF16, s)
                ya = sb("m_ya", [128, 4, NT], BF16, s)
                ys = sb("m_ys", [128, 8, NT], BF16, s)
                hf2 = [sb("m_h%d" % i, [128, KC, NT], F32, s) for i in range(2)]
                u = sb("m_u", [128, 2 + NT], F32, s)
                acc = sb("m_acc", [128, NT], F32, s)
                yc = sb("m_yc", [128, 4, NT], BF16, s)
                sg2 = [sb("m_sg%d" % i, [128, 3, NT], F32, s) for i in range(2)]
                mt2 = [sb("m_mt%d" % i, [128, 3, NT], F32, s) for i in range(2)]
                mg2 = [sb("m_mg%d" % i, [128, 8, NT], BF16, s) for i in range(2)]
                r = sb("m_r", [128, KC, NT], F32, s)
                pp2 = [[ps("m_p%d_%d" % (i, j), [128, NT], F32, s) for i in range(3)] for j in range(2)]
                po2 = [ps("m_po%d" % i, [128, NT], F32, s) for i in range(2)]
                lt = ln_tiles(s, NT, lps=[po2[0], po2[1]], lpk=["m_po0", "m_po1"])
                groups = _blocks(NB, 4)

                def stage_a(gi):
                    b0, nb = groups[gi]
                    t0, nt = b0 * 128, nb * 128
                    q = gi % 2
                    hf, kh = hf2[q], "m_h%d" % q
                    mg, kmg = mg2[q], "m_mg%d" % q
                    dma("sp", sct[:, :, 0:2 + nt], scT[:, :, 128 + t0 - 2:128 + t0 + nt].rearrange("c p t -> p c t"), writes=["m_sct"])
                    dma("sp", gt[:, :, :nt], gT[:, :, t0:t0 + nt].rearrange("c p t -> p c t"), writes=["m_gt"])
                    dma("sp", ya[:, :, :nt], yattT[:, :, t0:t0 + nt].rearrange("c p t -> p c t"), writes=["m_ya"])
                    dma("sp", ys[:, :, :nt], yssdT[:, :, t0:t0 + nt].rearrange("c p t -> p c t"), writes=["m_ys"])
                    dma("sp", hf[:, :, :nt], h_in[:, :, t0:t0 + nt].rearrange("c p t -> p c t"), writes=[kh])

                    def conv_cc(cc):
                        op("dve", lambda v: v.tensor_tensor(out=u[:, :2 + nt], in0=sct[:, 4 + cc, :2 + nt], in1=sct[:, 8 + cc, :2 + nt], op=ALU.mult), reads=["m_sct"], writes=["m_u"])
                        op("dve", lambda v: v.tensor_scalar(out=acc[:, :nt], in0=u[:, 2:2 + nt], scalar1=scw[:, 2, cc:cc + 1], scalar2=None, op0=ALU.mult),
                           reads=["m_u", "m_scw"], writes=["m_acc"])
                        for k in range(2):
                            op("dve", lambda v, k=k: v.scalar_tensor_tensor(out=acc[:, :nt], in0=u[:, k:k + nt], scalar=scw[:, k, cc:cc + 1], in1=acc[:, :nt], op0=ALU.mult, op1=ALU.add),
                               reads=["m_u", "m_scw", "m_acc"], writes=["m_acc"])
                        op("dve", lambda v: v.tensor_tensor(out=yc[:, cc, :nt], in0=acc[:, :nt], in1=sct[:, cc, 2:2 + nt], op=ALU.mult), reads=["m_acc", "m_sct"], writes=["m_yc"])

                    for cc in range(4):
                        conv_cc(cc)

                    def merge_n(n):
                        pn = n % 2
                        pp, sg, mt = pp2[pn], sg2[pn], mt2[pn]
                        branches = [(ya, "m_ya", 0, 4), (ys, "m_ys", 4, 8), (yc, "m_yc", 12, 4)]
                        for bi_, (src_t, skey, w0, nk) in enumerate(branches):
                            for kc in range(nk):
                                op("pe", lambda p, bi_=bi_, src_t=src_t, w0=w0, kc=kc, nk=nk: p.matmul(pp[bi_][:, :nt], lhsT=Wp[:, w0 + kc, n * 128:(n + 1) * 128], rhs=src_t[:, kc, :nt],
                                                                                                     start=(kc == 0), stop=(kc == nk - 1)),
                                   reads=["m_Wp", skey], writes=["m_p%d_%d" % (bi_, pn)])
                        for j in range(3):
                            op("act", lambda a, j=j: a.activation(out=sg[:, j, :nt], in_=gt[:, j * 8 + n, :nt], func=AF.Sigmoid), reads=["m_gt"], writes=[("m_sg", pn, j)])
                        for j in range(3):
                            op("dve", lambda v, j=j: v.tensor_tensor(out=mt[:, j, :nt], in0=pp[j][:, :nt], in1=sg[:, j, :nt], op=ALU.mult),
                               reads=["m_p%d_%d" % (j, pn), ("m_sg", pn, j)], writes=[("m_mt", pn, j)])
                        op("pool", lambda v: v.tensor_tensor(out=mt[:, 0, :nt], in0=mt[:, 0, :nt], in1=mt[:, 1, :nt], op=ALU.add),
                           reads=[("m_mt", pn, 0), ("m_mt", pn, 1)], writes=[("m_mt", pn, 0)])
                        op("pool", lambda v: v.tensor_tensor(out=mg[:, n, :nt], in0=mt[:, 0, :nt], in1=mt[:, 2, :nt], op=ALU.add),
                           reads=[("m_mt", pn, 0), ("m_mt", pn, 2)], writes=[(kmg, n)])

                    for n in range(8):
                        merge_n(n)

                def stage_b(gi):
                    b0, nb = groups[gi]
                    t0, nt = b0 * 128, nb * 128
                    q = gi % 2
                    hf, kh = hf2[q], "m_h%d" % q
                    mg, kmg = mg2[q], "m_mg%d" % q

                    def out_n(n):
                        po, pok_ = po2[n % 2], "m_po%d" % (n % 2)
                        for kc in range(8):
                            op("pe", lambda p, kc=kc: p.matmul(po[:, :nt], lhsT=Wo[:, kc, n * 128:(n + 1) * 128], rhs=mg[:, kc, :nt], start=(kc == 0), stop=(kc == 7)),
                               reads=["m_Wo", (kmg, kc)], writes=[pok_])
                        op("dve", lambda v: v.scalar_tensor_tensor(out=r[:, n, :nt], in0=hf[:, n, :nt], scalar=ALPHA, in1=po[:, :nt], op0=ALU.mult, op1=ALU.add),
                           reads=[kh, pok_], writes=["m_r"])

                    for n in range(8):
                        out_n(n)
                    ln_fm(lt, r, "m_r", hf, kh, gb, "m_gb", nt, zero_pads=(b0 == 0))
                    dma("pool", h_out[:, :, t0:t0 + nt].rearrange("c p t -> p c t"), hf[:, :, :nt], reads=[kh], writes=[("hT", gi)])

                for it in record(stage_a, 0):
                    replay(it)
                for gi in range(1, len(groups)):
                    la = record(stage_a, gi)
                    lb = record(stage_b, gi - 1)
                    replay_interleaved(la, lb)
                for it in record(stage_b, len(groups) - 1):
                    replay(it)
                cx.barrier()

        def phase_ffn_dense(l, h_in, h_out):
            with ExitStack() as s:
                NFF = FF_DENSE // 128
                Wg = sb("f_Wg", [128, KC, 2 * FF_DENSE], BF16, s)
                Wd = sb("f_Wd", [128, NFF, D], BF16, s)
                with ExitStack() as s2:
                    wstg = mk_wstg(s2)
                    load_w(Wg, dense_w_gu, 2 * FF_DENSE, "f_Wg", KC, wstg)
                    load_w(Wd, dense_w_down, D, "f_Wd", NFF, wstg)
                    cx.barrier()
                gb = load_gb(s, ln_ffn_gb[l], "f_gb")
                NT = 256
                hf2 = [sb("f_h%d" % i, [128, KC, NT], F32, s) for i in range(2)]
                hb2 = [sb("f_hb%d" % i, [128, KC, NT], BF16, s) for i in range(2)]
                act2 = [sb("f_act%d" % i, [128, NFF, NT], BF16, s) for i in range(2)]
                sgl2 = [sb("f_sgl%d" % i, [128, NT], F32, s) for i in range(2)]
                r = sb("f_r", [128, KC, NT], F32, s)
                lt = ln_tiles(s, NT)
                pg = [ps("f_pg%d" % i, [128, NT], F32, s) for i in range(2)]
                pu = [ps("f_pu%d" % i, [128, NT], F32, s) for i in range(2)]
                po2 = [ps("f_po%d" % i, [128, NT], F32, s) for i in range(2)]
                groups = _blocks(NB, 2)

                def load_cast(gi):
                    b0, nb = groups[gi]
                    t0, nt = b0 * 128, nb * 128
                    q = gi % 2
                    dma("sp", hf2[q][:, :, :nt], h_in[:, :, t0:t0 + nt].rearrange("c p t -> p c t"), writes=["f_h%d" % q])
                    for c in range(KC):
                        evac(c, hb2[q][:, c, :nt], hf2[q][:, c, :nt], ["f_h%d" % q], ["f_hb%d" % q])

                load_cast(0)
                for gi, (b0, nb) in enumerate(groups):
                    t0, nt = b0 * 128, nb * 128
                    q = gi % 2
                    hf, hb, act = hf2[q], hb2[q], act2[q]
                    kh, khb, kact = "f_h%d" % q, "f_hb%d" % q, "f_act%d" % q
                    for f in range(NFF):
                        i = f % 2
                        sgl = sgl2[i]
                        for c in range(KC):
                            op("pe", lambda p, c=c: p.matmul(pg[i][:, :nt], lhsT=Wg[:, c, f * 128:(f + 1) * 128], rhs=hb[:, c, :nt], start=(c == 0), stop=(c == KC - 1)),
                               reads=["f_Wg", khb], writes=["f_pg%d" % i])
                        for c in range(KC):
                            op("pe", lambda p, c=c: p.matmul(pu[i][:, :nt], lhsT=Wg[:, c, FF_DENSE + f * 128:FF_DENSE + (f + 1) * 128], rhs=hb[:, c, :nt], start=(c == 0), stop=(c == KC - 1)),
                               reads=["f_Wg", khb], writes=["f_pu%d" % i])
                        op("act", lambda a: a.activation(out=sgl[:, :nt], in_=pg[i][:, :nt], func=AF.Silu), reads=["f_pg%d" % i], writes=["f_sgl%d" % i])
                        op("dve", lambda v: v.tensor_tensor(out=act[:, f, :nt], in0=pu[i][:, :nt], in1=sgl[:, :nt], op=ALU.mult), reads=["f_pu%d" % i, "f_sgl%d" % i], writes=[(kact, f)])
                    if gi + 1 < len(groups):
                        load_cast(gi + 1)
                    for n in range(8):
                        po, kpo = po2[n % 2], "f_po%d" % (n % 2)
                        for f in range(NFF):
                            op("pe", lambda p, f=f: p.matmul(po[:, :nt], lhsT=Wd[:, f, n * 128:(n + 1) * 128], rhs=act[:, f, :nt], start=(f == 0), stop=(f == NFF - 1)),
                               reads=["f_Wd", (kact, f)], writes=[kpo])
                        op("dve", lambda v: v.scalar_tensor_tensor(out=r[:, n, :nt], in0=hf[:, n, :nt], scalar=ALPHA, in1=po[:, :nt], op0=ALU.mult, op1=ALU.add),
                           reads=[kh, kpo], writes=["f_r"])
                    ln_fm(lt, r, "f_r", hf, kh, gb, "f_gb", nt, zero_pads=(b0 == 0))
                    dma("pool", h_out[:, :, t0:t0 + nt].rearrange("c p t -> p c t"), hf[:, :, :nt], reads=[kh], writes=[("hT", gi)])
                cx.barrier()

        def phase_moe(l, h_in):
            with ExitStack() as s:
                NQF = 4
                SG = 1024 if TH >= 1024 else 256
                Wg = [sb("e_Wg%d" % i, [128, KC, 2, NQF * 128], BF16, s) for i in range(2)]
                Wd = [sb("e_Wd%d" % i, [128, NQF, D], BF16, s) for i in range(2)]
                gb = load_gb(s, ln_ffn_gb[l], "e_gb")
                wr = sb("e_wr", [128, KC, NEXP], F32, s)
                rb = sb("e_rb", [128, NEXP], F32, s)
                sel = sb("e_sel", [8, NEXP, 128], F32, s)
                with nc.allow_non_contiguous_dma(reason="tiny param"):
                    dma("sp", wr[:], router_w.rearrange("(c p) e -> p c e", p=128), writes=["e_wr"])
                    dma("sp", rb[:], router_b[0].partition_broadcast(128), writes=["e_rb"])
                op("pool", lambda g: g.memset(sel[:], 1.0), writes=["e_sel"])
                op("pool", lambda g: g.affine_select(out=sel[:], in_=sel[:], pattern=[[-1, NEXP], [0, 128]], base=0, channel_multiplier=1,
                                                      compare_op=ALU.is_equal, fill=0.0), reads=["e_sel"], writes=["e_sel"])
                hs = sb("e_hs", [128, KC, SG], F32, s)
                hb = sb("e_hb", [128, KC, SG], BF16, s)
                h2 = sb("e_h2", [128, KC, 256], F32, s)
                accm = sb("e_acc", [128, KC, SG], F32, s)
                cbc = sb("e_cbc", [128, NEXP, SG], BF16, s)
                lg = sb("e_lg", [128, 4, NEXP], F32, s)
                combT = sb("e_combT", [8, SG], F32, s)
                actb = [sb("e_act%d" % i, [128, NQF, 512], BF16, s) for i in range(2)]
                abi = [0]
                sgl2 = [sb("e_sgl%d" % i, [128, 512], F32, s) for i in range(2)]
                a12 = [sb("e_a1%d" % i, [128, 512], F32, s) for i in range(2)]
                osb = sb("e_osb", [128, D], F32, s)
                lt = ln_tiles(s, 256)
                pg = [ps("e_pg%d" % i, [128, 512], F32, s) for i in range(2)]
                pu = [ps("e_pu%d" % i, [128, 512], F32, s) for i in range(2)]
                po = ps("e_po", [128, 512], F32, s)
                po2 = [po, ps("e_po2", [128, 512], F32, s)]
                pok = ["e_po", "e_po2"]
                wic = [0]
                stg = [sb("e_stg%d" % i, [128, 4, NQF * 128], F32, s) for i in range(2)]
                stgi = [0]
                def moe_prologue(sg0):
                    for t1 in range(0, SG, 256):
                        for half in range(2):
                            ta = 128 + half * TH + sg0 + t1
                            gv = stg[half][:].rearrange("p c n -> p (c n)").rearrange("p (c n) -> p c n", c=KC)
                            dma("sp", gv, h_in[:, :, ta:ta + 256].rearrange("c p t -> p c t"), writes=["e_stg%d" % half])
                            if half == 0:
                                op("dve", lambda v, t1=t1, gv=gv: v.tensor_scalar(out=hs[:, :, t1:t1 + 256], in0=gv, scalar1=hsel[:, 0:1], scalar2=None, op0=ALU.mult),
                                   reads=["e_stg0", "hsel"], writes=["e_hs"])
                            else:
                                op("dve", lambda v, t1=t1, gv=gv: v.scalar_tensor_tensor(out=hs[:, :, t1:t1 + 256], in0=gv, scalar=hsel[:, 1:2], in1=hs[:, :, t1:t1 + 256], op0=ALU.mult, op1=ALU.add),
                                   reads=["e_stg1", "hsel", "e_hs"], writes=["e_hs"])
                    for c in range(KC):
                        evac(c, hb[:, c, :], hs[:, c, :], ["e_hs"], ["e_hb"])
                    for tb in range(SG // 128):
                        for c in range(KC):
                            op("pe", lambda p, c=c, tb=tb: p.matmul(po[:, 0:NEXP], lhsT=hs[:, c, tb * 128:(tb + 1) * 128], rhs=wr[:, c, :], start=(c == 0), stop=(c == KC - 1)),
                               reads=["e_hs", "e_wr"], writes=["e_po"])
                        op("dve", lambda v: v.tensor_tensor(out=lg[:, 0, :], in0=po[:, 0:NEXP], in1=rb[:], op=ALU.add), reads=["e_po", "e_rb"], writes=["e_lg"])
                        op("dve", lambda v: v.max(out=lg[:, 1, :], in_=lg[:, 0, :]), reads=["e_lg"], writes=["e_lg"])
                        op("dve", lambda v: v.tensor_scalar(out=lg[:, 2, :], in0=lg[:, 0, :], scalar1=lg[:, 1, 1:2], scalar2=None, op0=ALU.is_ge), reads=["e_lg"], writes=["e_lg"])
                        op("dve", lambda v: v.tensor_scalar(out=lg[:, 3, 0:1], in0=lg[:, 1, 0:1], scalar1=-1.0, scalar2=None, op0=ALU.mult), reads=["e_lg"], writes=["e_lg"])
                        op("act", lambda a: a.activation(out=lg[:, 0, :], in_=lg[:, 0, :], func=AF.Exp, bias=lg[:, 3, 0:1], scale=1.0), reads=["e_lg"], writes=["e_lg"])
                        op("dve", lambda v: v.tensor_tensor(out=lg[:, 0, :], in0=lg[:, 0, :], in1=lg[:, 2, :], op=ALU.mult), reads=["e_lg"], writes=["e_lg"])
                        op("dve", lambda v: v.tensor_reduce(out=lg[:, 3, 1:2], in_=lg[:, 0, :], axis=AX.X, op=ALU.add), reads=["e_lg"], writes=["e_lg"])
                        op("dve", lambda v: v.reciprocal(out=lg[:, 3, 1:2], in_=lg[:, 3, 1:2]), reads=["e_lg"], writes=["e_lg"])
                        op("dve", lambda v: v.tensor_scalar(out=lg[:, 0, :], in0=lg[:, 0, :], scalar1=lg[:, 3, 1:2], scalar2=None, op0=ALU.mult), reads=["e_lg"], writes=["e_lg"])
                        op("pe", lambda p: p.transpose(out=po[0:8, 128:256], in_=lg[:, 0, :], identity=ident[:]), reads=["e_lg", "ident"], writes=["e_po"])
                        op("dve", lambda v, tb=tb: v.tensor_copy(out=combT[:, tb * 128:(tb + 1) * 128], in_=po[0:8, 128:256]), reads=["e_po"], writes=["e_combT"])
                    for e in range(NEXP):
                        for t1 in range(0, SG, 512):
                            n1 = min(512, SG - t1)
                            op("pe", lambda p, e=e, t1=t1, n1=n1: p.matmul(po[:, :n1], lhsT=sel[:, e, :], rhs=combT[:, t1:t1 + n1], start=True, stop=True),
                               reads=["e_sel", "e_combT"], writes=["e_po"])
                            op("act", lambda a, e=e, t1=t1, n1=n1: a.activation(out=cbc[:, e, t1:t1 + n1], in_=po[:, :n1], func=AF.Copy), reads=["e_po"], writes=["e_cbc"])
                def moe_experts(sg0):
                    for e in range(NEXP):
                        for qq in range(FF_EXP // 128 // NQF):
                            w = wic[0] % 2
                            wic[0] += 1
                            f0 = qq * NQF * 128
                            for gu in range(2):
                                srcw = moe_w_gu[e][:, gu * FF_EXP + f0:gu * FF_EXP + f0 + NQF * 128].rearrange("(c p) n -> p c n", p=128)
                                for hc in range(2):
                                    sgi = stgi[0] % 2
                                    stgi[0] += 1
                                    dma("sp", stg[sgi][:], srcw[:, hc * 4:(hc + 1) * 4, :], writes=["e_stg%d" % sgi])
                                    op("act", lambda a, gu=gu, sgi=sgi, hc=hc: a.activation(out=Wg[w][:, hc * 4:(hc + 1) * 4, gu, :], in_=stg[sgi][:], func=AF.Copy),
                                       reads=["e_stg%d" % sgi], writes=[("e_Wg%d" % w, gu, hc)])
                            srcw = moe_w_down[e][f0:f0 + NQF * 128, :].rearrange("(c p) n -> p c n", p=128)
                            for hc in range(2):
                                sgi = stgi[0] % 2
                                stgi[0] += 1
                                stv = stg[sgi][:].rearrange("p c n -> p (c n)").rearrange("p (c n) -> p c n", c=2)
                                dma("sp", stv, srcw[:, hc * 2:(hc + 1) * 2, :], writes=["e_stg%d" % sgi])
                                op("act", lambda a, sgi=sgi, hc=hc, stv=stv: a.activation(out=Wd[w][:, hc * 2:(hc + 1) * 2, :], in_=stv, func=AF.Copy),
                                   reads=["e_stg%d" % sgi], writes=[("e_Wd%d" % w, hc)])
                            for t1 in range(0, SG, 512):
                                n1 = min(512, SG - t1)
                                ab = abi[0] % 2
                                for f in range(NQF):
                                    i = f % 2
                                    for c in range(KC):
                                        op("pe", lambda p, c=c, f=f: p.matmul(pg[i][:, :n1], lhsT=Wg[w][:, c, 0, f * 128:(f + 1) * 128], rhs=hb[:, c, t1:t1 + n1], start=(c == 0), stop=(c == KC - 1)),
                                           reads=[("e_Wg%d" % w, 0, 0), ("e_Wg%d" % w, 0, 1), "e_hb"], writes=["e_pg%d" % i])
                                    for c in range(KC):
                                        op("pe", lambda p, c=c, f=f: p.matmul(pu[i][:, :n1], lhsT=Wg[w][:, c, 1, f * 128:(f + 1) * 128], rhs=hb[:, c, t1:t1 + n1], start=(c == 0), stop=(c == KC - 1)),
                                           reads=[("e_Wg%d" % w, 1, 0), ("e_Wg%d" % w, 1, 1), "e_hb"], writes=["e_pu%d" % i])
                                    sgl, a1 = sgl2[i], a12[i]
                                    op("act", lambda a: a.activation(out=sgl[:, :n1], in_=pg[i][:, :n1], func=AF.Silu), reads=["e_pg%d" % i], writes=["e_sgl%d" % i])
                                    op("dve", lambda v: v.tensor_tensor(out=a1[:, :n1], in0=pu[i][:, :n1], in1=sgl[:, :n1], op=ALU.mult), reads=["e_pu%d" % i, "e_sgl%d" % i], writes=["e_a1%d" % i])
                                    op("dve", lambda v, f=f: v.tensor_tensor(out=actb[ab][:, f, :n1], in0=a1[:, :n1], in1=cbc[:, e, t1:t1 + n1], op=ALU.mult), reads=["e_a1%d" % i, "e_cbc"], writes=["e_act%d" % ab])
                                for n in range(8):
                                    pq = po2[n % 2]
                                    for f in range(NQF):
                                        op("pe", lambda p, f=f: p.matmul(pq[:, :n1], lhsT=Wd[w][:, f, n * 128:(n + 1) * 128], rhs=actb[ab][:, f, :n1], start=(f == 0), stop=(f == NQF - 1)),
                                           reads=[("e_Wd%d" % w, 0), ("e_Wd%d" % w, 1), "e_act%d" % ab], writes=[pok[n % 2]])
                                    if e == 0 and qq == 0:
                                        op("dve", lambda v, n=n: v.tensor_copy(out=accm[:, n, t1:t1 + n1], in_=pq[:, :n1]), reads=[pok[n % 2]], writes=[("e_acc", n, t1)])
                                    else:
                                        op("dve", lambda v, n=n: v.tensor_tensor(out=accm[:, n, t1:t1 + n1], in0=accm[:, n, t1:t1 + n1], in1=pq[:, :n1], op=ALU.add), reads=[pok[n % 2], ("e_acc", n, t1)], writes=[("e_acc", n, t1)])
                                abi[0] += 1
                acck = [("e_acc", n, t5) for n in range(8) for t5 in range(0, SG, 512)]

                def moe_res(sg0):
                    for n in range(8):
                        op("dve", lambda v, n=n: v.scalar_tensor_tensor(out=accm[:, n, :], in0=hs[:, n, :], scalar=ALPHA, in1=accm[:, n, :], op0=ALU.mult, op1=ALU.add),
                           reads=["e_hs"] + [("e_acc", n, t5) for t5 in range(0, SG, 512)], writes=[("e_acc", n, t5) for t5 in range(0, SG, 512)] + ["e_res"])

                def moe_tail(sg0):
                    for t1 in range(0, SG, 256):
                        ln_fm(lt, accm[:, :, t1:t1 + 256], "e_res", h2, "e_h2o", gb, "e_gb", 256, zero_pads=False, extra_reads=acck)
                        for tb in range(2):
                            for c in range(KC):
                                op("pe", lambda p, c=c, tb=tb: p.transpose(out=pg[c // 4][:, (c % 4) * 128:(c % 4 + 1) * 128], in_=h2[:, c, tb * 128:(tb + 1) * 128], identity=ident[:]),
                                   reads=["e_h2o", "ident"], writes=["e_pg%d" % (c // 4)])
                            for hh in range(2):
                                evac(hh, osb[:, hh * 512:(hh + 1) * 512], pg[hh][:, :], ["e_pg%d" % hh], ["e_osb"])
                            r0 = sg0 + t1 + tb * 128
                            dma("pool", out[r0:r0 + 128, :], osb[:], reads=["e_osb"], writes=[("out", r0)])

                sgs = list(range(0, TH, SG))
                moe_prologue(sgs[0])
                moe_experts(sgs[0])
                for ki in range(1, len(sgs)):
                    moe_res(sgs[ki - 1])
                    lp_ = record(moe_prologue, sgs[ki])
                    lt_ = record(moe_tail, sgs[ki - 1])
                    replay_interleaved(lp_, lt_)
                    moe_experts(sgs[ki])
                moe_res(sgs[-1])
                moe_tail(sgs[-1])
                cx.barrier()

        stages = set(STAGES)
        if "ln_in" in stages:
            phase_ln_in(hT[0])
        if "win0" in stages:
            phase_win(0, hT[0])
        if "cprep0" in stages:
            phase_cprep(0)
        if "attn0" in stages:
            phase_attn(0)
        if "ssd0" in stages:
            phase_ssd(0)
        if "merge0" in stages:
            phase_merge(0, hT[0], hT[1])
        if "ffn0" in stages:
            phase_ffn_dense(0, hT[1], hT[0])
        if "layer1" in stages:
            phase_win(1, hT[0])
            phase_cprep(1)
            phase_attn(1)
            phase_ssd(1)
            phase_merge(1, hT[0], hT[1])
        if "moe" in stages:
            phase_moe(1, hT[1])

        cx.barrier()
    nc._in_names = in_names
    return nc


ALL_STAGES = ("ln_in", "win0", "cprep0", "attn0", "ssd0", "merge0", "ffn0", "layer1", "moe")


def kernel(**inputs):
    x = np.asarray(inputs["x"])
    B, S, _ = x.shape
    NB = S // 128 + 1
    HB = (NB - 1) // 2
    nc = build_program(NB, STAGES=ALL_STAGES)
    maps = prep_inputs(inputs, NB)
    maps = [{k: m[k] for k in nc._in_names} for m in maps]
    res = run_bass_kernel_spmd(nc, maps, core_ids=list(range(8)))
    out = np.zeros((B, S, D), np.float32)
    for c in range(8):
        b, half = c // 2, c % 2
        out[b, half * HB * 128:(half + 1) * HB * 128] = np.asarray(res.results[c]["out"])
    return out


def prep_inputs(inp, NB):
    S = (NB - 1) * 128
    f = lambda a: np.ascontiguousarray(np.asarray(a, dtype=np.float32))
    x = f(inp["x"])
    B = x.shape[0]
    shared = {
        "ln_in_gb": f(np.stack([inp["ln_in_g"], inp["ln_in_b"]], 0)),
        "w_in": f(inp["w_in"]),
        "b_forget": f(inp["b_forget"]),
        "ssd_conv_w": f(inp["ssd_conv_w"]),
        "ssd_conv_b": f(inp["ssd_conv_b"]),
        "ssd_vec": f(np.stack([inp["ssd_dt_bias"], inp["ssd_a_log"], inp["ssd_d"]], 1)),
        "ssd_norm_w": f(inp["ssd_norm_w"]),
        "sc_conv_w": f(inp["sc_conv_w"]),
        "w_proj": f(np.concatenate([inp["w_proj_attn"], inp["w_proj_ssd"], inp["w_proj_conv"]], 1)),
        "w_out": f(inp["w_out"]),
        "ln_mix_gb": f(np.stack([inp["ln_mix_g"], inp["ln_mix_b"]], 1)),
        "dense_w_gu": f(inp["dense_w_gu"][0]),
        "dense_w_down": f(inp["dense_w_down"][0]),
        "router_w": f(inp["router_w"][0]),
        "router_b": f(np.asarray(inp["router_b"]).reshape(1, NEXP)),
        "moe_w_gu": f(inp["moe_w_gu"][0]),
        "moe_w_down": f(inp["moe_w_down"][0]),
        "ln_ffn_gb": f(np.stack([inp["ln_ffn_g"], inp["ln_ffn_b"]], 1)),
    }
    maps = []
    for c in range(8):
        b, half = (c // 2) % B, c % 2
        xin = np.zeros((NB * 128, D), np.float32)
        xin[112:128] = f(inp["meta_tokens"])
        xin[128:] = x[b, :S]
        hs = np.zeros((128, 2), np.float32)
        hs[:, half] = 1.0
        m = dict(shared)
        m["xin"] = xin
        m["halfsel"] = hs
        maps.append(m)
    return maps
```

```python
import numpy as np
from contextlib import ExitStack
import concourse.bass as bass
import concourse.mybir as mybir
from concourse.bass_utils import run_bass_kernel_spmd

F32 = mybir.dt.float32
BF16 = mybir.dt.bfloat16
AF = mybir.ActivationFunctionType
ALU = mybir.AluOpType
AX = mybir.AxisListType

D = 1024
KC = 8
NQ = 512
FF_DENSE = 2816
FF_EXP = 3584
NEXP = 8
ALPHA = 4 ** 0.25
LN_EPS = 1e-5
RMS_EPS = 1e-5
O_Q, O_K, O_V, O_F, O_Z, O_XBC, O_DT, O_SC, O_G = 0, 512, 1024, 1536, 1544, 2568, 4104, 4120, 5656
N_IN = 8728


class Res:
    __slots__ = ("w", "r")

    def __init__(self):
        self.w = None
        self.r = []


class Eng:
    def __init__(self, name, h, sem):
        self.name, self.h, self.sem = name, h, sem
        self.count = 0
        self.known = {}


class Ctx:
    def __init__(self, nc, st, n_dma_sems=24):
        self.nc = nc
        self.res = {}
        self.eng = {}
        for name, h in (("pe", nc.tensor), ("act", nc.scalar), ("dve", nc.vector), ("pool", nc.gpsimd), ("sp", nc.sync)):
            self.eng[name] = Eng(name, h, st.enter_context(nc.semaphore("sem_" + name)))
        self.dsems = [st.enter_context(nc.semaphore("dsem%d" % i)) for i in range(2 * n_dma_sems)]
        self.dval = [0] * len(self.dsems)
        self.dq = {"sp": list(range(0, n_dma_sems)), "pool": list(range(n_dma_sems, 2 * n_dma_sems))}
        self.dqi = {"sp": 0, "pool": 0}

    def R(self, key):
        r = self.res.get(key)
        if r is None:
            r = self.res[key] = Res()
        return r

    def _wait(self, E, deps, raw_self_only):
        need = {}
        for d in deps:
            k = (d[0], d[1])
            if need.get(k, 0) < d[2]:
                need[k] = d[2]
        for (kind, key), val in need.items():
            if kind == "e" and key == E.name:
                if E.name in ("pe", "sp"):
                    continue
            if E.known.get((kind, key), 0) >= val:
                continue
            sem = self.eng[key].sem if kind == "e" else self.dsems[key]
            E.h.wait_ge(sem, val)
            E.known[(kind, key)] = val

    def _collect(self, reads, writes):
        deps, raw = [], set()
        for k in reads:
            r = self.R(k)
            if r.w is not None:
                deps.append(r.w)
                raw.add(r.w)
        for k in writes:
            r = self.R(k)
            if r.w is not None:
                deps.append(r.w)
            deps.extend(r.r)
        return deps, raw

    def _commit(self, tok, reads, writes):
        for k in reads:
            self.R(k).r.append(tok)
        for k in writes:
            r = self.R(k)
            r.w = tok
            r.r = []

    def op(self, en, fn, reads=(), writes=()):
        E = self.eng[en]
        deps, raw = self._collect(reads, writes)
        self._wait(E, deps, raw)
        inst = fn(E.h)
        E.count += 1
        inst.then_inc(E.sem, 1)
        self._commit(("e", en, E.count), reads, writes)

    def dma(self, q, out, in_, reads=(), writes=(), **kw):
        E = self.eng[q]
        lst = self.dq[q]
        si = lst[self.dqi[q] % len(lst)]
        self.dqi[q] += 1
        deps, raw = self._collect(reads, writes)
        if self.dval[si] > 0:
            deps.append(("d", si, self.dval[si]))
        self._wait(E, deps, raw)
        E.h.dma_start(out=out, in_=in_, **kw).then_inc(self.dsems[si], 16)
        self.dval[si] += 16
        self._commit(("d", si, self.dval[si]), reads, writes)

    def barrier(self):
        deps = [("e", n, e.count) for n, e in self.eng.items() if e.count > 0 and n != "sp"]
        deps += [("d", i, v) for i, v in enumerate(self.dval) if v > 0]
        for E in self.eng.values():
            self._wait(E, deps, set(deps))
        self.res = {}


def _blocks(nb, g):
    out = []
    b = 0
    while b < nb:
        n = min(g, nb - b)
        out.append((b, n))
        b += n
    return out


def build_program(NB, debug=(), STAGES=("ln_in", "win0", "cprep0", "attn0")):
    T = NB * 128
    HB = (NB - 1) // 2
    TH = HB * 128
    nc = bass.Bass("TRN2", target_bir_lowering=False)
    dbg = set(debug)

    in_names = []

    def dram_in(name, shape, dt=F32):
        if name.startswith("moe_w") and "moe" not in STAGES:
            return None
        in_names.append(name)
        return nc.dram_tensor(name, list(shape), dt, kind="ExternalInput").ap()

    def dram_scr(name, shape, dt):
        kind = "ExternalOutput" if name in dbg else "Internal"
        return nc.dram_tensor(name, list(shape), dt, kind=kind).ap()

    xin = dram_in("xin", [T, D])
    ln_in_gb = dram_in("ln_in_gb", [2, D])
    w_in = dram_in("w_in", [2, D, N_IN])
    b_forget = dram_in("b_forget", [2, 8])
    ssd_conv_w = dram_in("ssd_conv_w", [2, 4, 1536])
    ssd_conv_b = dram_in("ssd_conv_b", [2, 1536])
    ssd_vec = dram_in("ssd_vec", [2, 3, 16])
    ssd_norm_w = dram_in("ssd_norm_w", [2, 1024])
    sc_conv_w = dram_in("sc_conv_w", [2, 3, 512])
    w_proj = dram_in("w_proj", [2, 2048, D])
    w_out = dram_in("w_out", [2, D, D])
    ln_mix_gb = dram_in("ln_mix_gb", [2, 2, D])
    dense_w_gu = dram_in("dense_w_gu", [D, 2 * FF_DENSE])
    dense_w_down = dram_in("dense_w_down", [FF_DENSE, D])
    router_w = dram_in("router_w", [D, NEXP])
    router_b = dram_in("router_b", [1, NEXP])
    moe_w_gu = dram_in("moe_w_gu", [NEXP, D, 2 * FF_EXP])
    moe_w_down = dram_in("moe_w_down", [NEXP, FF_EXP, D])
    ln_ffn_gb = dram_in("ln_ffn_gb", [2, 2, D])
    halfsel = dram_in("halfsel", [128, 2])
    out = nc.dram_tensor("out", [TH, D], F32, kind="ExternalOutput").ap()

    hT = [dram_scr("hT%d" % i, [KC, 128, T], F32) for i in range(2)]
    qkT = dram_scr("qkT", [8, 128, T], BF16)
    xbcT = dram_scr("xbcT", [12, 128, T + 128], BF16)
    scT = dram_scr("scT", [12, 128, T + 128], BF16)
    gT = dram_scr("gT", [24, 128, T], BF16)
    vtm = dram_scr("vtm", [T, 512], BF16)
    ztm = dram_scr("ztm", [T, 1024], BF16)
    dtr = dram_scr("dtr", [T, 16], F32)
    fT = dram_scr("fT", [8, T], F32)
    caugQ = dram_scr("caugQ", [8, 6, T], BF16)
    caugK = dram_scr("caugK", [8, 6, T], BF16)
    yattT = dram_scr("yattT", [4, 128, T], BF16)
    yssdT = dram_scr("yssdT", [8, 128, T], BF16)

    with ExitStack() as st:
        cx = Ctx(nc, st)
        Rk = cx.R
        rec_list = [None]

        def op(en, fn, reads=(), writes=()):
            if rec_list[0] is not None:
                rec_list[0].append(("op", en, fn, tuple(reads), tuple(writes), None))
            else:
                cx.op(en, fn, reads, writes)

        def dma(q, out, in_, reads=(), writes=(), **kw):
            if rec_list[0] is not None:
                rec_list[0].append(("dma", q, (out, in_), tuple(reads), tuple(writes), kw))
            else:
                cx.dma(q, out, in_, reads, writes, **kw)

        def record(f, *a):
            rec_list[0] = []
            f(*a)
            lst = rec_list[0]
            rec_list[0] = None
            return lst

        def replay(item):
            kind, e, x, rd, wr, kw = item
            if kind == "op":
                cx.op(e, x, rd, wr)
            else:
                cx.dma(e, x[0], x[1], rd, wr, **kw)

        def replay_interleaved(l1, l2):
            i = j = 0
            n1, n2 = len(l1), len(l2)
            while i < n1 or j < n2:
                if j >= n2 or (i < n1 and i * n2 <= j * n1):
                    replay(l1[i])
                    i += 1
                else:
                    replay(l2[j])
                    j += 1

        uid = [0]

        def sb(name, shape, dt, stk):
            uid[0] += 1
            return stk.enter_context(nc.sbuf_tensor("%s_u%d" % (name, uid[0]), list(shape), dt))

        def ps(name, shape, dt, stk):
            uid[0] += 1
            return stk.enter_context(nc.psum_tensor("%s_u%d" % (name, uid[0]), list(shape), dt))

        ident = sb("ident", [128, 128], F32, st)
        identb = sb("identb", [128, 128], BF16, st)
        ones_f = sb("ones_f", [128, 128], F32, st)
        tri_le = sb("tri_le", [128, 128], F32, st)
        tri_leb = sb("tri_leb", [128, 128], BF16, st)
        smask = sb("smask", [128, 128], F32, st)
        validcol = sb("validcol", [128, 1], F32, st)
        hsel = sb("hsel", [128, 2], F32, st)

        def c_init(g):
            return g.memset(ones_f[:], 1.0)
        op("pool", c_init, writes=["ones_f"])
        op("pool", lambda g: g.memset(ident[:], 0.0), writes=["ident"])
        op("pool", lambda g: g.affine_select(out=ident[:], in_=ones_f[:], pattern=[[-1, 128]], base=0,
                                              channel_multiplier=1, compare_op=ALU.is_equal, fill=0.0),
           reads=["ones_f"], writes=["ident"])
        op("pool", lambda g: g.affine_select(out=tri_le[:], in_=ones_f[:], pattern=[[1, 128]], base=0,
                                              channel_multiplier=-1, compare_op=ALU.is_ge, fill=0.0),
           reads=["ones_f"], writes=["tri_le"])
        op("pool", lambda g: g.affine_select(out=smask[:], in_=ones_f[:], pattern=[[-1, 128]], base=0,
                                              channel_multiplier=1, compare_op=ALU.is_gt, fill=0.0),
           reads=["ones_f"], writes=["smask"])
        op("pool", lambda g: g.affine_select(out=validcol[:], in_=ones_f[:, 0:1], pattern=[[0, 1]], base=-112,
                                              channel_multiplier=1, compare_op=ALU.is_ge, fill=0.0),
           reads=["ones_f"], writes=["validcol"])
        op("dve", lambda v: v.tensor_copy(out=identb[:], in_=ident[:]), reads=["ident"], writes=["identb"])
        op("dve", lambda v: v.tensor_copy(out=tri_leb[:], in_=tri_le[:]), reads=["tri_le"], writes=["tri_leb"])
        dma("sp", hsel[:], halfsel[:, :], writes=["hsel"])

        def phase_ln_in(hT_out):
            with ExitStack() as s:
                gbc = sb("p0_gb", [128, 2, KC], F32, s)
                xt = [sb("p0_x%d" % i, [128, D], F32, s) for i in range(2)]
                xn = [sb("p0_xn%d" % i, [128, D], F32, s) for i in range(2)]
                stt = [sb("p0_st%d" % i, [128, 2, 6], F32, s) for i in range(2)]
                mv = [sb("p0_mv%d" % i, [128, 4], F32, s) for i in range(2)]
                ho = [sb("p0_ho%d" % i, [128, KC, 128], F32, s) for i in range(2)]
                pt = [ps("p0_pt%d" % i, [128, D], F32, s) for i in range(2)]
                with nc.allow_non_contiguous_dma(reason="tiny param"):
                    dma("sp", gbc[:], ln_in_gb.rearrange("t (c p) -> p t c", p=128), writes=["p0_gb"])
                for b in range(NB):
                    i = b % 2
                    kx, kn, ks, km, kh, kp = ("p0_x%d" % i, "p0_xn%d" % i, "p0_st%d" % i, "p0_mv%d" % i, "p0_ho%d" % i, "p0_pt%d" % i)
                    dma("sp", xt[i][:], xin[b * 128:(b + 1) * 128, :], writes=[kx])
                    for hh in range(2):
                        op("dve", lambda v, hh=hh: v.bn_stats(out=stt[i][:, hh, :], in_=xt[i][:, hh * 512:(hh + 1) * 512]),
                           reads=[kx], writes=[ks])
                    op("dve", lambda v: v.bn_aggr(out=mv[i][:, 0:2], in_=stt[i][:].rearrange("p a b -> p (a b)")),
                       reads=[ks], writes=[km])
                    op("act", lambda a: a.activation(out=mv[i][:, 2:3], in_=mv[i][:, 1:2], func=AF.Sqrt, bias=LN_EPS_T[:], scale=1.0),
                       reads=[km, "eps"], writes=[km])
                    op("dve", lambda v: v.reciprocal(out=mv[i][:, 3:4], in_=mv[i][:, 2:3]), reads=[km], writes=[km])
                    op("dve", lambda v: v.tensor_scalar(out=xn[i][:], in0=xt[i][:], scalar1=mv[i][:, 0:1], scalar2=mv[i][:, 3:4],
                                                        op0=ALU.subtract, op1=ALU.mult), reads=[kx, km], writes=[kn])
                    for c in range(KC):
                        op("pe", lambda p, c=c: p.transpose(out=pt[i][:, c * 128:(c + 1) * 128], in_=xn[i][:, c * 128:(c + 1) * 128], identity=ident[:]),
                           reads=[kn, "ident"], writes=[kp])
                    for c in range(KC):
                        op("act", lambda a, c=c: a.activation(out=ho[i][:, c, :], in_=pt[i][:, c * 128:(c + 1) * 128], func=AF.Identity,
                                                               scale=gbc[:, 0, c:c + 1], bias=gbc[:, 1, c:c + 1]),
                           reads=[kp, "p0_gb"], writes=[kh])
                    if b == 0:
                        op("dve", lambda v: v.memset(ho[i][:, :, 0:112], 0.0), reads=[], writes=[kh])
                    dma("pool", hT_out[:, :, b * 128:(b + 1) * 128].rearrange("c p t -> p c t"), ho[i][:], reads=[kh], writes=[("hT", b)])
                cx.barrier()

        LN_EPS_T = sb("eps_t", [128, 1], F32, st)
        op("pool", lambda g: g.memset(LN_EPS_T[:], LN_EPS), writes=["eps"])
        RMS_EPS_T = LN_EPS_T

        def evac(i, out_ap, in_ap, reads, writes):
            if i % 2 == 0:
                op("act", lambda a: a.activation(out=out_ap, in_=in_ap, func=AF.Copy), reads=reads, writes=writes)
            else:
                op("dve", lambda v: v.tensor_copy(out=out_ap, in_=in_ap), reads=reads, writes=writes)

        wst_i = [0]

        def mk_wstg(s):
            return [sb("wstg%d" % i, [128, 2048], F32, s) for i in range(3)]

        def load_w(dst, src2d, ncols, key, rows_c, wstg):
            for c in range(rows_c):
                n0 = 0
                while n0 < ncols:
                    n1 = min(ncols, n0 + 2048)
                    si = wst_i[0] % 3
                    wst_i[0] += 1
                    dma("sp", wstg[si][:, :n1 - n0], src2d[c * 128:(c + 1) * 128, n0:n1], writes=["wstg%d" % si])
                    if si == 0:
                        op("act", lambda a: a.activation(out=dst[:, c, n0:n1], in_=wstg[si][:, :n1 - n0], func=AF.Copy), reads=["wstg%d" % si], writes=[key])
                    elif si == 1:
                        op("dve", lambda v: v.tensor_copy(out=dst[:, c, n0:n1], in_=wstg[si][:, :n1 - n0]), reads=["wstg%d" % si], writes=[key])
                    else:
                        op("pool", lambda v: v.tensor_copy(out=dst[:, c, n0:n1], in_=wstg[si][:, :n1 - n0]), reads=["wstg%d" % si], writes=[key])
                    n0 = n1

        def phase_win(l, h_in):
            for pas in (0, 1):
                with ExitStack() as s:
                    if pas == 0:
                        segs = [(O_Q, 1024), (O_XBC, 1536), (O_V, 512), (O_Z, 1024), (O_F, 8), (O_DT, 16)]
                    else:
                        segs = [(O_SC, 1536), (O_G, 3072)]
                    ncols = sum(n for _, n in segs)
                    W = sb("p1_W", [128, KC, ncols], BF16, s)
                    wstg = mk_wstg(s)
                    off = {}
                    o = 0
                    for (c0, n) in segs:
                        load_w(W[:, :, o:o + n], w_in[l][:, c0:c0 + n], n, "p1_W", KC, wstg)
                        off[c0] = o
                        o += n
                    hf = [sb("p1_hf%d" % i, [128, KC, 512], F32, s) for i in range(2)]
                    hb = [sb("p1_hb%d" % i, [128, KC, 512], BF16, s) for i in range(2)]
                    ob = [sb("p1_ob%d" % i, [128, 4, 512], BF16, s) for i in range(3)]
                    pf = [ps("p1_pf%d" % i, [128, 512], F32, s) for i in range(4)]
                    if pas == 0:
                        zt = sb("p1_z", [128, 128], BF16, s)
                        op("dve", lambda v: v.memset(zt[:], 0.0), writes=["p1_z"])
                        for cc in range(12):
                            dma("pool", xbcT[cc, :, 0:128], zt[:], reads=["p1_z"], writes=[("xbcT", -1)])
                            dma("pool", scT[cc, :, 0:128], zt[:], reads=["p1_z"], writes=[("scT", -1)])
                        tv = sb("p1_tv", [128, 4, 512], BF16, s)
                        tz = sb("p1_tz", [128, 4, 1024], BF16, s)
                        tdt = sb("p1_tdt", [128, 4, 16], F32, s)
                        fst = sb("p1_fst", [8, 512], F32, s)
                        pt = [ps("p1_pt%d" % i, [128, 512], F32, s) for i in range(3)]
                    ei = 0
                    pi = 0
                    obi = 0
                    for gi, (b0, nb) in enumerate(_blocks(NB, 4)):
                        i = gi % 2
                        t0, nt = b0 * 128, nb * 128
                        dma("sp", hf[i][:, :, :nt], h_in[:, :, t0:t0 + nt].rearrange("c p t -> p c t"), writes=["p1_hf%d" % i])
                        for c in range(KC):
                            evac(c, hb[i][:, c, :nt], hf[i][:, c, :nt], ["p1_hf%d" % i], ["p1_hb%d" % i])
                        if pas == 0:
                            fm = [(O_Q, 8, qkT, 0, 0), (O_XBC, 12, xbcT, 0, 128)]
                        else:
                            fm = [(O_SC, 12, scT, 0, 128), (O_G, 24, gT, 0, 0)]
                        for (c0, nch, dst, dch, dof) in fm:
                            for n4 in range(0, nch, 4):
                                oi = obi % 3
                                obi += 1
                                for n in range(n4, n4 + 4):
                                    pp = pi % 4
                                    pi += 1
                                    col = off[c0] + n * 128
                                    for c in range(KC):
                                        op("pe", lambda p, c=c, col=col, pp=pp: p.matmul(pf[pp][:, :nt], lhsT=W[:, c, col:col + 128], rhs=hb[i][:, c, :nt],
                                                                                          start=(c == 0), stop=(c == KC - 1)),
                                           reads=["p1_W", "p1_hb%d" % i], writes=["p1_pf%d" % pp])
                                    evac(ei, ob[oi][:, n - n4, :nt], pf[pp][:, :nt], ["p1_pf%d" % pp], ["p1_ob%d" % oi])
                                    ei += 1
                                dma("pool", dst[dch + n4:dch + n4 + 4, :, dof + t0:dof + t0 + nt].rearrange("c p t -> p c t"), ob[oi][:, :, :nt],
                                    reads=["p1_ob%d" % oi], writes=[(id(dst), gi, n4)])
                        if pas == 0:
                            pp = pi % 4
                            pi += 1
                            col = off[O_F]
                            for c in range(KC):
                                op("pe", lambda p, c=c, col=col, pp=pp: p.matmul(pf[pp][0:8, :nt], lhsT=W[:, c, col:col + 8], rhs=hb[i][:, c, :nt],
                                                                                  start=(c == 0), stop=(c == KC - 1)),
                                   reads=["p1_W", "p1_hb%d" % i], writes=["p1_pf%d" % pp])
                            op("dve", lambda v, pp=pp: v.tensor_copy(out=fst[:, :nt], in_=pf[pp][0:8, :nt]), reads=["p1_pf%d" % pp], writes=["p1_fst"])
                            dma("pool", fT[:, t0:t0 + nt], fst[:, :nt], reads=["p1_fst"], writes=[("fT", gi)])
                            pti = 0
                            for bi in range(nb):
                                for (c0, n, dstt, do) in ((O_V, 512, tv, 0), (O_Z, 512, tz, 0), (O_Z + 512, 512, tz, 512), (O_DT, 16, tdt, 0)):
                                    pq = pti % 3
                                    pti += 1
                                    col = off[O_Z] + (c0 - O_Z) if c0 >= O_Z and c0 < O_XBC else off[c0]
                                    for c in range(KC):
                                        op("pe", lambda p, c=c, col=col, pq=pq, n=n: p.matmul(pt[pq][:, :n], lhsT=hb[i][:, c, bi * 128:(bi + 1) * 128], rhs=W[:, c, col:col + n],
                                                                                                start=(c == 0), stop=(c == KC - 1)),
                                           reads=["p1_W", "p1_hb%d" % i], writes=["p1_pt%d" % pq])
                                    evac(ei, dstt[:, bi, do:do + n], pt[pq][:, :n], ["p1_pt%d" % pq], [id(dstt)])
                                    ei += 1
                            dma("pool", vtm[t0:t0 + nt, :].rearrange("(b p) n -> p b n", p=128), tv[:, :nb, :], reads=[id(tv)], writes=[("vtm", gi)])
                            dma("pool", ztm[t0:t0 + nt, :].rearrange("(b p) n -> p b n", p=128), tz[:, :nb, :], reads=[id(tz)], writes=[("ztm", gi)])
                            with nc.allow_non_contiguous_dma(reason="small dt rows"):
                                dma("pool", dtr[t0:t0 + nt, :].rearrange("(b p) n -> p b n", p=128), tdt[:, :nb, :], reads=[id(tdt)], writes=[("dtr", gi)])
                    cx.barrier()

        def phase_cprep(l):
            with ExitStack() as s:
                t1 = sb("c_t1", [8, T], F32, s)
                t2 = sb("c_t2", [8, T], F32, s)
                t3 = sb("c_t3", [8, T], F32, s)
                b1 = sb("c_b1", [8, T], BF16, s)
                b2 = sb("c_b2", [8, T], BF16, s)
                b3 = sb("c_b3", [8, T], BF16, s)
                nb_ = sb("c_nb", [8, 1], F32, s)
                with nc.allow_non_contiguous_dma(reason="tiny param"):
                    dma("sp", nb_[:], b_forget[l].rearrange("(h o) -> h o", o=1), writes=["c_nb"])
                dma("sp", t1[:], fT[:, :], writes=["c_t1"])
                op("dve", lambda v: v.tensor_scalar(out=nb_[:], in0=nb_[:], scalar1=-1.0, scalar2=None, op0=ALU.mult), reads=["c_nb"], writes=["c_nb"])
                op("act", lambda a: a.activation(out=t1[:], in_=t1[:], func=AF.Exp, scale=-1.0, bias=nb_[:]), reads=["c_t1", "c_nb"], writes=["c_t1"])
                op("act", lambda a: a.activation(out=t1[:], in_=t1[:], func=AF.Ln, scale=1.0, bias=ones_f[0:8, 0:1]), reads=["c_t1", "ones_f"], writes=["c_t1"])
                op("dve", lambda v: v.memset(t1[:, 0:112], 0.0), writes=["c_t1"])
                op("dve", lambda v: v.memset(t2[:], 1.0), writes=["c_t2"])
                op("dve", lambda v: v.tensor_tensor_scan(out=t3[:], data0=t2[:], data1=t1[:], initial=0.0, op0=ALU.mult, op1=ALU.add),
                   reads=["c_t1", "c_t2"], writes=["c_t3"])

                def split3(dst, rows):
                    bb = [b1, b2, b1]
                    kk = ["c_b1", "c_b2", "c_b1"]
                    for j in range(3):
                        op("dve", lambda v, j=j: v.tensor_copy(out=bb[j][:], in_=t1[:]), reads=["c_t1"], writes=[kk[j]])
                        dma("sp", dst[:, rows[j], :], bb[j][:], reads=[kk[j]], writes=[(id(dst), rows[j])])
                        if j < 2:
                            op("dve", lambda v, j=j: v.tensor_copy(out=t2[:], in_=bb[j][:]), reads=[kk[j]], writes=["c_t2"])
                            op("dve", lambda v: v.tensor_tensor(out=t1[:], in0=t1[:], in1=t2[:], op=ALU.subtract), reads=["c_t1", "c_t2"], writes=["c_t1"])

                op("dve", lambda v: v.tensor_scalar(out=t1[:], in0=t3[:], scalar1=8.0, scalar2=None, op0=ALU.mult), reads=["c_t3"], writes=["c_t1"])
                op("dve", lambda v: v.memset(t1[:, 0:112], -240000.0), writes=["c_t1"])
                split3(caugK, [3, 4, 5])
                t3v = t3[:, :].rearrange("h (b t) -> h b t", t=128)
                op("dve", lambda v: v.tensor_scalar(out=t1[:, :].rearrange("h (b t) -> h b t", t=128), in0=t3v[:, :, 127:128].broadcast_to([8, NB, 128]),
                                                    scalar1=-4.0, scalar2=None, op0=ALU.mult), reads=["c_t3"], writes=["c_t1"])
                if NB > 1:
                    src_ = t3[:, 0:T - 128].rearrange("h (b t) -> h b t", t=128)[:, :, 127:128].broadcast_to([8, NB - 1, 128])
                    op("dve", lambda v: v.scalar_tensor_tensor(out=t1[:, 128:T].rearrange("h (b t) -> h b t", t=128), in0=src_, scalar=-4.0,
                                                               in1=t1[:, 128:T].rearrange("h (b t) -> h b t", t=128), op0=ALU.mult, op1=ALU.add),
                       reads=["c_t3", "c_t1"], writes=["c_t1"])
                split3(caugQ, [0, 1, 2])
                op("dve", lambda v: v.memset(b3[:], 1.0), writes=["c_b3"])
                for r in (3, 4, 5):
                    dma("sp", caugQ[:, r, :], b3[:], reads=["c_b3"], writes=[(id(caugQ), r)])
                for r in (0, 1, 2):
                    dma("sp", caugK[:, r, :], b3[:], reads=["c_b3"], writes=[(id(caugK), r)])
                cx.barrier()

        def phase_attn(l):
            with ExitStack() as s:
                Qp = [sb("a_Q%d" % i, [128, T], BF16, s) for i in range(2)]
                Kp = [sb("a_K%d" % i, [128, T], BF16, s) for i in range(2)]
                Vp = [sb("a_V%d" % i, [128, NB, 128], BF16, s) for i in range(2)]
                NSB = 3
                sbase = [0]
                obase = [0]
                Pt = [sb("a_P%d" % i, [128, 1024], BF16, s) for i in range(NSB)]
                rec = sb("a_rec", [128, 512], F32, s)
                yo = [sb("a_yo%d" % i, [64, 512], BF16, s) for i in range(2)]
                Sp = [ps("a_S%d" % i, [128, 1024], F32, s) for i in range(NSB)]
                Op = [ps("a_O%d" % i, [128, 512], F32, s) for i in range(2)]
                for i in range(2):
                    op("dve", lambda v, i=i: v.memset(Vp[i][:, :, 64:128], 1.0), writes=["a_V%d" % i])
                for h in range(8):
                    i = h % 2
                    r0 = (h % 2) * 64
                    dma("sp", Qp[i][0:64, :], qkT[h // 2, r0:r0 + 64, :], writes=["a_Q%d" % i])
                    dma("sp", Qp[i][64:70, :], caugQ[h], writes=["a_Q%d" % i])
                    dma("sp", Kp[i][0:64, :], qkT[4 + h // 2, r0:r0 + 64, :], writes=["a_K%d" % i])
                    dma("sp", Kp[i][64:70, :], caugK[h], writes=["a_K%d" % i])
                    with nc.allow_non_contiguous_dma(reason="128B v rows"):
                        for (vb0, vnb) in _blocks(NB, 8):
                            dma("sp", Vp[i][:, vb0:vb0 + vnb, 0:64],
                                vtm[vb0 * 128:(vb0 + vnb) * 128, h * 64:(h + 1) * 64].rearrange("(b p) d -> p b d", p=128), writes=["a_V%d" % i])
                    steps = []
                    for gi, (q0b, nqb) in enumerate(_blocks(NB, 4)):
                        js = list(range(q0b + nqb))
                        for a_ in range(0, len(js), 2):
                            parts = []
                            off = 0
                            for j in js[a_:a_ + 2]:
                                qs = max(q0b, j)
                                ncol = (q0b + nqb - qs) * 128
                                if off + ncol > 512 and off > 0:
                                    off = 512
                                parts.append((j, qs, ncol, off))
                                off += ncol
                            steps.append((gi, q0b, nqb, parts))
                    LOOK = NSB - 1

                    def emit_qk(k):
                        gi, q0b, nqb, parts = steps[k]
                        sp_ = (sbase[0] + k) % NSB
                        for (j, qs, ncol, off) in parts:
                            op("pe", lambda p: p.matmul(Sp[sp_][:, off:off + ncol], lhsT=Kp[i][0:70, j * 128:(j + 1) * 128],
                                                        rhs=Qp[i][0:70, qs * 128:qs * 128 + ncol], start=True, stop=True),
                               reads=["a_Q%d" % i, "a_K%d" % i], writes=["a_S%d" % sp_])

                    def emit_rest(k):
                        gi, q0b, nqb, parts = steps[k]
                        q1b = q0b + nqb
                        sp_ = (sbase[0] + k) % NSB
                        oo = (obase[0] + gi) % 2
                        tot = parts[-1][3] + parts[-1][2]
                        op("act", lambda a: a.activation(out=Pt[sp_][:, :tot], in_=Sp[sp_][:, :tot], func=AF.Exp, scale=0.125),
                           reads=["a_S%d" % sp_], writes=["a_P%d" % sp_])
                        for (j, qs, ncol, off) in parts:
                            if j >= q0b:
                                op("pool", lambda v: v.tensor_tensor(out=Pt[sp_][:, off:off + 128], in0=Pt[sp_][:, off:off + 128], in1=tri_leb[:], op=ALU.mult),
                                   reads=["a_P%d" % sp_, "tri_leb"], writes=["a_P%d" % sp_])
                        for (j, qs, ncol, off) in parts:
                            c0 = (qs - q0b) * 128
                            op("pe", lambda p: p.matmul(Op[oo][:, c0:c0 + ncol], lhsT=Vp[i][:, j, :], rhs=Pt[sp_][:, off:off + ncol],
                                                        start=(j == 0), stop=(j == q1b - 1)),
                               reads=["a_V%d" % i, "a_P%d" % sp_], writes=["a_O%d" % oo])
                        if parts[-1][0] == q1b - 1:
                            nt = nqb * 128
                            op("dve", lambda v: v.tensor_scalar(out=rec[64:128, :nt], in0=Op[oo][64:128, :nt], scalar1=1e-36, scalar2=None, op0=ALU.add),
                               reads=["a_O%d" % oo], writes=["a_rec"])
                            op("dve", lambda v: v.reciprocal(out=rec[64:128, :nt], in_=rec[64:128, :nt]), reads=["a_rec"], writes=["a_rec"])
                            yi = gi % 2
                            op("dve", lambda v: v.tensor_tensor(out=yo[yi][0:64, :nt], in0=Op[oo][0:64, :nt], in1=rec[64:128, :nt], op=ALU.mult),
                               reads=["a_O%d" % oo, "a_rec"], writes=["a_yo%d" % yi])
                            dma("pool", yattT[h // 2, r0:r0 + 64, q0b * 128:q0b * 128 + nt], yo[yi][0:64, :nt], reads=["a_yo%d" % yi], writes=[("yattT", h, gi)])

                    for k in range(min(LOOK, len(steps))):
                        emit_qk(k)
                    for k in range(len(steps)):
                        if k + LOOK < len(steps):
                            emit_qk(k + LOOK)
                        emit_rest(k)
                    sbase[0] += len(steps)
                    obase[0] += len(_blocks(NB, 4))
                cx.barrier()


        def bc(ap, dim, shape):
            return ap.unsqueeze(dim).broadcast_to(list(shape))

        def phase_ssd(l):
            with ExitStack() as s:
                cw = sb("s_cw", [128, 4, 12], F32, s)
                cbias = sb("s_cb", [128, 12], F32, s)
                vec = sb("s_vec", [128, 3, 16], F32, s)
                nw = sb("s_nw", [128, 1024], F32, s)
                dt_all = sb("s_dt", [128, NB, 16], F32, s)
                dtA = sb("s_dtA", [128, NB, 16], F32, s)
                xr_1 = sb("s_xr", [128, 12, 3 + 512], BF16, s)
                xr_2 = [xr_1, xr_1]
                acc12 = sb("s_acc", [128, 12, 512], F32, s)
                xc_2 = [sb("s_xc_%d" % i_, [128, 12, 512], BF16, s) for i_ in range(2)]
                xs_tm_2 = [sb("s_xs_%d" % i_, [128, 1024], BF16, s) for i_ in range(2)]
                Btm_2 = [sb("s_B_%d" % i_, [128, 256], BF16, s) for i_ in range(2)]
                Rt_2 = [sb("s_R_%d" % i_, [128, 16, 128], F32, s) for i_ in range(2)]
                acs_2 = [sb("s_acs_%d" % i_, [128, 32], F32, s) for i_ in range(2)]
                sm_2 = [sb("s_sm_%d" % i_, [128, 4, 16], F32, s) for i_ in range(2)]
                cbm_2 = [sb("s_cbm_%d" % i_, [128, 2, 128], BF16, s) for i_ in range(2)]
                dec = sb("s_dec", [128, 8, 128], BF16, s)
                M_2 = [sb("s_M_%d" % i_, [128, 16, 128], BF16, s) for i_ in range(2)]
                xdt_2 = [sb("s_xdt_%d" % i_, [128, 1024], BF16, s) for i_ in range(2)]
                xdtS_2 = [sb("s_xdtS_%d" % i_, [128, 1024], BF16, s) for i_ in range(2)]
                state = sb("s_state", [128, 1024], F32, s)
                prevb = sb("s_prevb", [128, 1024], BF16, s)
                yoff_2 = [sb("s_yoff_%d" % i_, [128, 1024], F32, s) for i_ in range(2)]
                ysb_2 = [sb("s_y_%d" % i_, [128, 1024], F32, s) for i_ in range(2)]
                tmp_2 = [sb("s_tmp_%d" % i_, [128, 1024], F32, s) for i_ in range(2)]
                ztg = sb("s_ztg", [128, 4, 1024], BF16, s)
                szg_2 = [sb("s_szg_%d" % i_, [128, 4, 1024], F32, s) for i_ in range(2)]
                ss = sb("s_ss", [128, 4], F32, s)
                yn_2 = [sb("s_yn_%d" % i_, [128, 1024], BF16, s) for i_ in range(2)]
                ysT_2 = [sb("s_ysT_%d" % i_, [128, 8, 512], BF16, s) for i_ in range(2)]
                ptr = ps("s_ptr", [128, 1024], BF16, s)
                ptb = ps("s_ptb", [128, 256], BF16, s)
                segp = ps("s_seg", [128, 1024], F32, s)
                Yp = ps("s_Y", [128, 512], F32, s)
                ptry = ps("s_ptry", [128, 1024], BF16, s)
                mA = ps("s_mA", [128, 512], F32, s)
                mB = ps("s_mB", [128, 512], F32, s)
                with nc.allow_non_contiguous_dma(reason="tiny params"):
                    for k in range(4):
                        dma("sp", cw[:, k, :], ssd_conv_w[l, k].rearrange("(c p) -> p c", p=128), writes=["s_cw"])
                    dma("sp", cbias[:], ssd_conv_b[l].rearrange("(c p) -> p c", p=128), writes=["s_cb"])
                    dma("sp", vec[:].rearrange("p a b -> p (a b)"), ssd_vec[l].rearrange("a b -> (a b)").partition_broadcast(128), writes=["s_vec"])
                    dma("sp", nw[:], ssd_norm_w[l].partition_broadcast(128), writes=["s_nw"])
                    dma("sp", dt_all[:], dtr.rearrange("(b p) n -> p b n", p=128), writes=["s_dt"])
                op("act", lambda a: a.activation(out=vec[:, 1, :], in_=vec[:, 1, :], func=AF.Exp), reads=["s_vec"], writes=["s_vec"])
                op("dve", lambda v: v.tensor_scalar(out=vec[:, 1, :], in0=vec[:, 1, :], scalar1=-1.0, scalar2=None, op0=ALU.mult), reads=["s_vec"], writes=["s_vec"])
                op("dve", lambda v: v.tensor_tensor(out=dt_all[:], in0=dt_all[:], in1=bc(vec[:, 0, :], 1, [128, NB, 16]), op=ALU.add), reads=["s_dt", "s_vec"], writes=["s_dt"])
                op("act", lambda a: a.activation(out=dt_all[:], in_=dt_all[:], func=AF.Exp), reads=["s_dt"], writes=["s_dt"])
                op("act", lambda a: a.activation(out=dt_all[:], in_=dt_all[:], func=AF.Ln, bias=ones_f[:, 0:1], scale=1.0), reads=["s_dt", "ones_f"], writes=["s_dt"])
                op("dve", lambda v: v.tensor_scalar(out=dt_all[:, 0, :], in0=dt_all[:, 0, :], scalar1=validcol[:, 0:1], scalar2=None, op0=ALU.mult),
                   reads=["s_dt", "validcol"], writes=["s_dt"])
                op("dve", lambda v: v.tensor_tensor(out=dtA[:], in0=dt_all[:], in1=bc(vec[:, 1, :], 1, [128, NB, 16]), op=ALU.mult), reads=["s_dt", "s_vec"], writes=["s_dtA"])
                op("dve", lambda v: v.memset(state[:], 0.0), writes=["s_state"])
                op("dve", lambda v: v.memset(prevb[:], 0.0), writes=["s_prevb"])

                def ssd_front(b0, bi, gi):
                    xc = xc_2[gi % 2]
                    b = b0 + bi
                    c0 = bi * 128
                    pb = b % 2
                    xs_tm = xs_tm_2[pb]
                    Btm = Btm_2[pb]
                    Rt = Rt_2[pb]
                    acs = acs_2[pb]
                    cbm = cbm_2[pb]
                    M = M_2[pb]
                    xdt = xdt_2[pb]
                    xdtS = xdtS_2[pb]
                    yoff = yoff_2[pb]
                    ysb = ysb_2[pb]
                    tmp = tmp_2[pb]
                    yn = yn_2[pb]
                    sm = sm_2[pb]
                    for cc in range(8):
                        op("pe", lambda p, cc=cc: p.transpose(out=ptr[:, cc * 128:(cc + 1) * 128], in_=xc[:, cc, c0:c0 + 128], identity=identb[:]),
                           reads=["s_xc_%d" % (gi % 2), "identb"], writes=["s_ptr"])
                    for g in range(2):
                        op("pe", lambda p, g=g: p.transpose(out=ptb[:, g * 128:(g + 1) * 128], in_=xc[:, 8 + g, c0:c0 + 128], identity=identb[:]),
                           reads=["s_xc_%d" % (gi % 2), "identb"], writes=["s_ptb"])
                    op("act", lambda a: a.activation(out=xs_tm[:], in_=ptr[:], func=AF.Copy), reads=["s_ptr"], writes=["s_xs_%d" % pb])
                    op("dve", lambda v: v.tensor_copy(out=Btm[:], in_=ptb[:]), reads=["s_ptb"], writes=["s_B_%d" % pb])
                    op("pool", lambda v: v.tensor_tensor(out=Rt[:], in0=bc(tri_le[:], 1, [128, 16, 128]), in1=bc(dtA[:, b, :], 2, [128, 16, 128]), op=ALU.mult),
                       reads=["tri_le", "s_dtA"], writes=["s_R_%d" % pb])
                    op("pe", lambda p: p.matmul(mB[:, 256:272], lhsT=tri_le[:], rhs=dtA[:, b, :], start=True, stop=True), reads=["tri_le", "s_dtA"], writes=["s_mB"])
                    op("pe", lambda p: p.matmul(mB[:, 272:288], lhsT=ones_f[:], rhs=dtA[:, b, :], start=True, stop=True), reads=["ones_f", "s_dtA"], writes=["s_mB"])
                    for g in range(2):
                        op("pe", lambda p, g=g: p.matmul(mB[:, g * 128:(g + 1) * 128], lhsT=xc[:, 8 + g, c0:c0 + 128], rhs=xc[:, 10 + g, c0:c0 + 128], start=True, stop=True),
                           reads=["s_xc_%d" % (gi % 2)], writes=["s_mB"])
                    op("dve", lambda v: v.tensor_copy(out=acs[:], in_=mB[:, 256:288]), reads=["s_mB"], writes=["s_acs_%d" % pb])
                    op("dve", lambda v: v.tensor_tensor(out=cbm[:], in0=mB[:, 0:256].rearrange("p (g l) -> p g l", g=2), in1=bc(tri_le[:], 1, [128, 2, 128]), op=ALU.mult),
                       reads=["s_mB", "tri_le"], writes=["s_cbm_%d" % pb])
                    op("act", lambda a: a.activation(out=sm[:, 0, :], in_=acs[:, 0:16], func=AF.Exp), reads=["s_acs_%d" % pb], writes=["s_sm0_%d" % pb])
                    op("dve", lambda v: v.tensor_tensor(out=sm[:, 3, :], in0=acs[:, 16:32], in1=acs[:, 0:16], op=ALU.subtract), reads=["s_acs_%d" % pb], writes=["s_sm3_%d" % pb])
                    op("act", lambda a: a.activation(out=sm[:, 1, :], in_=sm[:, 3, :], func=AF.Exp), reads=["s_sm3_%d" % pb], writes=["s_sm1_%d" % pb])
                    op("act", lambda a: a.activation(out=sm[:, 2, :], in_=acs[:, 16:32], func=AF.Exp), reads=["s_acs_%d" % pb], writes=["s_sm2_%d" % pb])
                    for hf in range(2):
                        for q in range(2):
                            h0_ = hf * 8 + q * 4
                            op("pe", lambda p, q=q, h0_=h0_: p.matmul(segp[:, q * 512:(q + 1) * 512], lhsT=smask[:], rhs=Rt[:, h0_:h0_ + 4, :].rearrange("p h l -> p (h l)"),
                                                                       start=True, stop=True), reads=["smask", "s_R_%d" % pb], writes=["s_seg"])
                        op("act", lambda a: a.activation(out=dec[:].rearrange("p h l -> p (h l)"), in_=segp[:], func=AF.Exp), reads=["s_seg"], writes=["s_dec"])
                        op("dve", lambda v, hf=hf: v.tensor_tensor(out=M[:, hf * 8:(hf + 1) * 8, :], in0=dec[:], in1=bc(cbm[:, hf, :], 1, [128, 8, 128]), op=ALU.mult),
                           reads=["s_dec", "s_cbm_%d" % pb], writes=["s_M_%d" % pb])
                    op("dve", lambda v: v.tensor_tensor(out=xdt[:].rearrange("p (h d) -> p h d", d=64), in0=xs_tm[:].rearrange("p (h d) -> p h d", d=64),
                                                        in1=bc(dt_all[:, b, :], 2, [128, 16, 64]), op=ALU.mult), reads=["s_xs_%d" % pb, "s_dt"], writes=["s_xdt_%d" % pb])
                    op("dve", lambda v: v.tensor_tensor(out=xdtS[:].rearrange("p (h d) -> p h d", d=64), in0=xdt[:].rearrange("p (h d) -> p h d", d=64),
                                                        in1=bc(sm[:, 1, :], 2, [128, 16, 64]), op=ALU.mult), reads=["s_xdt_%d" % pb, "s_sm1_%d" % pb], writes=["s_xdtS_%d" % pb])
                    for g in range(2):
                        op("pe", lambda p, g=g: p.matmul(mA[:, :], lhsT=xc[:, 10 + g, c0:c0 + 128], rhs=prevb[:, g * 512:(g + 1) * 512], start=True, stop=True),
                           reads=["s_xc_%d" % (gi % 2), "s_prevb"], writes=["s_mA"])
                        op("dve", lambda v, g=g: v.tensor_tensor(out=yoff[:, g * 512:(g + 1) * 512].rearrange("p (h d) -> p h d", d=64), in0=mA[:, :].rearrange("p (h d) -> p h d", d=64),
                                                                 in1=bc(sm[:, 0, g * 8:(g + 1) * 8], 2, [128, 8, 64]), op=ALU.mult), reads=["s_mA", "s_sm0_%d" % pb], writes=["s_yoff_%d" % pb])
                    for g in range(2):
                        for hh in range(8 * g, 8 * g + 8):
                            op("pe", lambda p, hh=hh, g=g: p.matmul(Yp[:, (hh - 8 * g) * 64:(hh - 8 * g + 1) * 64], lhsT=M[:, hh, :], rhs=xdt[:, hh * 64:(hh + 1) * 64], start=True, stop=True),
                               reads=["s_M_%d" % pb, "s_xdt_%d" % pb], writes=["s_Y"])
                        op("dve", lambda v, g=g: v.tensor_tensor(out=ysb[:, g * 512:(g + 1) * 512], in0=Yp[:, :], in1=yoff[:, g * 512:(g + 1) * 512], op=ALU.add),
                           reads=["s_Y", "s_yoff_%d" % pb], writes=["s_y_%d" % pb])
                    for g in range(2):
                        op("pe", lambda p, g=g: p.matmul(mA[:, :], lhsT=Btm[:, g * 128:(g + 1) * 128], rhs=xdtS[:, g * 512:(g + 1) * 512], start=True, stop=True),
                           reads=["s_B_%d" % pb, "s_xdtS_%d" % pb], writes=["s_mA"])
                        op("pool", lambda v, g=g: v.tensor_tensor(out=state[:, g * 512:(g + 1) * 512].rearrange("p (h d) -> p h d", d=64),
                                                                 in0=state[:, g * 512:(g + 1) * 512].rearrange("p (h d) -> p h d", d=64),
                                                                 in1=bc(sm[:, 2, g * 8:(g + 1) * 8], 2, [128, 8, 64]), op=ALU.mult), reads=["s_state", "s_sm2_%d" % pb], writes=["s_state"])
                        op("dve", lambda v, g=g: v.tensor_tensor(out=state[:, g * 512:(g + 1) * 512], in0=state[:, g * 512:(g + 1) * 512], in1=mA[:, :], op=ALU.add),
                           reads=["s_state", "s_mA"], writes=["s_state"])
                    op("act", lambda a: a.activation(out=prevb[:], in_=state[:], func=AF.Copy), reads=["s_state"], writes=["s_prevb"])

                def ssd_tail(b0, bi, gi):
                    b = b0 + bi
                    c0 = bi * 128
                    pb = b % 2
                    xs_tm = xs_tm_2[pb]
                    Btm = Btm_2[pb]
                    Rt = Rt_2[pb]
                    acs = acs_2[pb]
                    cbm = cbm_2[pb]
                    M = M_2[pb]
                    xdt = xdt_2[pb]
                    xdtS = xdtS_2[pb]
                    yoff = yoff_2[pb]
                    ysb = ysb_2[pb]
                    tmp = tmp_2[pb]
                    yn = yn_2[pb]
                    sm = sm_2[pb]
                    op("pool", lambda v: v.tensor_tensor(out=tmp[:].rearrange("p (h d) -> p h d", d=64), in0=xs_tm[:].rearrange("p (h d) -> p h d", d=64),
                                                        in1=bc(vec[:, 2, :], 2, [128, 16, 64]), op=ALU.mult), reads=["s_xs_%d" % pb, "s_vec"], writes=["s_tmp_%d" % pb])
                    op("dve", lambda v: v.tensor_tensor(out=ysb[:], in0=ysb[:], in1=tmp[:], op=ALU.add), reads=["s_y_%d" % pb, "s_tmp_%d" % pb], writes=["s_y_%d" % pb])
                    op("dve", lambda v: v.tensor_tensor(out=ysb[:], in0=ysb[:], in1=szg_2[gi % 2][:, bi, :], op=ALU.mult), reads=["s_y_%d" % pb, "s_szg_%d" % (gi % 2)], writes=["s_y_%d" % pb])
                    op("act", lambda a: a.activation(out=tmp[:], in_=ysb[:], func=AF.Square), reads=["s_y_%d" % pb], writes=["s_tmp_%d" % pb])
                    op("dve", lambda v: v.tensor_reduce(out=ss[:, 0:2], in_=tmp[:].rearrange("p (g d) -> p g d", g=2), axis=AX.X, op=ALU.add), reads=["s_tmp_%d" % pb], writes=["s_ss"])
                    op("act", lambda a: a.activation(out=ss[:, 2:4], in_=ss[:, 0:2], func=AF.Ln, scale=1.0 / 512, bias=RMS_EPS_T[:]), reads=["s_ss", "eps"], writes=["s_ss"])
                    op("act", lambda a: a.activation(out=ss[:, 2:4], in_=ss[:, 2:4], func=AF.Exp, scale=-0.5), reads=["s_ss"], writes=["s_ss"])
                    for g in range(2):
                        op("dve", lambda v, g=g: v.scalar_tensor_tensor(out=yn[:, g * 512:(g + 1) * 512], in0=ysb[:, g * 512:(g + 1) * 512], scalar=ss[:, 2 + g:3 + g],
                                                                        in1=nw[:, g * 512:(g + 1) * 512], op0=ALU.mult, op1=ALU.mult),
                           reads=["s_y_%d" % pb, "s_ss", "s_nw"], writes=["s_yn_%d" % pb])
                    for cc in range(8):
                        op("pe", lambda p, cc=cc: p.transpose(out=ptry[:, cc * 128:(cc + 1) * 128], in_=yn[:, cc * 128:(cc + 1) * 128], identity=identb[:]),
                           reads=["s_yn_%d" % pb, "identb"], writes=["s_ptry"])
                    op("act", lambda a: a.activation(out=ysT_2[gi % 2][:, :, c0:c0 + 128], in_=ptry[:].rearrange("p (c t) -> p c t", c=8), func=AF.Copy), reads=["s_ptry"], writes=["s_ysT_%d" % (gi % 2)])


                pending = []

                def flush_tail():
                    if not pending:
                        return
                    pb0, pbi, pgi, pnb, pt0, pnt = pending.pop()
                    ssd_tail(pb0, pbi, pgi)
                    if pbi == pnb - 1:
                        dma("pool", yssdT[:, :, pt0:pt0 + pnt].rearrange("c p t -> p c t"), ysT_2[pgi % 2][:, :, :pnt], reads=["s_ysT_%d" % (pgi % 2)], writes=[("yssdT", pgi)])

                groups = _blocks(NB, 4)

                def conv_dve(gi):
                    b0, nb = groups[gi]
                    t0, nt = b0 * 128, nb * 128
                    xr = xr_2[gi % 2]
                    kxr = "s_xr"
                    dma("sp", xr[:, :, 0:3 + nt], xbcT[:, :, 128 + t0 - 3:128 + t0 + nt].rearrange("c p t -> p c t"), writes=[kxr])
                    dma("sp", ztg[:, :nb, :], ztm[t0:t0 + nt, :].rearrange("(b p) n -> p b n", p=128), writes=["s_ztg"])
                    for cc in range(12):
                        op("dve", lambda v, cc=cc: v.tensor_scalar(out=acc12[:, cc, :nt], in0=xr[:, cc, 3:3 + nt], scalar1=cw[:, 3, cc:cc + 1], scalar2=None, op0=ALU.mult),
                           reads=[kxr, "s_cw"], writes=[("s_acc", cc)])
                        for k in range(3):
                            op("dve", lambda v, cc=cc, k=k: v.scalar_tensor_tensor(out=acc12[:, cc, :nt], in0=xr[:, cc, k:k + nt], scalar=cw[:, k, cc:cc + 1], in1=acc12[:, cc, :nt],
                                                                                   op0=ALU.mult, op1=ALU.add), reads=[kxr, "s_cw", ("s_acc", cc)], writes=[("s_acc", cc)])

                def conv_act(gi):
                    b0, nb = groups[gi]
                    nt = nb * 128
                    xc = xc_2[gi % 2]
                    for cc in range(12):
                        op("act", lambda a, cc=cc: a.activation(out=xc[:, cc, :nt], in_=acc12[:, cc, :nt], func=AF.Silu, bias=cbias[:, cc:cc + 1], scale=1.0),
                           reads=[("s_acc", cc), "s_cb"], writes=["s_xc_%d" % (gi % 2)])
                    op("act", lambda a: a.activation(out=szg_2[gi % 2][:, :nb, :], in_=ztg[:, :nb, :], func=AF.Silu), reads=["s_ztg"], writes=["s_szg_%d" % (gi % 2)])

                def merge_lists(l1, l2):
                    out_, i, j = [], 0, 0
                    n1, n2 = len(l1), len(l2)
                    while i < n1 or j < n2:
                        if j >= n2 or (i < n1 and i * n2 <= j * n1):
                            out_.append(l1[i])
                            i += 1
                        else:
                            out_.append(l2[j])
                            j += 1
                    return out_

                conv_dve(0)
                conv_act(0)
                for gi, (b0, nb) in enumerate(groups):
                    t0, nt = b0 * 128, nb * 128
                    for bi in range(nb):
                        lf = record(ssd_front, b0, bi, gi)
                        lt_ = record(flush_tail)
                        merged = merge_lists(lf, lt_)
                        nxt = (bi == nb - 1 and gi + 1 < len(groups))
                        if nxt:
                            merged = merge_lists(merged, record(conv_dve, gi + 1))
                        for it in merged:
                            replay(it)
                        if nxt:
                            conv_act(gi + 1)
                        pending.append((b0, bi, gi, nb, t0, nt))
                flush_tail()
                cx.barrier()


        def ln_fm(s_tiles, r, rkey, dst, dkey, gb, gbkey, nt, zero_pads, extra_reads=()):
            xr_ = list(extra_reads)
            sq, st1, st2, mean, rstd, lps, lpk = s_tiles
            for c in range(KC):
                op("act", lambda a, c=c: a.activation(out=sq[:, :nt], in_=r[:, c, :nt], func=AF.Square), reads=[rkey] + xr_, writes=["ln_sq"])
                op("pe", lambda p, c=c: p.matmul(lps[0][:, :nt], lhsT=ones_f[:], rhs=r[:, c, :nt], start=(c == 0), stop=(c == KC - 1)), reads=["ones_f", rkey] + xr_, writes=[lpk[0]])
                op("pe", lambda p, c=c: p.matmul(lps[1][:, :nt], lhsT=ones_f[:], rhs=sq[:, :nt], start=(c == 0), stop=(c == KC - 1)), reads=["ones_f", "ln_sq"], writes=[lpk[1]])
            op("act", lambda a: a.activation(out=mean[:, :nt], in_=lps[0][:, :nt], func=AF.Copy, scale=1.0 / D), reads=[lpk[0]], writes=["ln_mean"])
            op("dve", lambda v: v.tensor_tensor(out=st1[:, :nt], in0=mean[:, :nt], in1=mean[:, :nt], op=ALU.mult), reads=["ln_mean"], writes=["ln_st1"])
            op("dve", lambda v: v.scalar_tensor_tensor(out=st2[:, :nt], in0=lps[1][:, :nt], scalar=1.0 / D, in1=st1[:, :nt], op0=ALU.mult, op1=ALU.subtract),
               reads=[lpk[1], "ln_st1"], writes=["ln_st2"])
            op("act", lambda a: a.activation(out=st2[:, :nt], in_=st2[:, :nt], func=AF.Sqrt, bias=LN_EPS_T[:], scale=1.0), reads=["ln_st2", "eps"], writes=["ln_st2"])
            op("dve", lambda v: v.reciprocal(out=rstd[:, :nt], in_=st2[:, :nt]), reads=["ln_st2"], writes=["ln_rstd"])
            for c in range(KC):
                op("dve", lambda v, c=c: v.tensor_tensor(out=sq[:, :nt], in0=r[:, c, :nt], in1=mean[:, :nt], op=ALU.subtract), reads=[rkey, "ln_mean"] + xr_, writes=["ln_sq"])
                op("dve", lambda v, c=c: v.tensor_tensor(out=sq[:, :nt], in0=sq[:, :nt], in1=rstd[:, :nt], op=ALU.mult), reads=["ln_sq", "ln_rstd"], writes=["ln_sq"])
                op("act", lambda a, c=c: a.activation(out=dst[:, c, :nt], in_=sq[:, :nt], func=AF.Identity, scale=gb[:, 0, c:c + 1], bias=gb[:, 1, c:c + 1]),
                   reads=["ln_sq", gbkey], writes=[dkey])
            if zero_pads:
                op("dve", lambda v: v.memset(dst[:, :, 0:112], 0.0), writes=[dkey])

        def ln_tiles(s, n, lps=None, lpk=None):
            if lps is None:
                lps = [ps("ln_ps%d" % i, [128, n], F32, s) for i in range(2)]
                lpk = ["ln_ps0", "ln_ps1"]
            return (sb("ln_sq", [128, n], F32, s), sb("ln_st1", [128, n], F32, s), sb("ln_st2", [128, n], F32, s), sb("ln_mean", [128, n], F32, s),
                    sb("ln_rstd", [128, n], F32, s), lps, lpk)

        def load_gb(s, src, name):
            t = sb(name, [128, 2, KC], F32, s)
            with nc.allow_non_contiguous_dma(reason="tiny param"):
                dma("sp", t[:], src.rearrange("t (c p) -> p t c", p=128), writes=[name])
            return t

        def phase_merge(l, h_in, h_out):
            with ExitStack() as s:
                Wp = sb("m_Wp", [128, 16, D], BF16, s)
                Wo = sb("m_Wo", [128, 8, D], BF16, s)
                with ExitStack() as s2:
                    wstg = mk_wstg(s2)
                    load_w(Wp, w_proj[l], D, "m_Wp", 16, wstg)
                    load_w(Wo, w_out[l], D, "m_Wo", 8, wstg)
                    cx.barrier()
                gb = load_gb(s, ln_mix_gb[l], "m_gb")
                scw = sb("m_scw", [128, 3, 4], F32, s)
                with nc.allow_non_contiguous_dma(reason="tiny param"):
                    for k in range(3):
                        dma("sp", scw[:, k, :], sc_conv_w[l, k].rearrange("(c p) -> p c", p=128), writes=["m_scw"])
                NT = 512
                sct = sb("m_sct", [128, 12, 2 + NT], BF16, s)
                gt = sb("m_gt", [128, 24, NT], BF16, s)
                ya = sb("m_ya", [128, 4, NT], BF16, s)
                ys = sb("m_ys", [128, 8, NT], BF16, s)
                hf2 = [sb("m_h%d" % i, [128, KC, NT], F32, s) for i in range(2)]
                u = sb("m_u", [128, 2 + NT], F32, s)
                acc = sb("m_acc", [128, NT], F32, s)
                yc = sb("m_yc", [128, 4, NT], BF16, s)
                sg2 = [sb("m_sg%d" % i, [128, 3, NT], F32, s) for i in range(2)]
                mt2 = [sb("m_mt%d" % i, [128, 3, NT], F32, s) for i in range(2)]
                mg2 = [sb("m_mg%d" % i, [128, 8, NT], BF16, s) for i in range(2)]
                r = sb("m_r", [128, KC, NT], F32, s)
                pp2 = [[ps("m_p%d_%d" % (i, j), [128, NT], F32, s) for i in range(3)] for j in range(2)]
                po2 = [ps("m_po%d" % i, [128, NT], F32, s) for i in range(2)]
                lt = ln_tiles(s, NT, lps=[po2[0], po2[1]], lpk=["m_po0", "m_po1"])
                groups = _blocks(NB, 4)

                def stage_a(gi):
                    b0, nb = groups[gi]
                    t0, nt = b0 * 128, nb * 128
                    q = gi % 2
                    hf, kh = hf2[q], "m_h%d" % q
                    mg, kmg = mg2[q], "m_mg%d" % q
                    dma("sp", sct[:, :, 0:2 + nt], scT[:, :, 128 + t0 - 2:128 + t0 + nt].rearrange("c p t -> p c t"), writes=["m_sct"])
                    dma("sp", gt[:, :, :nt], gT[:, :, t0:t0 + nt].rearrange("c p t -> p c t"), writes=["m_gt"])
                    dma("sp", ya[:, :, :nt], yattT[:, :, t0:t0 + nt].rearrange("c p t -> p c t"), writes=["m_ya"])
                    dma("sp", ys[:, :, :nt], yssdT[:, :, t0:t0 + nt].rearrange("c p t -> p c t"), writes=["m_ys"])
                    dma("sp", hf[:, :, :nt], h_in[:, :, t0:t0 + nt].rearrange("c p t -> p c t"), writes=[kh])

                    def conv_cc(cc):
                        op("dve", lambda v: v.tensor_tensor(out=u[:, :2 + nt], in0=sct[:, 4 + cc, :2 + nt], in1=sct[:, 8 + cc, :2 + nt], op=ALU.mult), reads=["m_sct"], writes=["m_u"])
                        op("dve", lambda v: v.tensor_scalar(out=acc[:, :nt], in0=u[:, 2:2 + nt], scalar1=scw[:, 2, cc:cc + 1], scalar2=None, op0=ALU.mult),
                           reads=["m_u", "m_scw"], writes=["m_acc"])
                        for k in range(2):
                            op("dve", lambda v, k=k: v.scalar_tensor_tensor(out=acc[:, :nt], in0=u[:, k:k + nt], scalar=scw[:, k, cc:cc + 1], in1=acc[:, :nt], op0=ALU.mult, op1=ALU.add),
                               reads=["m_u", "m_scw", "m_acc"], writes=["m_acc"])
                        op("dve", lambda v: v.tensor_tensor(out=yc[:, cc, :nt], in0=acc[:, :nt], in1=sct[:, cc, 2:2 + nt], op=ALU.mult), reads=["m_acc", "m_sct"], writes=["m_yc"])

                    for cc in range(4):
                        conv_cc(cc)

                    def merge_n(n):
                        pn = n % 2
                        pp, sg, mt = pp2[pn], sg2[pn], mt2[pn]
                        branches = [(ya, "m_ya", 0, 4), (ys, "m_ys", 4, 8), (yc, "m_yc", 12, 4)]
                        for bi_, (src_t, skey, w0, nk) in enumerate(branches):
                            for kc in range(nk):
                                op("pe", lambda p, bi_=bi_, src_t=src_t, w0=w0, kc=kc, nk=nk: p.matmul(pp[bi_][:, :nt], lhsT=Wp[:, w0 + kc, n * 128:(n + 1) * 128], rhs=src_t[:, kc, :nt],
                                                                                                     start=(kc == 0), stop=(kc == nk - 1)),
                                   reads=["m_Wp", skey], writes=["m_p%d_%d" % (bi_, pn)])
                        for j in range(3):
                            op("act", lambda a, j=j: a.activation(out=sg[:, j, :nt], in_=gt[:, j * 8 + n, :nt], func=AF.Sigmoid), reads=["m_gt"], writes=[("m_sg", pn, j)])
                        for j in range(3):
                            op("dve", lambda v, j=j: v.tensor_tensor(out=mt[:, j, :nt], in0=pp[j][:, :nt], in1=sg[:, j, :nt], op=ALU.mult),
                               reads=["m_p%d_%d" % (j, pn), ("m_sg", pn, j)], writes=[("m_mt", pn, j)])
                        op("pool", lambda v: v.tensor_tensor(out=mt[:, 0, :nt], in0=mt[:, 0, :nt], in1=mt[:, 1, :nt], op=ALU.add),
                           reads=[("m_mt", pn, 0), ("m_mt", pn, 1)], writes=[("m_mt", pn, 0)])
                        op("pool", lambda v: v.tensor_tensor(out=mg[:, n, :nt], in0=mt[:, 0, :nt], in1=mt[:, 2, :nt], op=ALU.add),
                           reads=[("m_mt", pn, 0), ("m_mt", pn, 2)], writes=[(kmg, n)])

                    for n in range(8):
                        merge_n(n)

                def stage_b(gi):
                    b0, nb = groups[gi]
                    t0, nt = b0 * 128, nb * 128
                    q = gi % 2
                    hf, kh = hf2[q], "m_h%d" % q
                    mg, kmg = mg2[q], "m_mg%d" % q

                    def out_n(n):
                        po, pok_ = po2[n % 2], "m_po%d" % (n % 2)
                        for kc in range(8):
                            op("pe", lambda p, kc=kc: p.matmul(po[:, :nt], lhsT=Wo[:, kc, n * 128:(n + 1) * 128], rhs=mg[:, kc, :nt], start=(kc == 0), stop=(kc == 7)),
                               reads=["m_Wo", (kmg, kc)], writes=[pok_])
                        op("dve", lambda v: v.scalar_tensor_tensor(out=r[:, n, :nt], in0=hf[:, n, :nt], scalar=ALPHA, in1=po[:, :nt], op0=ALU.mult, op1=ALU.add),
                           reads=[kh, pok_], writes=["m_r"])

                    for n in range(8):
                        out_n(n)
                    ln_fm(lt, r, "m_r", hf, kh, gb, "m_gb", nt, zero_pads=(b0 == 0))
                    dma("pool", h_out[:, :, t0:t0 + nt].rearrange("c p t -> p c t"), hf[:, :, :nt], reads=[kh], writes=[("hT", gi)])

                for it in record(stage_a, 0):
                    replay(it)
                for gi in range(1, len(groups)):
                    la = record(stage_a, gi)
                    lb = record(stage_b, gi - 1)
                    replay_interleaved(la, lb)
                for it in record(stage_b, len(groups) - 1):
                    replay(it)
                cx.barrier()

        def phase_ffn_dense(l, h_in, h_out):
            with ExitStack() as s:
                NFF = FF_DENSE // 128
                Wg = sb("f_Wg", [128, KC, 2 * FF_DENSE], BF16, s)
                Wd = sb("f_Wd", [128, NFF, D], BF16, s)
                with ExitStack() as s2:
                    wstg = mk_wstg(s2)
                    load_w(Wg, dense_w_gu, 2 * FF_DENSE, "f_Wg", KC, wstg)
                    load_w(Wd, dense_w_down, D, "f_Wd", NFF, wstg)
                    cx.barrier()
                gb = load_gb(s, ln_ffn_gb[l], "f_gb")
                NT = 256
                hf2 = [sb("f_h%d" % i, [128, KC, NT], F32, s) for i in range(2)]
                hb2 = [sb("f_hb%d" % i, [128, KC, NT], BF16, s) for i in range(2)]
                act2 = [sb("f_act%d" % i, [128, NFF, NT], BF16, s) for i in range(2)]
                sgl2 = [sb("f_sgl%d" % i, [128, NT], F32, s) for i in range(2)]
                r = sb("f_r", [128, KC, NT], F32, s)
                lt = ln_tiles(s, NT)
                pg = [ps("f_pg%d" % i, [128, NT], F32, s) for i in range(2)]
                pu = [ps("f_pu%d" % i, [128, NT], F32, s) for i in range(2)]
                po2 = [ps("f_po%d" % i, [128, NT], F32, s) for i in range(2)]
                groups = _blocks(NB, 2)

                def load_cast(gi):
                    b0, nb = groups[gi]
                    t0, nt = b0 * 128, nb * 128
                    q = gi % 2
                    dma("sp", hf2[q][:, :, :nt], h_in[:, :, t0:t0 + nt].rearrange("c p t -> p c t"), writes=["f_h%d" % q])
                    for c in range(KC):
                        evac(c, hb2[q][:, c, :nt], hf2[q][:, c, :nt], ["f_h%d" % q], ["f_hb%d" % q])

                load_cast(0)
                for gi, (b0, nb) in enumerate(groups):
                    t0, nt = b0 * 128, nb * 128
                    q = gi % 2
                    hf, hb, act = hf2[q], hb2[q], act2[q]
                    kh, khb, kact = "f_h%d" % q, "f_hb%d" % q, "f_act%d" % q
                    for f in range(NFF):
                        i = f % 2
                        sgl = sgl2[i]
                        for c in range(KC):
                            op("pe", lambda p, c=c: p.matmul(pg[i][:, :nt], lhsT=Wg[:, c, f * 128:(f + 1) * 128], rhs=hb[:, c, :nt], start=(c == 0), stop=(c == KC - 1)),
                               reads=["f_Wg", khb], writes=["f_pg%d" % i])
                        for c in range(KC):
                            op("pe", lambda p, c=c: p.matmul(pu[i][:, :nt], lhsT=Wg[:, c, FF_DENSE + f * 128:FF_DENSE + (f + 1) * 128], rhs=hb[:, c, :nt], start=(c == 0), stop=(c == KC - 1)),
                               reads=["f_Wg", khb], writes=["f_pu%d" % i])
                        op("act", lambda a: a.activation(out=sgl[:, :nt], in_=pg[i][:, :nt], func=AF.Silu), reads=["f_pg%d" % i], writes=["f_sgl%d" % i])
                        op("dve", lambda v: v.tensor_tensor(out=act[:, f, :nt], in0=pu[i][:, :nt], in1=sgl[:, :nt], op=ALU.mult), reads=["f_pu%d" % i, "f_sgl%d" % i], writes=[(kact, f)])
                    if gi + 1 < len(groups):
                        load_cast(gi + 1)
                    for n in range(8):
                        po, kpo = po2[n % 2], "f_po%d" % (n % 2)
                        for f in range(NFF):
                            op("pe", lambda p, f=f: p.matmul(po[:, :nt], lhsT=Wd[:, f, n * 128:(n + 1) * 128], rhs=act[:, f, :nt], start=(f == 0), stop=(f == NFF - 1)),
                               reads=["f_Wd", (kact, f)], writes=[kpo])
                        op("dve", lambda v: v.scalar_tensor_tensor(out=r[:, n, :nt], in0=hf[:, n, :nt], scalar=ALPHA, in1=po[:, :nt], op0=ALU.mult, op1=ALU.add),
                           reads=[kh, kpo], writes=["f_r"])
                    ln_fm(lt, r, "f_r", hf, kh, gb, "f_gb", nt, zero_pads=(b0 == 0))
                    dma("pool", h_out[:, :, t0:t0 + nt].rearrange("c p t -> p c t"), hf[:, :, :nt], reads=[kh], writes=[("hT", gi)])
                cx.barrier()

        def phase_moe(l, h_in):
            with ExitStack() as s:
                NQF = 4
                SG = 1024 if TH >= 1024 else 256
                Wg = [sb("e_Wg%d" % i, [128, KC, 2, NQF * 128], BF16, s) for i in range(2)]
                Wd = [sb("e_Wd%d" % i, [128, NQF, D], BF16, s) for i in range(2)]
                gb = load_gb(s, ln_ffn_gb[l], "e_gb")
                wr = sb("e_wr", [128, KC, NEXP], F32, s)
                rb = sb("e_rb", [128, NEXP], F32, s)
                sel = sb("e_sel", [8, NEXP, 128], F32, s)
                with nc.allow_non_contiguous_dma(reason="tiny param"):
                    dma("sp", wr[:], router_w.rearrange("(c p) e -> p c e", p=128), writes=["e_wr"])
                    dma("sp", rb[:], router_b[0].partition_broadcast(128), writes=["e_rb"])
                op("pool", lambda g: g.memset(sel[:], 1.0), writes=["e_sel"])
                op("pool", lambda g: g.affine_select(out=sel[:], in_=sel[:], pattern=[[-1, NEXP], [0, 128]], base=0, channel_multiplier=1,
                                                      compare_op=ALU.is_equal, fill=0.0), reads=["e_sel"], writes=["e_sel"])
                hs = sb("e_hs", [128, KC, SG], F32, s)
                hb = sb("e_hb", [128, KC, SG], BF16, s)
                h2 = sb("e_h2", [128, KC, 256], F32, s)
                accm = sb("e_acc", [128, KC, SG], F32, s)
                cbc = sb("e_cbc", [128, NEXP, SG], BF16, s)
                lg = sb("e_lg", [128, 4, NEXP], F32, s)
                combT = sb("e_combT", [8, SG], F32, s)
                actb = [sb("e_act%d" % i, [128, NQF, 512], BF16, s) for i in range(2)]
                abi = [0]
                sgl2 = [sb("e_sgl%d" % i, [128, 512], F32, s) for i in range(2)]
                a12 = [sb("e_a1%d" % i, [128, 512], F32, s) for i in range(2)]
                osb = sb("e_osb", [128, D], F32, s)
                lt = ln_tiles(s, 256)
                pg = [ps("e_pg%d" % i, [128, 512], F32, s) for i in range(2)]
                pu = [ps("e_pu%d" % i, [128, 512], F32, s) for i in range(2)]
                po = ps("e_po", [128, 512], F32, s)
                po2 = [po, ps("e_po2", [128, 512], F32, s)]
                pok = ["e_po", "e_po2"]
                wic = [0]
                stg = [sb("e_stg%d" % i, [128, 4, NQF * 128], F32, s) for i in range(2)]
                stgi = [0]
                def moe_prologue(sg0):
                    for t1 in range(0, SG, 256):
                        for half in range(2):
                            ta = 128 + half * TH + sg0 + t1
                            gv = stg[half][:].rearrange("p c n -> p (c n)").rearrange("p (c n) -> p c n", c=KC)
                            dma("sp", gv, h_in[:, :, ta:ta + 256].rearrange("c p t -> p c t"), writes=["e_stg%d" % half])
                            if half == 0:
                                op("dve", lambda v, t1=t1, gv=gv: v.tensor_scalar(out=hs[:, :, t1:t1 + 256], in0=gv, scalar1=hsel[:, 0:1], scalar2=None, op0=ALU.mult),
                                   reads=["e_stg0", "hsel"], writes=["e_hs"])
                            else:
                                op("dve", lambda v, t1=t1, gv=gv: v.scalar_tensor_tensor(out=hs[:, :, t1:t1 + 256], in0=gv, scalar=hsel[:, 1:2], in1=hs[:, :, t1:t1 + 256], op0=ALU.mult, op1=ALU.add),
                                   reads=["e_stg1", "hsel", "e_hs"], writes=["e_hs"])
                    for c in range(KC):
                        evac(c, hb[:, c, :], hs[:, c, :], ["e_hs"], ["e_hb"])
                    for tb in range(SG // 128):
                        for c in range(KC):
                            op("pe", lambda p, c=c, tb=tb: p.matmul(po[:, 0:NEXP], lhsT=hs[:, c, tb * 128:(tb + 1) * 128], rhs=wr[:, c, :], start=(c == 0), stop=(c == KC - 1)),
                               reads=["e_hs", "e_wr"], writes=["e_po"])
                        op("dve", lambda v: v.tensor_tensor(out=lg[:, 0, :], in0=po[:, 0:NEXP], in1=rb[:], op=ALU.add), reads=["e_po", "e_rb"], writes=["e_lg"])
                        op("dve", lambda v: v.max(out=lg[:, 1, :], in_=lg[:, 0, :]), reads=["e_lg"], writes=["e_lg"])
                        op("dve", lambda v: v.tensor_scalar(out=lg[:, 2, :], in0=lg[:, 0, :], scalar1=lg[:, 1, 1:2], scalar2=None, op0=ALU.is_ge), reads=["e_lg"], writes=["e_lg"])
                        op("dve", lambda v: v.tensor_scalar(out=lg[:, 3, 0:1], in0=lg[:, 1, 0:1], scalar1=-1.0, scalar2=None, op0=ALU.mult), reads=["e_lg"], writes=["e_lg"])
                        op("act", lambda a: a.activation(out=lg[:, 0, :], in_=lg[:, 0, :], func=AF.Exp, bias=lg[:, 3, 0:1], scale=1.0), reads=["e_lg"], writes=["e_lg"])
                        op("dve", lambda v: v.tensor_tensor(out=lg[:, 0, :], in0=lg[:, 0, :], in1=lg[:, 2, :], op=ALU.mult), reads=["e_lg"], writes=["e_lg"])
                        op("dve", lambda v: v.tensor_reduce(out=lg[:, 3, 1:2], in_=lg[:, 0, :], axis=AX.X, op=ALU.add), reads=["e_lg"], writes=["e_lg"])
                        op("dve", lambda v: v.reciprocal(out=lg[:, 3, 1:2], in_=lg[:, 3, 1:2]), reads=["e_lg"], writes=["e_lg"])
                        op("dve", lambda v: v.tensor_scalar(out=lg[:, 0, :], in0=lg[:, 0, :], scalar1=lg[:, 3, 1:2], scalar2=None, op0=ALU.mult), reads=["e_lg"], writes=["e_lg"])
                        op("pe", lambda p: p.transpose(out=po[0:8, 128:256], in_=lg[:, 0, :], identity=ident[:]), reads=["e_lg", "ident"], writes=["e_po"])
                        op("dve", lambda v, tb=tb: v.tensor_copy(out=combT[:, tb * 128:(tb + 1) * 128], in_=po[0:8, 128:256]), reads=["e_po"], writes=["e_combT"])
                    for e in range(NEXP):
                        for t1 in range(0, SG, 512):
                            n1 = min(512, SG - t1)
                            op("pe", lambda p, e=e, t1=t1, n1=n1: p.matmul(po[:, :n1], lhsT=sel[:, e, :], rhs=combT[:, t1:t1 + n1], start=True, stop=True),
                               reads=["e_sel", "e_combT"], writes=["e_po"])
                            op("act", lambda a, e=e, t1=t1, n1=n1: a.activation(out=cbc[:, e, t1:t1 + n1], in_=po[:, :n1], func=AF.Copy), reads=["e_po"], writes=["e_cbc"])
                def moe_experts(sg0):
                    for e in range(NEXP):
                        for qq in range(FF_EXP // 128 // NQF):
                            w = wic[0] % 2
                            wic[0] += 1
                            f0 = qq * NQF * 128
                            for gu in range(2):
                                srcw = moe_w_gu[e][:, gu * FF_EXP + f0:gu * FF_EXP + f0 + NQF * 128].rearrange("(c p) n -> p c n", p=128)
                                for hc in range(2):
                                    sgi = stgi[0] % 2
                                    stgi[0] += 1
                                    dma("sp", stg[sgi][:], srcw[:, hc * 4:(hc + 1) * 4, :], writes=["e_stg%d" % sgi])
                                    op("act", lambda a, gu=gu, sgi=sgi, hc=hc: a.activation(out=Wg[w][:, hc * 4:(hc + 1) * 4, gu, :], in_=stg[sgi][:], func=AF.Copy),
                                       reads=["e_stg%d" % sgi], writes=[("e_Wg%d" % w, gu, hc)])
                            srcw = moe_w_down[e][f0:f0 + NQF * 128, :].rearrange("(c p) n -> p c n", p=128)
                            for hc in range(2):
                                sgi = stgi[0] % 2
                                stgi[0] += 1
                                stv = stg[sgi][:].rearrange("p c n -> p (c n)").rearrange("p (c n) -> p c n", c=2)
                                dma("sp", stv, srcw[:, hc * 2:(hc + 1) * 2, :], writes=["e_stg%d" % sgi])
                                op("act", lambda a, sgi=sgi, hc=hc, stv=stv: a.activation(out=Wd[w][:, hc * 2:(hc + 1) * 2, :], in_=stv, func=AF.Copy),
                                   reads=["e_stg%d" % sgi], writes=[("e_Wd%d" % w, hc)])
                            for t1 in range(0, SG, 512):
                                n1 = min(512, SG - t1)
                                ab = abi[0] % 2
                                for f in range(NQF):
                                    i = f % 2
                                    for c in range(KC):
                                        op("pe", lambda p, c=c, f=f: p.matmul(pg[i][:, :n1], lhsT=Wg[w][:, c, 0, f * 128:(f + 1) * 128], rhs=hb[:, c, t1:t1 + n1], start=(c == 0), stop=(c == KC - 1)),
                                           reads=[("e_Wg%d" % w, 0, 0), ("e_Wg%d" % w, 0, 1), "e_hb"], writes=["e_pg%d" % i])
                                    for c in range(KC):
                                        op("pe", lambda p, c=c, f=f: p.matmul(pu[i][:, :n1], lhsT=Wg[w][:, c, 1, f * 128:(f + 1) * 128], rhs=hb[:, c, t1:t1 + n1], start=(c == 0), stop=(c == KC - 1)),
                                           reads=[("e_Wg%d" % w, 1, 0), ("e_Wg%d" % w, 1, 1), "e_hb"], writes=["e_pu%d" % i])
                                    sgl, a1 = sgl2[i], a12[i]
                                    op("act", lambda a: a.activation(out=sgl[:, :n1], in_=pg[i][:, :n1], func=AF.Silu), reads=["e_pg%d" % i], writes=["e_sgl%d" % i])
                                    op("dve", lambda v: v.tensor_tensor(out=a1[:, :n1], in0=pu[i][:, :n1], in1=sgl[:, :n1], op=ALU.mult), reads=["e_pu%d" % i, "e_sgl%d" % i], writes=["e_a1%d" % i])
                                    op("dve", lambda v, f=f: v.tensor_tensor(out=actb[ab][:, f, :n1], in0=a1[:, :n1], in1=cbc[:, e, t1:t1 + n1], op=ALU.mult), reads=["e_a1%d" % i, "e_cbc"], writes=["e_act%d" % ab])
                                for n in range(8):
                                    pq = po2[n % 2]
                                    for f in range(NQF):
                                        op("pe", lambda p, f=f: p.matmul(pq[:, :n1], lhsT=Wd[w][:, f, n * 128:(n + 1) * 128], rhs=actb[ab][:, f, :n1], start=(f == 0), stop=(f == NQF - 1)),
                                           reads=[("e_Wd%d" % w, 0), ("e_Wd%d" % w, 1), "e_act%d" % ab], writes=[pok[n % 2]])
                                    if e == 0 and qq == 0:
                                        op("dve", lambda v, n=n: v.tensor_copy(out=accm[:, n, t1:t1 + n1], in_=pq[:, :n1]), reads=[pok[n % 2]], writes=[("e_acc", n, t1)])
                                    else:
                                        op("dve", lambda v, n=n: v.tensor_tensor(out=accm[:, n, t1:t1 + n1], in0=accm[:, n, t1:t1 + n1], in1=pq[:, :n1], op=ALU.add), reads=[pok[n % 2], ("e_acc", n, t1)], writes=[("e_acc", n, t1)])
                                abi[0] += 1
                acck = [("e_acc", n, t5) for n in range(8) for t5 in range(0, SG, 512)]

                def moe_res(sg0):
                    for n in range(8):
                        op("dve", lambda v, n=n: v.scalar_tensor_tensor(out=accm[:, n, :], in0=hs[:, n, :], scalar=ALPHA, in1=accm[:, n, :], op0=ALU.mult, op1=ALU.add),
                           reads=["e_hs"] + [("e_acc", n, t5) for t5 in range(0, SG, 512)], writes=[("e_acc", n, t5) for t5 in range(0, SG, 512)] + ["e_res"])

                def moe_tail(sg0):
                    for t1 in range(0, SG, 256):
                        ln_fm(lt, accm[:, :, t1:t1 + 256], "e_res", h2, "e_h2o", gb, "e_gb", 256, zero_pads=False, extra_reads=acck)
                        for tb in range(2):
                            for c in range(KC):
                                op("pe", lambda p, c=c, tb=tb: p.transpose(out=pg[c // 4][:, (c % 4) * 128:(c % 4 + 1) * 128], in_=h2[:, c, tb * 128:(tb + 1) * 128], identity=ident[:]),
                                   reads=["e_h2o", "ident"], writes=["e_pg%d" % (c // 4)])
                            for hh in range(2):
                                evac(hh, osb[:, hh * 512:(hh + 1) * 512], pg[hh][:, :], ["e_pg%d" % hh], ["e_osb"])
                            r0 = sg0 + t1 + tb * 128
                            dma("pool", out[r0:r0 + 128, :], osb[:], reads=["e_osb"], writes=[("out", r0)])

                sgs = list(range(0, TH, SG))
                moe_prologue(sgs[0])
                moe_experts(sgs[0])
                for ki in range(1, len(sgs)):
                    moe_res(sgs[ki - 1])
                    lp_ = record(moe_prologue, sgs[ki])
                    lt_ = record(moe_tail, sgs[ki - 1])
                    replay_interleaved(lp_, lt_)
                    moe_experts(sgs[ki])
                moe_res(sgs[-1])
                moe_tail(sgs[-1])
                cx.barrier()

        stages = set(STAGES)
        if "ln_in" in stages:
            phase_ln_in(hT[0])
        if "win0" in stages:
            phase_win(0, hT[0])
        if "cprep0" in stages:
            phase_cprep(0)
        if "attn0" in stages:
            phase_attn(0)
        if "ssd0" in stages:
            phase_ssd(0)
        if "merge0" in stages:
            phase_merge(0, hT[0], hT[1])
        if "ffn0" in stages:
            phase_ffn_dense(0, hT[1], hT[0])
        if "layer1" in stages:
            phase_win(1, hT[0])
            phase_cprep(1)
            phase_attn(1)
            phase_ssd(1)
            phase_merge(1, hT[0], hT[1])
        if "moe" in stages:
            phase_moe(1, hT[1])

        cx.barrier()
    nc._in_names = in_names
    return nc


ALL_STAGES = ("ln_in", "win0", "cprep0", "attn0", "ssd0", "merge0", "ffn0", "layer1", "moe")


def kernel(**inputs):
    x = np.asarray(inputs["x"])
    B, S, _ = x.shape
    NB = S // 128 + 1
    HB = (NB - 1) // 2
    nc = build_program(NB, STAGES=ALL_STAGES)
    maps = prep_inputs(inputs, NB)
    maps = [{k: m[k] for k in nc._in_names} for m in maps]
    res = run_bass_kernel_spmd(nc, maps, core_ids=list(range(8)))
    out = np.zeros((B, S, D), np.float32)
    for c in range(8):
        b, half = c // 2, c % 2
        out[b, half * HB * 128:(half + 1) * HB * 128] = np.asarray(res.results[c]["out"])
    return out


def prep_inputs(inp, NB):
    S = (NB - 1) * 128
    f = lambda a: np.ascontiguousarray(np.asarray(a, dtype=np.float32))
    x = f(inp["x"])
    B = x.shape[0]
    shared = {
        "ln_in_gb": f(np.stack([inp["ln_in_g"], inp["ln_in_b"]], 0)),
        "w_in": f(inp["w_in"]),
        "b_forget": f(inp["b_forget"]),
        "ssd_conv_w": f(inp["ssd_conv_w"]),
        "ssd_conv_b": f(inp["ssd_conv_b"]),
        "ssd_vec": f(np.stack([inp["ssd_dt_bias"], inp["ssd_a_log"], inp["ssd_d"]], 1)),
        "ssd_norm_w": f(inp["ssd_norm_w"]),
        "sc_conv_w": f(inp["sc_conv_w"]),
        "w_proj": f(np.concatenate([inp["w_proj_attn"], inp["w_proj_ssd"], inp["w_proj_conv"]], 1)),
        "w_out": f(inp["w_out"]),
        "ln_mix_gb": f(np.stack([inp["ln_mix_g"], inp["ln_mix_b"]], 1)),
        "dense_w_gu": f(inp["dense_w_gu"][0]),
        "dense_w_down": f(inp["dense_w_down"][0]),
        "router_w": f(inp["router_w"][0]),
        "router_b": f(np.asarray(inp["router_b"]).reshape(1, NEXP)),
        "moe_w_gu": f(inp["moe_w_gu"][0]),
        "moe_w_down": f(inp["moe_w_down"][0]),
        "ln_ffn_gb": f(np.stack([inp["ln_ffn_g"], inp["ln_ffn_b"]], 1)),
    }
    maps = []
    for c in range(8):
        b, half = (c // 2) % B, c % 2
        xin = np.zeros((NB * 128, D), np.float32)
        xin[112:128] = f(inp["meta_tokens"])
        xin[128:] = x[b, :S]
        hs = np.zeros((128, 2), np.float32)
        hs[:, half] = 1.0
        m = dict(shared)
        m["xin"] = xin
        m["halfsel"] = hs
        maps.append(m)
    return maps
```
